# Optimizing a Trainium2 kernel written in Bass

```python
import math
import jax, jax.numpy as jnp
from jax import lax
import numpy as np

D_MODEL = 1024
BATCH = 8
SEQ = 4096
DEPTH = 2

D_MIX = 1024
GDN_HEADS = 4
GDN_HEAD_DIM = 128
GDN_WIDTH = 512
GDN_CONV = 4
GDN_CHUNK = 64
DSA_HEADS = 4
DSA_HEAD_DIM = 64
DSA_WIDTH = 256
DSA_Q_RANK = 256
DSA_KV_RANK = 128
IDX_HEADS = 4
IDX_DIM = 64
DSA_TOPK_MAX = 256
Q_BLOCK = 128
CONV_CH = 256
CONV_WIDTH = 31
MEM_LEN = 256
XA_HEADS = 4
XA_HEAD_DIM = 128
XA_WIDTH = 512
D_FF = 4 * D_MODEL
EPS = 1e-6

IN_SPLITS = (3 * GDN_WIDTH, GDN_WIDTH, GDN_HEADS, GDN_HEADS,
             DSA_Q_RANK, DSA_KV_RANK, IDX_DIM, IDX_HEADS, 2 * CONV_CH)
N_IN = 3 * GDN_WIDTH + GDN_WIDTH + 2 * GDN_HEADS + DSA_Q_RANK + DSA_KV_RANK + IDX_DIM + IDX_HEADS + 2 * CONV_CH

kernel_name = "hybrid_gdn_dsa_conformer_block"


def rms_norm(x, g):
    xf = x.astype(jnp.float32)
    y = xf * lax.rsqrt(jnp.mean(xf * xf, axis=-1, keepdims=True) + EPS)
    return (y * g.astype(jnp.float32)).astype(x.dtype)


def layer_norm(x, g, b):
    xf = x.astype(jnp.float32)
    mu = jnp.mean(xf, axis=-1, keepdims=True)
    xc = xf - mu
    var = jnp.mean(xc * xc, axis=-1, keepdims=True)
    y = xc * lax.rsqrt(var + EPS) * g.astype(jnp.float32) + b.astype(jnp.float32)
    return y.astype(x.dtype)


def l2_norm(x):
    return x * lax.rsqrt(jnp.sum(x * x, axis=-1, keepdims=True) + EPS)


def split_cols(t, sizes):
    out = []
    off = 0
    for n in sizes:
        out.append(t[..., off:off + n])
        off += n
    return out


def causal_depthwise_conv(x, w):
    k, c = w.shape
    return lax.conv_general_dilated(
        x, w[:, None, :].astype(x.dtype), window_strides=(1,), padding=((k - 1, 0),),
        dimension_numbers=("NWC", "WIO", "NWC"), feature_group_count=c)


def gated_delta_rule(q, k, v, g, beta):
    b, s, h, dk = q.shape
    dv = v.shape[-1]
    c = GDN_CHUNK
    n = s // c
    q = q * (dk ** -0.5)

    def chunk(t):
        return jnp.moveaxis(t.reshape((b, n, c) + t.shape[2:]), 3, 2)

    qc, kc, vc = chunk(q), chunk(k), chunk(v)
    gc, bc = chunk(g), chunk(beta)
    decay = jnp.cumsum(gc, axis=-1)
    tril = jnp.tril(jnp.ones((c, c), bool))
    strict = jnp.tril(jnp.ones((c, c), bool), -1)
    diff = decay[..., :, None] - decay[..., None, :]
    gamma = jnp.where(tril, jnp.exp(jnp.where(tril, diff, 0.0)), 0.0)
    kb = kc * bc[..., None]
    a_mat = jnp.where(strict, jnp.einsum("bnhid,bnhjd->bnhij", kb, kc) * gamma, 0.0)
    lhs = a_mat + jnp.eye(c, dtype=a_mat.dtype)
    rhs = jnp.concatenate([vc * bc[..., None], kb * jnp.exp(decay)[..., None]], axis=-1)
    sol = lax.linalg.triangular_solve(lhs, rhs, left_side=True, lower=True, unit_diagonal=True)
    u, w = sol[..., :dv], sol[..., dv:]
    qk = jnp.where(tril, jnp.einsum("bnhid,bnhjd->bnhij", qc, kc) * gamma, 0.0)

    def step(state, inp):
        q_i, k_i, u_i, w_i, qk_i, dec_i = inp
        v_new = u_i - jnp.einsum("bhcd,bhde->bhce", w_i, state)
        o = (jnp.einsum("bhcd,bhde->bhce", q_i * jnp.exp(dec_i)[..., None], state)
             + jnp.einsum("bhij,bhje->bhie", qk_i, v_new))
        last = dec_i[..., -1]
        k_dec = k_i * jnp.exp(last[..., None] - dec_i)[..., None]
        state = state * jnp.exp(last)[..., None, None] + jnp.einsum("bhcd,bhce->bhde", k_dec, v_new)
        return state, o

    xs = tuple(jnp.moveaxis(t, 1, 0) for t in (qc, kc, u, w, qk, decay))
    _, o = lax.scan(step, jnp.zeros((b, h, dk, dv), jnp.float32), xs)
    o = jnp.swapaxes(jnp.moveaxis(o, 0, 1), 2, 3)
    return o.reshape(b, s, h, dv)


def gdn_mixer(qkv, z, a_g, b_g, conv_w, a_log, dt_bias, norm_g):
    b, s, _ = qkv.shape
    dt = qkv.dtype
    qkv = jax.nn.silu(causal_depthwise_conv(qkv, conv_w)).astype(jnp.float32)
    q, k, v = jnp.split(qkv, 3, axis=-1)
    shp = (b, s, GDN_HEADS, GDN_HEAD_DIM)
    q = l2_norm(q.reshape(shp))
    k = l2_norm(k.reshape(shp))
    v = v.reshape(shp)
    g = -jnp.exp(a_log.astype(jnp.float32)) * jax.nn.softplus(a_g.astype(jnp.float32) + dt_bias.astype(jnp.float32))
    beta = jax.nn.sigmoid(b_g.astype(jnp.float32))
    o = gated_delta_rule(q, k, v, g, beta)
    o = rms_norm(o, norm_g) * jax.nn.silu(z.astype(jnp.float32).reshape(shp))
    return o.reshape(b, s, GDN_WIDTH).astype(dt)


def dsa_mixer(c_q, c_kv, k_idx, w_idx, w_qb, w_qi, w_kvb, q_norm_g, k_norm_g):
    b, s, _ = c_q.shape
    q = rms_norm((c_q @ w_qb).reshape(b, s, DSA_HEADS, DSA_HEAD_DIM), q_norm_g)
    kv = c_kv @ w_kvb
    k = rms_norm(kv[..., :DSA_HEAD_DIM], k_norm_g)
    v = kv[..., DSA_HEAD_DIM:]
    q_idx = (c_q @ w_qi).reshape(b, s, IDX_HEADS, IDX_DIM)
    w_idx = w_idx.astype(jnp.float32) * (IDX_HEADS ** -0.5 * IDX_DIM ** -0.5)
    k_idx = k_idx.astype(jnp.float32)
    topk = min(DSA_TOPK_MAX, s // 4)
    nb = s // Q_BLOCK
    pos = jnp.arange(s, dtype=jnp.int32)

    def blk(t):
        return jnp.moveaxis(t.reshape((b, nb, Q_BLOCK) + t.shape[2:]), 1, 0)

    def attend(inp):
        q_b, qi_b, w_b, t_b = inp
        causal = pos[None, :] <= t_b[:, None]
        idx_logits = jnp.einsum("bqhd,bsd->bqhs", qi_b.astype(jnp.float32), k_idx)
        score = jnp.einsum("bqh,bqhs->bqs", w_b, jax.nn.relu(idx_logits))
        score = jnp.where(causal[None], score, -jnp.inf)
        _, sel = lax.top_k(score, topk)
        k_sel = jax.vmap(lambda kk, ii: kk[ii])(k, sel)
        v_sel = jax.vmap(lambda vv, ii: vv[ii])(v, sel)
        valid = sel <= t_b[None, :, None]
        logits = jnp.einsum("bqhd,bqkd->bqhk", q_b.astype(jnp.float32), k_sel.astype(jnp.float32)) * (DSA_HEAD_DIM ** -0.5)
        logits = jnp.where(valid[:, :, None, :], logits, -jnp.inf)
        p = jax.nn.softmax(logits, axis=-1)
        return jnp.einsum("bqhk,bqkd->bqhd", p, v_sel.astype(jnp.float32)).astype(q_b.dtype)

    o = lax.map(attend, (blk(q), blk(q_idx), blk(w_idx), pos.reshape(nb, Q_BLOCK)))
    return jnp.moveaxis(o, 0, 1).reshape(b, s, DSA_WIDTH)


def conv_mixer(ug, dw, dw_b, ln_g, ln_b):
    u, gate = jnp.split(ug, 2, axis=-1)
    y = u * jax.nn.sigmoid(gate)
    y = causal_depthwise_conv(y, dw) + dw_b
    y = layer_norm(y, ln_g, ln_b)
    return jax.nn.silu(y)


def memory_cross_attention(h, mem_n, wq, wkv, q_g, k_g, wo):
    b, s, _ = h.shape
    m = mem_n.shape[1]
    q = rms_norm((h @ wq).reshape(b, s, XA_HEADS, XA_HEAD_DIM), q_g)
    kv = mem_n @ wkv
    k = rms_norm(kv[..., :XA_WIDTH].reshape(b, m, XA_HEADS, XA_HEAD_DIM), k_g)
    v = kv[..., XA_WIDTH:].reshape(b, m, XA_HEADS, XA_HEAD_DIM)
    logits = jnp.einsum("bshd,bmhd->bhsm", q.astype(jnp.float32), k.astype(jnp.float32)) * (XA_HEAD_DIM ** -0.5)
    p = jax.nn.softmax(logits, axis=-1)
    o = jnp.einsum("bhsm,bmhd->bshd", p, v.astype(jnp.float32)).astype(h.dtype)
    return o.reshape(b, s, XA_WIDTH) @ wo


def setup_inputs(seed: int = 0) -> dict:
    key = jax.random.key(seed)
    ks = jax.random.split(key, 32)
    f32 = jnp.float32

    def nrm(k, shape, scale):
        return jax.random.normal(k, shape, f32) * scale

    def gain(k, shape):
        return 1.0 + 0.02 * jax.random.normal(k, shape, f32)

    dt = jnp.exp(jax.random.uniform(ks[5], (DEPTH, GDN_HEADS), f32, math.log(1e-3), math.log(1e-1)))
    dt_bias = dt + jnp.log(-jnp.expm1(-dt))
    return {
        "x": nrm(ks[0], (BATCH, SEQ, D_MODEL), 1.0),
        "mem": nrm(ks[1], (BATCH, MEM_LEN, D_MODEL), 1.0),
        "norm_mix": gain(ks[2], (DEPTH, D_MODEL)),
        "w_in": nrm(ks[3], (DEPTH, D_MODEL, N_IN), D_MODEL ** -0.5),
        "gdn_conv": nrm(ks[4], (DEPTH, GDN_CONV, 3 * GDN_WIDTH), GDN_CONV ** -0.5),
        "gdn_a_log": jnp.log(jax.random.uniform(ks[6], (DEPTH, GDN_HEADS), f32, 1.0, 16.0)),
        "gdn_dt_bias": dt_bias,
        "gdn_norm": gain(ks[7], (DEPTH, GDN_HEAD_DIM)),
        "dsa_w_qb": nrm(ks[8], (DEPTH, DSA_Q_RANK, DSA_WIDTH), DSA_Q_RANK ** -0.5),
        "dsa_w_qi": nrm(ks[9], (DEPTH, DSA_Q_RANK, IDX_HEADS * IDX_DIM), DSA_Q_RANK ** -0.5),
        "dsa_w_kvb": nrm(ks[10], (DEPTH, DSA_KV_RANK, 2 * DSA_HEAD_DIM), DSA_KV_RANK ** -0.5),
        "dsa_q_norm": gain(ks[11], (DEPTH, DSA_HEAD_DIM)),
        "dsa_k_norm": gain(ks[12], (DEPTH, DSA_HEAD_DIM)),
        "conv_dw": nrm(ks[13], (DEPTH, CONV_WIDTH, CONV_CH), CONV_WIDTH ** -0.5),
        "conv_dw_b": nrm(ks[14], (DEPTH, CONV_CH), 0.02),
        "conv_ln_g": gain(ks[15], (DEPTH, CONV_CH)),
        "conv_ln_b": nrm(ks[16], (DEPTH, CONV_CH), 0.02),
        "w_out": nrm(ks[17], (DEPTH, D_MIX, D_MODEL), D_MIX ** -0.5),
        "norm_mem": gain(ks[18], (D_MODEL,)),
        "norm_cross": gain(ks[19], (DEPTH, D_MODEL)),
        "xa_wq": nrm(ks[20], (DEPTH, D_MODEL, XA_WIDTH), D_MODEL ** -0.5),
        "xa_wkv": nrm(ks[21], (DEPTH, D_MODEL, 2 * XA_WIDTH), D_MODEL ** -0.5),
        "xa_q_norm": gain(ks[22], (DEPTH, XA_HEAD_DIM)),
        "xa_k_norm": gain(ks[23], (DEPTH, XA_HEAD_DIM)),
        "xa_wo": nrm(ks[24], (DEPTH, XA_WIDTH, D_MODEL), XA_WIDTH ** -0.5),
        "norm_mlp": gain(ks[25], (DEPTH, D_MODEL)),
        "mlp_w1": nrm(ks[26], (DEPTH, D_MODEL, D_FF), D_MODEL ** -0.5),
        "mlp_w2": nrm(ks[27], (DEPTH, D_FF, D_MODEL), D_FF ** -0.5),
    }


def reference(x, mem, norm_mix, w_in, gdn_conv, gdn_a_log, gdn_dt_bias, gdn_norm,
              dsa_w_qb, dsa_w_qi, dsa_w_kvb, dsa_q_norm, dsa_k_norm,
              conv_dw, conv_dw_b, conv_ln_g, conv_ln_b, w_out,
              norm_mem, norm_cross, xa_wq, xa_wkv, xa_q_norm, xa_k_norm, xa_wo,
              norm_mlp, mlp_w1, mlp_w2):
    mem_n = rms_norm(mem, norm_mem)
    for l in range(DEPTH):
        h = rms_norm(x, norm_mix[l])
        proj = h @ w_in[l]
        qkv, z, a_g, b_g, c_q, c_kv, k_idx, w_idx, ug = split_cols(proj, IN_SPLITS)
        y_a = gdn_mixer(qkv, z, a_g, b_g, gdn_conv[l], gdn_a_log[l], gdn_dt_bias[l], gdn_norm[l])
        y_b = dsa_mixer(c_q, c_kv, k_idx, w_idx, dsa_w_qb[l], dsa_w_qi[l], dsa_w_kvb[l],
                        dsa_q_norm[l], dsa_k_norm[l])
        y_c = conv_mixer(ug, conv_dw[l], conv_dw_b[l], conv_ln_g[l], conv_ln_b[l])
        x = x + jnp.concatenate([y_a, y_b, y_c], axis=-1) @ w_out[l]
        x = x + memory_cross_attention(rms_norm(x, norm_cross[l]), mem_n, xa_wq[l], xa_wkv[l],
                                       xa_q_norm[l], xa_k_norm[l], xa_wo[l])
        hm = rms_norm(x, norm_mlp[l]) @ mlp_w1[l]
        x = x + jnp.square(jax.nn.relu(hm)) @ mlp_w2[l]
    return x
```

```python
import contextlib
import numpy as np
import concourse.bass as bass
import concourse.mybir as mybir
from concourse.bass_utils import run_bass_kernel_spmd

F32 = mybir.dt.float32
BF16 = mybir.dt.bfloat16
AF = mybir.ActivationFunctionType
ALU = mybir.AluOpType
AX = mybir.AxisListType

D = 1024
NIN = 3020
MEM = 256
EPS = 1e-6
C_A, C_B, C_CQ, C_CKV, C_KI, C_WI, C_UG = 2048, 2052, 2056, 2312, 2440, 2504, 2508
NEG_NC = -2.0e38
NEG_SEL = -3.0e38
ST_A = 128
ST_B = 256


class Buf:
    __slots__ = ("w", "r", "dsem", "dcnt", "name")

    def __init__(self, name=""):
        self.w = {}
        self.r = {}
        self.dsem = None
        self.dcnt = 0
        self.name = name


class K:
    def __init__(self, nc, es):
        self.nc = nc
        self.es = es
        self.eng = {"pe": nc.tensor, "act": nc.scalar, "dve": nc.vector, "pool": nc.gpsimd, "sp": nc.sync}
        self.sem = {e: es.enter_context(nc.semaphore("s_" + e)) for e in ("pe", "act", "dve", "pool")}
        self.cnt = {e: 0 for e in self.sem}
        self.waited = {e: {} for e in self.eng}
        self.nsem = 0
        self.dbufs = []
        self.ninst = 0

    def _wait(self, e, deps):
        need = {}
        for key, (sem, val) in deps:
            if key == "pe" and e == "pe":
                continue
            if need.get(key, (None, 0))[1] < val:
                need[key] = (sem, val)
        wd = self.waited[e]
        for key, (sem, val) in need.items():
            if wd.get(key, 0) >= val:
                continue
            self.eng[e].wait_ge(sem, val)
            wd[key] = val
            self.ninst += 1

    @staticmethod
    def _deps(reads, writes):
        deps = []
        for b in reads:
            deps.extend(b.w.items())
        for b in writes:
            deps.extend(b.w.items())
            deps.extend(b.r.items())
        return deps

    @staticmethod
    def _mark(tokkey, tok, reads, writes):
        for b in reads:
            b.r[tokkey] = tok
        for b in writes:
            if b.r:
                b.w = {}
                b.r = {}
            b.w[tokkey] = tok

    def op(self, e, fn, R=(), W=(), **kw):
        self._wait(e, self._deps(R, W))
        ins = fn(**kw)
        self.cnt[e] += 1
        ins.then_inc(self.sem[e], 1)
        self.ninst += 1
        self._mark(e, (self.sem[e], self.cnt[e]), R, W)

    def dma(self, q, out, in_, sb, R=(), W=(), group=False, **kw):
        if sb.dsem is None:
            sb.dsem = self.es.enter_context(self.nc.semaphore("d%d" % self.nsem))
            self.nsem += 1
            self.dbufs.append(sb)
        key = "d%d" % id(sb)
        deps = self._deps(R, W)
        if group:
            deps = [d for d in deps if d[0] != key]
        self._wait(q, deps)
        ins = self.eng[q].dma_start(out=out, in_=in_, **kw)
        sb.dcnt += 16
        ins.then_inc(sb.dsem, 16)
        self.ninst += 1
        self._mark(key, (sb.dsem, sb.dcnt), R, W)

    def barrier(self):
        deps = [("d%d" % id(b), (b.dsem, b.dcnt)) for b in self.dbufs]
        for e in self.sem:
            deps.append((e, (self.sem[e], self.cnt[e])))
        for e in self.eng:
            self._wait(e, [d for d in deps if d[0] != e])

    def finish(self):
        deps = [("d%d" % id(b), (b.dsem, b.dcnt)) for b in self.dbufs]
        for e in self.sem:
            deps.append((e, (self.sem[e], self.cnt[e])))
        self._wait("sp", deps)


def bc(ap, shape):
    return ap.to_broadcast(list(shape))


def build(S=4096, NL=2, stop=None):
    NT = S // 128
    nc = bass.Bass("TRN2", target_bir_lowering=False)
    es = contextlib.ExitStack()

    def din(name, shape):
        return nc.dram_tensor(name, list(shape), F32, kind="ExternalInput").ap()

    x_d = din("x", [S, D])
    mem_d = din("mem", [MEM, D])
    norm_mix_d = din("norm_mix", [2, D])
    w_in_d = din("w_in", [2, D, NIN])
    gdn_conv_d = din("gdn_conv", [2, 4, 1536])
    a_log_d = din("gdn_a_log", [2, 4])
    dt_bias_d = din("gdn_dt_bias", [2, 4])
    gdn_norm_d = din("gdn_norm", [2, 128])
    w_qb_d = din("dsa_w_qb", [2, 256, 256])
    w_qi_d = din("dsa_w_qi", [2, 256, 256])
    w_kvb_d = din("dsa_w_kvb", [2, 128, 128])
    dq_norm_d = din("dsa_q_norm", [2, 64])
    dk_norm_d = din("dsa_k_norm", [2, 64])
    conv_dw_d = din("conv_dw", [2, 31, 256])
    conv_b_d = din("conv_dw_b", [2, 256])
    ln_g_d = din("conv_ln_g", [2, 256])
    ln_b_d = din("conv_ln_b", [2, 256])
    w_out_d = din("w_out", [2, D, D])
    norm_mem_d = din("norm_mem", [D])
    norm_cross_d = din("norm_cross", [2, D])
    wq_d = din("xa_wq", [2, D, 512])
    wkv_d = din("xa_wkv", [2, D, 1024])
    xq_norm_d = din("xa_q_norm", [2, 128])
    xk_norm_d = din("xa_k_norm", [2, 128])
    wo_d = din("xa_wo", [2, 512, D])
    norm_mlp_d = din("norm_mlp", [2, D])
    w1_d = din("mlp_w1", [2, D, 4096])
    w2_d = din("mlp_w2", [2, 4096, D])
    out_d = nc.dram_tensor("out", [S, D], F32, kind="ExternalOutput").ap()

    k = K(nc, es)
    V, ACT, PE, POOL = nc.vector, nc.scalar, nc.tensor, nc.gpsimd

    def sb(name, shape, dt=F32):
        t = es.enter_context(nc.sbuf_tensor(name, list(shape), dt))
        return t, Buf(name)

    psb = []
    for i in range(8):
        t = es.enter_context(nc.psum_tensor("ps%d" % i, [128, 512], F32))
        psb.append((t, Buf("ps%d" % i)))
    pctr = [0]

    def psum():
        t, b = psb[pctr[0] % 7]
        pctr[0] += 1
        return t, b

    def bfv(t):
        return t[:, :].bitcast(BF16)

    ones_f, onesB = sb("ones_f", [128, 512])
    ident_f, identB = sb("ident_f", [128, 128])
    ident_b, identbB = sb("ident_b", [128, 128], BF16)
    ones128, o128B = sb("ones128", [128, 128])
    ones256, o256B = sb("ones256", [128, 128])
    onesS, onesSB = sb("onesS", [128, 128])
    sel, selB = sb("sel", [4, 4, 128])
    epsc, epsB = sb("epsc", [128, 1])
    onec, oneB = sb("onec", [128, 1])
    FILL0 = POOL.to_reg(0.0)
    FILLM = POOL.to_reg(-1.0e30)
    FILLNC = POOL.to_reg(NEG_NC)
    k.op("pool", POOL.memset, W=[onesB], ap=ones_f[:, :], constant=1.0)
    k.op("pool", POOL.memset, W=[o128B], ap=ones128[:, :], constant=1.0 / 128)
    k.op("pool", POOL.memset, W=[o256B], ap=ones256[:, :], constant=1.0 / 256)
    k.op("pool", POOL.memset, W=[onesSB], ap=onesS[:, :], constant=1.0)
    k.op("pool", POOL.memset, W=[epsB], ap=epsc[:, :], constant=EPS)
    k.op("pool", POOL.memset, W=[oneB], ap=onec[:, :], constant=1.0)
    k.op("pool", POOL.affine_select, R=[onesB], W=[identB], out=ident_f[:, :], in_=ones_f[:, 0:128],
         pattern=[[-1, 128]], compare_op=ALU.is_equal, fill=FILL0, base=0, channel_multiplier=1)
    k.op("pool", POOL.affine_select, R=[onesB], W=[identbB], out=ident_b[:, :], in_=ones_f[:, 0:128],
         pattern=[[-1, 128]], compare_op=ALU.is_equal, fill=FILL0, base=0, channel_multiplier=1)
    k.op("pool", POOL.affine_select, R=[onesB], W=[selB], out=sel[:, :, :],
         in_=ones_f[0:4, :].rearrange("p (a b) -> p a b", a=4),
         pattern=[[-1, 4], [0, 128]], compare_op=ALU.is_equal, fill=FILL0, base=0, channel_multiplier=1)

    NXT = 2
    xts = [sb("xt%d" % i, [128, D]) for i in range(NXT)]
    hb, hbB = sb("hb", [128, D], BF16)
    hT, hTB = sb("hT", [128, 8, ST_B], BF16)
    st5 = [sb("st5_%d" % i, [128, 8]) for i in range(4)]
    stage = [None, None]
    sctr = [0]
    gcol, gcolB = sb("gcol", [128, 8])

    def load_gcol(vec_ap):
        k.dma("sp", gcol[:, :], vec_ap.rearrange("(c p) -> p c", p=128), gcolB, W=[gcolB],
              allow_slow_non_contiguous=True)

    cast_rr = [0]

    def cast_rows(dst_ap, dstB, src_rows_ap, ncols, scol=None):
        for c0 in range(0, ncols, 2048):
            cw = min(2048, ncols - c0)
            stg, stgB = stage[sctr[0] % 2]
            sctr[0] += 1
            k.dma("sp", stg[:, :cw], src_rows_ap[:, c0:c0 + cw], stgB, W=[stgB])
            e = ("pool", "act")[cast_rr[0] % 2]
            cast_rr[0] += 1
            R = [stgB] + ([gcolB] if scol is not None else [])
            if e == "pool":
                if scol is None:
                    k.op("pool", POOL.tensor_copy, R=R, W=[dstB], out=dst_ap[:, c0:c0 + cw], in_=stg[:, :cw])
                else:
                    k.op("pool", POOL.tensor_scalar, R=R, W=[dstB], out=dst_ap[:, c0:c0 + cw], in0=stg[:, :cw],
                         scalar1=scol, scalar2=1.0, op0=ALU.mult, op1=ALU.mult)
            else:
                if scol is None:
                    k.op("act", ACT.copy, R=R, W=[dstB], out=dst_ap[:, c0:c0 + cw], in_=stg[:, :cw])
                else:
                    k.op("act", ACT.activation, R=R, W=[dstB], out=dst_ap[:, c0:c0 + cw], in_=stg[:, :cw],
                         func=AF.Copy, scale=scol)

    def norm_T(xt, xtB, ncol0):
        s5, s5B = st5[ncol0 // 128 % 4]
        k.op("act", ACT.activation, R=[xtB], W=[hbB, s5B], out=hb[:, :], in_=xt[:, :], func=AF.Square,
             accum_out=s5[:, 0:1])
        k.op("dve", V.tensor_scalar, R=[s5B], W=[s5B], out=s5[:, 1:2], in0=s5[:, 0:1], scalar1=1.0 / D,
             scalar2=EPS, op0=ALU.mult, op1=ALU.add)
        k.op("act", ACT.activation, R=[s5B], W=[s5B], out=s5[:, 2:3], in_=s5[:, 1:2], func=AF.Sqrt)
        k.op("dve", V.reciprocal, R=[s5B], W=[s5B], out=s5[:, 3:4], in_=s5[:, 2:3])
        k.op("act", ACT.activation, R=[xtB, s5B], W=[hbB], out=hb[:, :], in_=xt[:, :], func=AF.Copy,
             scale=s5[:, 3:4])
        pt, pB = psum()
        for c in range(8):
            k.op("pe", PE.transpose, R=[hbB, identbB], W=[pB], out=bfv(pt)[:, c * 128:(c + 1) * 128],
                 in_=hb[:, c * 128:(c + 1) * 128], identity=ident_b[:, :])
        k.op("dve", V.tensor_copy, R=[pB], W=[hTB], out=hT[:, :, ncol0:ncol0 + 128],
             in_=bfv(pt).rearrange("p (c t) -> p c t", c=8))

    xdB = [Buf("xd%d" % t) for t in range(NT)]

    for l in range(NL):
        xsrc = x_d if l == 0 else out_d
        with contextlib.ExitStack() as pa:
            def sba(name, shape, dt=F32):
                t = pa.enter_context(nc.sbuf_tensor("%s_l%d" % (name, l), list(shape), dt))
                return t, Buf(name)

            ST = ST_A
            NTS = ST // 128
            NST = S // ST
            WA, WAB = sba("WA", [128, 33536], BF16)
            prep = contextlib.ExitStack()
            for i in range(2):
                stage[i] = (prep.enter_context(nc.sbuf_tensor("stgA%d_%d" % (l, i), [128, 2048], F32)), Buf("stg"))
            w_in = WA[:, 0:8 * NIN].rearrange("p (c n) -> p c n", c=8)
            o1 = 8 * NIN
            w_out = WA[:, o1:o1 + 8 * D].rearrange("p (c n) -> p c n", c=8)
            o2 = o1 + 8 * D
            w_qb = WA[:, o2:o2 + 512].rearrange("p (c n) -> p c n", c=2)
            w_qi = WA[:, o2 + 512:o2 + 1024].rearrange("p (c n) -> p c n", c=2)
            w_kvb = WA[:, o2 + 1024:o2 + 1152]
            load_gcol(norm_mix_d[l])
            for c in range(8):
                cast_rows(w_in[:, c, :], WAB, w_in_d[l, c * 128:(c + 1) * 128, :], NIN, gcol[:, c:c + 1])
            for c in range(8):
                cast_rows(w_out[:, c, :], WAB, w_out_d[l, c * 128:(c + 1) * 128, :], D)
            for c in range(2):
                cast_rows(w_qb[:, c, :], WAB, w_qb_d[l, c * 128:(c + 1) * 128, :], 256)
                cast_rows(w_qi[:, c, :], WAB, w_qi_d[l, c * 128:(c + 1) * 128, :], 256)
            cast_rows(w_kvb, WAB, w_kvb_d[l, :, :], 128)
            k.barrier()
            prep.close()
            cw, cwB = sba("cw", [128, 12, 4])
            for c in range(12):
                k.dma("sp", cw[:, c, :], gdn_conv_d[l][:, c * 128:(c + 1) * 128].rearrange("j p -> p j"), cwB,
                      W=[cwB], group=True, allow_slow_non_contiguous=True)
            cdw, cdwB = sba("cdw", [128, 2, 31])
            for c in range(2):
                k.dma("sp", cdw[:, c, :], conv_dw_d[l][:, c * 128:(c + 1) * 128].rearrange("j p -> p j"), cdwB,
                      W=[cdwB], group=True, allow_slow_non_contiguous=True)
            cvec, cvecB = sba("cvec", [128, 3, 2])
            for i, v in enumerate((conv_b_d, ln_g_d, ln_b_d)):
                k.dma("sp", cvec[:, i, :], v[l].rearrange("(c p) -> p c", p=128), cvecB, W=[cvecB], group=True,
                      allow_slow_non_contiguous=True)
            gnc, gncB = sba("gnc", [128, 1])
            k.dma("sp", gnc[:, :], gdn_norm_d[l].rearrange("(p o) -> p o", o=1), gncB, W=[gncB])
            gv, gvB = sba("gv", [4, 4])
            k.dma("sp", gv[:, 0:1], a_log_d[l].rearrange("(p o) -> p o", o=1), gvB, W=[gvB])
            k.dma("sp", gv[:, 1:2], dt_bias_d[l].rearrange("(p o) -> p o", o=1), gvB, W=[gvB], group=True)
            k.op("act", ACT.activation, R=[gvB], W=[gvB], out=gv[:, 2:3], in_=gv[:, 0:1], func=AF.Exp)
            k.op("dve", V.tensor_scalar, R=[gvB], W=[gvB], out=gv[:, 3:4], in0=gv[:, 2:3], scalar1=-1.0,
                 scalar2=None, op0=ALU.mult)
            gq8, gq8B = sba("gq8", [128, 64])
            gkb, gkbB = sba("gkb", [128, 64])
            k.dma("sp", gq8[:, :], dq_norm_d[l].partition_broadcast(128), gq8B, W=[gq8B])
            k.dma("sp", gkb[:, :], dk_norm_d[l].partition_broadcast(128), gkbB, W=[gkbB])
            k.op("dve", V.tensor_scalar, R=[gq8B], W=[gq8B], out=gq8[:, :], in0=gq8[:, :], scalar1=0.125,
                 scalar2=None, op0=ALU.mult)
            dgc, dgcB = sba("dgc", [128, 2, 31, 128], BF16)
            for i in range(2):
                for j in range(31):
                    k.op("pool", POOL.tensor_scalar, R=[identB, cdwB], W=[dgcB], out=dgc[:, i, j, :],
                         in0=ident_f[:, :], scalar1=cdw[:, i, j:j + 1], scalar2=1.0, op0=ALU.mult, op1=ALU.mult)
            pre, preB = sba("pre", [128, 12, 3 + ST])
            gl, glB = sba("gl", [128, 2, 30 + ST], BF16)
            k.op("pool", POOL.memset, W=[preB], ap=pre[:, :, :], constant=0.0)
            k.op("pool", POOL.memset, W=[glB], ap=gl[:, :, :], constant=0.0)
            qkvT, _ = sba("qkvT", [128, 12, ST])
            qkvB = [Buf("qkv%d" % i) for i in range(12)]
            zs, zsB = sba("zs", [128, 4, ST])
            cqT, cqB = sba("cqT", [128, 2, ST], BF16)
            ckvT, ckvB = sba("ckvT", [128, ST], BF16)
            yT, yTB = sba("yT", [128, 8, ST], BF16)
            Sst = [sba("S%d" % i, [128, 4, 128]) for i in range(2)]
            k.op("pool", POOL.memset, W=[Sst[0][1]], ap=Sst[0][0][:, :, :], constant=0.0)
            KKT, _ = sba("KKT", [128, S], BF16)
            kkB = [Buf("kk%d" % t) for t in range(NT)]
            Vc, _ = sba("Vc", [128, NT, 65], BF16)
            vcB = [Buf("vc%d" % t) for t in range(NT)]
            VcI, VcIB = sba("VcI", [128, 1], BF16)
            work, workB = sba("work", [128, S])
            rows, rowsB = sba("rows", [4, 8, ST])
            cols = [sba("cols%d" % i, [128, 16]) for i in range(2)]
            kkt, kktB = sba("kkt", [128, 2, 128], BF16)
            wab, wabB = sba("wab", [128, 2, 8])
            gd = {n: sba("gd_" + n, [128, 512]) for n in
                  ("Kbd", "Kdec", "Vb", "QKT", "qd", "P0", "P1", "PT0", "PT1", "RT0", "RT1")}
            QQs = [sba("QQ%d" % i, [128, 512], BF16) for i in range(2)]
            mks = [sba("mk%d" % i, [128, 512], BF16) for i in range(2)]
            mkc = [0]
            print("sbuf remaining (phase A)", nc.sbuf_bytes_remaining)
            NSC = 8
            scr = [sba("scr%d" % i, [128, 512]) for i in range(NSC)]
            scrc = [0]

            def scratch():
                t, b = scr[scrc[0] % NSC]
                scrc[0] += 1
                return t, b
            NSB = 6
            scb = [sba("scb%d" % i, [128, 512], BF16) for i in range(NSB)]
            scbc = [0]

            def scratchb():
                t, b = scb[scbc[0] % NSB]
                scbc[0] += 1
                return t, b
            m8s = [sba("m8_%d" % i, [128, 8]) for i in range(2)]
            sm = [sba("sm%d" % i, [128, 16]) for i in range(4)]
            smc = [0]

            def small():
                t, b = sm[smc[0] % 4]
                smc[0] += 1
                return t, b

            for t in range(NT):
                k.op("pool", POOL.memset, W=[vcB[t]], ap=Vc[:, t, 64:65], constant=1.0)

            def v3(ap, a=4):
                return ap.rearrange("p (a b) -> p a b", a=a)

            for s_i in range(NST):
                if stop == "W":
                    break
                t0 = s_i * ST
                tiles = [s_i * NTS + j for j in range(NTS)]
                for j in range(NTS):
                    xt, xtB = xts[j]
                    k.dma("sp", xt[:, :], xsrc[t0 + j * 128:t0 + (j + 1) * 128, :], xtB, R=[xdB[tiles[j]]],
                          W=[xtB])
                    norm_T(xt, xtB, j * 128)

                def featmajor(col0, M):
                    pt, pB = psum()
                    for c in range(8):
                        k.op("pe", PE.matmul, R=[WAB, hTB], W=[pB], out=pt[:M, :ST],
                             lhsT=w_in[:, c, col0:col0 + M], rhs=hT[:, c, 0:ST], start=(c == 0), stop=(c == 7))
                    return pt, pB

                for i in range(12):
                    pt, pB = featmajor(i * 128, 128)
                    k.op("act", ACT.copy, R=[pB], W=[preB], out=pre[:, i, 3:3 + ST], in_=pt[:, :ST])
                    k.op("dve", V.tensor_scalar, R=[preB, cwB], W=[qkvB[i]], out=qkvT[:, i, :], in0=pre[:, i, 0:ST],
                         scalar1=cw[:, i, 0:1], scalar2=None, op0=ALU.mult)
                    for j in range(1, 4):
                        k.op("dve", V.scalar_tensor_tensor, R=[preB, cwB, qkvB[i]], W=[qkvB[i]], out=qkvT[:, i, :],
                             in0=pre[:, i, j:j + ST], scalar=cw[:, i, j:j + 1], in1=qkvT[:, i, :], op0=ALU.mult,
                             op1=ALU.add)
                    k.op("act", ACT.activation, R=[qkvB[i]], W=[qkvB[i]], out=qkvT[:, i, :], in_=qkvT[:, i, :],
                         func=AF.Silu)
                k.op("dve", V.tensor_copy, R=[preB], W=[preB], out=pre[:, :, 0:3], in_=pre[:, :, ST:ST + 3])
                for i in range(8):
                    sq, sqB = scratch()
                    k.op("act", ACT.activation, R=[qkvB[i]], W=[sqB], out=sq[:, :ST], in_=qkvT[:, i, :],
                         func=AF.Square)
                    pt, pB = psum()
                    k.op("pe", PE.matmul, R=[onesSB, sqB], W=[pB], out=pt[:, :ST], lhsT=onesS[:, :],
                         rhs=sq[:, :ST], start=True, stop=True)
                    k.op("act", ACT.activation, R=[pB, epsB], W=[sqB], out=sq[:, :ST], in_=pt[:, :ST],
                         func=AF.Sqrt, bias=epsc[:, 0:1])
                    k.op("dve", V.reciprocal, R=[sqB], W=[sqB], out=sq[:, :ST], in_=sq[:, :ST])
                    k.op("dve", V.scalar_tensor_tensor, R=[qkvB[i], sqB], W=[qkvB[i]], out=qkvT[:, i, :],
                         in0=qkvT[:, i, :], scalar=(128 ** -0.5 if i < 4 else 1.0), in1=sq[:, :ST],
                         op0=ALU.mult, op1=ALU.mult)
                for i in range(4):
                    pt, pB = featmajor(1536 + i * 128, 128)
                    k.op("act", ACT.activation, R=[pB], W=[zsB], out=zs[:, i, :], in_=pt[:, :ST], func=AF.Silu)
                for i in range(2):
                    pt, pB = featmajor(C_CQ + i * 128, 128)
                    k.op("act", ACT.copy, R=[pB], W=[cqB], out=cqT[:, i, :], in_=pt[:, :ST])
                pt, pB = featmajor(C_CKV, 128)
                k.op("act", ACT.copy, R=[pB], W=[ckvB], out=ckvT[:, :], in_=pt[:, :ST])
                for i in range(2):
                    pu, puB = featmajor(C_UG + i * 128, 128)
                    pg, pgB = featmajor(C_UG + 256 + i * 128, 128)
                    sg, sgB = scratch()
                    k.op("act", ACT.activation, R=[pgB], W=[sgB], out=sg[:, :ST], in_=pg[:, :ST], func=AF.Sigmoid)
                    k.op("dve", V.tensor_tensor, R=[puB, sgB], W=[glB], out=gl[:, i, 30:30 + ST], in0=pu[:, :ST],
                         in1=sg[:, :ST], op=ALU.mult)
                for j in range(NTS):
                    pt, pB = psum()
                    for c in range(8):
                        k.op("pe", PE.matmul, R=[WAB, hTB], W=[pB], out=pt[:, :68],
                             lhsT=hT[:, c, j * 128:(j + 1) * 128], rhs=w_in[:, c, C_KI:C_KI + 68],
                             start=(c == 0), stop=(c == 7))
                    k.op("act", ACT.copy, R=[pB], W=[kktB], out=kkt[:, j, 64:128], in_=pt[:, 0:64])
                    k.op("act", ACT.activation, R=[pB], W=[wabB], out=wab[:, j, 0:4], in_=pt[:, 64:68],
                         func=AF.Abs, scale=1.0 / 16)
                    k.op("act", ACT.activation, R=[pB], W=[wabB], out=wab[:, j, 4:8], in_=pt[:, 64:68],
                         func=AF.Sign)
                if stop == "A1":
                    break
                pa_, paB = featmajor(C_A, 4)
                pb_, pbB = featmajor(C_B, 4)
                r = rows
                k.op("act", ACT.activation, R=[paB, gvB], W=[rowsB], out=r[:, 0, :], in_=pa_[:4, :ST],
                     func=AF.Abs, bias=gv[:, 1:2])
                k.op("act", ACT.activation, R=[rowsB], W=[rowsB], out=r[:, 1, :], in_=r[:, 0, :], func=AF.Exp,
                     scale=-1.0)
                k.op("act", ACT.activation, R=[rowsB, oneB], W=[rowsB], out=r[:, 1, :], in_=r[:, 1, :], func=AF.Ln,
                     bias=onec[0:4, 0:1])
                k.op("dve", V.tensor_scalar, R=[paB, gvB], W=[rowsB], out=r[:, 2, :], in0=pa_[:4, :ST],
                     scalar1=gv[:, 1:2], scalar2=0.0, op0=ALU.add, op1=ALU.max)
                k.op("dve", V.tensor_tensor, R=[rowsB], W=[rowsB], out=r[:, 2, :], in0=r[:, 2, :], in1=r[:, 1, :],
                     op=ALU.add)
                k.op("dve", V.tensor_scalar, R=[rowsB, gvB], W=[rowsB], out=r[:, 2, :], in0=r[:, 2, :],
                     scalar1=gv[:, 3:4], scalar2=None, op0=ALU.mult)
                for ch in range(NTS):
                    cs = slice(ch * 128, (ch + 1) * 128)
                    k.op("dve", V.tensor_tensor_scan, R=[rowsB, onesB], W=[rowsB], out=r[:, 3, cs],
                         data0=ones_f[0:4, 0:128], data1=r[:, 2, cs], initial=0.0, op0=ALU.mult, op1=ALU.add)
                k.op("act", ACT.activation, R=[pbB], W=[rowsB], out=r[:, 4, :], in_=pb_[:4, :ST], func=AF.Sigmoid)
                k.op("act", ACT.activation, R=[rowsB], W=[rowsB], out=r[:, 5, :], in_=r[:, 3, :], func=AF.Exp)
                for ch in range(NTS):
                    cs = slice(ch * 128, (ch + 1) * 128)
                    k.op("dve", V.tensor_scalar, R=[rowsB], W=[rowsB], out=r[:, 6, cs], in0=r[:, 3, cs],
                         scalar1=r[:, 3, ch * 128 + 127:ch * 128 + 128], scalar2=-1.0, op0=ALU.subtract,
                         op1=ALU.mult)
                k.op("act", ACT.activation, R=[rowsB], W=[rowsB], out=r[:, 6, :], in_=r[:, 6, :], func=AF.Exp)
                k.op("dve", V.tensor_tensor, R=[rowsB], W=[rowsB], out=r[:, 7, :], in0=r[:, 4, :], in1=r[:, 5, :],
                     op=ALU.mult)

                if stop == "A2":
                    break
                for ch in range(NTS):
                    cs = slice(ch * 128, (ch + 1) * 128)
                    col, colB = cols[ch]
                    pt, pB = psum()
                    for qi_, rq in enumerate((3, 4, 7, 6)):
                        k.op("pe", PE.matmul, R=[rowsB, identB], W=[pB], out=pt[:, qi_ * 4:(qi_ + 1) * 4],
                             lhsT=r[:, rq, cs], rhs=ident_f[0:4, 0:4], start=True, stop=True)
                    k.op("act", ACT.copy, R=[pB], W=[colB], out=col[:, :], in_=pt[:, 0:16])
                    c_b, c_beta, c_be, c_elb = (col[:, 0:4], col[:, 4:8], col[:, 8:12], col[:, 12:16])
                    prb, prbB = psum()
                    pre_, preB_ = psum()
                    for h in range(4):
                        k.op("pe", PE.matmul, R=[selB, rowsB], W=[prbB], out=prb[:, h * 128:(h + 1) * 128],
                             lhsT=sel[:, h, :], rhs=r[:, 3, cs], start=True, stop=True)
                    for h in range(4):
                        k.op("pe", PE.matmul, R=[selB, rowsB], W=[preB_], out=pre_[:, h * 128:(h + 1) * 128],
                             lhsT=sel[:, h, :], rhs=r[:, 5, cs], start=True, stop=True)
                    qd, qdB = gd["qd"]
                    k.op("dve", V.tensor_tensor, R=[preB_] + qkvB[0:4], W=[qdB], out=v3(qd[:, :]),
                         in0=qkvT[:, 0:4, cs], in1=v3(pre_[:, :]), op=ALU.mult)
                    ebl, eblB = small()
                    k.op("dve", V.tensor_copy, R=[preB_], W=[eblB], out=ebl[:, 0:4], in_=v3(pre_[:, :])[:, :, 127])
                    pk, pkB = psum()
                    pv, pvB = psum()
                    for h in range(4):
                        k.op("pe", PE.transpose, R=[qkvB[4 + h], identB], W=[pkB], out=pk[:, h * 128:(h + 1) * 128],
                             in_=qkvT[:, 4 + h, cs], identity=ident_f[:, :])
                    for h in range(4):
                        k.op("pe", PE.transpose, R=[qkvB[8 + h], identB], W=[pvB], out=pv[:, h * 128:(h + 1) * 128],
                             in_=qkvT[:, 8 + h, cs], identity=ident_f[:, :])
                    Kbd, KbdB = gd["Kbd"]
                    Kdec, KdecB = gd["Kdec"]
                    Vb, VbB = gd["Vb"]
                    k.op("dve", V.tensor_tensor, R=[pkB, colB], W=[KbdB], out=v3(Kbd[:, :]), in0=v3(pk[:, :]),
                         in1=bc(c_be.unsqueeze(2), [128, 4, 128]), op=ALU.mult)
                    k.op("dve", V.tensor_tensor, R=[pkB, colB], W=[KdecB], out=v3(Kdec[:, :]), in0=v3(pk[:, :]),
                         in1=bc(c_elb.unsqueeze(2), [128, 4, 128]), op=ALU.mult)
                    k.op("dve", V.tensor_tensor, R=[pvB, colB], W=[VbB], out=v3(Vb[:, :]), in0=v3(pv[:, :]),
                         in1=bc(c_beta.unsqueeze(2), [128, 4, 128]), op=ALU.mult)
                    if stop == "A3a":
                        break
                    pg1, pg1B = psum()
                    pg2, pg2B = psum()
                    for h in range(4):
                        k.op("pe", PE.matmul, R=[qkvB[4 + h]], W=[pg1B], out=pg1[:, h * 128:(h + 1) * 128],
                             lhsT=qkvT[:, 4 + h, cs], rhs=qkvT[:, 4 + h, cs], start=True, stop=True)
                    for h in range(4):
                        k.op("pe", PE.matmul, R=[qkvB[4 + h], qkvB[h]], W=[pg2B], out=pg2[:, h * 128:(h + 1) * 128],
                             lhsT=qkvT[:, 4 + h, cs], rhs=qkvT[:, h, cs], start=True, stop=True)
                    GT, GTB = scratch()
                    k.op("dve", V.tensor_tensor, R=[prbB, colB], W=[GTB], out=v3(GT[:, :]), in0=v3(prb[:, :]),
                         in1=bc(c_b.unsqueeze(2), [128, 4, 128]), op=ALU.subtract)
                    k.op("pool", POOL.affine_select, R=[GTB], W=[GTB], out=v3(GT[:, :]), in_=v3(GT[:, :]),
                         pattern=[[0, 4], [1, 128]], compare_op=ALU.is_ge, fill=FILLM, base=0,
                         channel_multiplier=-1)
                    k.op("act", ACT.activation, R=[GTB], W=[GTB], out=GT[:, :], in_=GT[:, :], func=AF.Exp)
                    QKT, QKTB = gd["QKT"]
                    k.op("dve", V.tensor_tensor, R=[pg2B, GTB], W=[QKTB], out=QKT[:, :], in0=pg2[:, :], in1=GT[:, :],
                         op=ALU.mult)
                    MT, MTB = scratch()
                    k.op("dve", V.tensor_tensor, R=[pg1B, GTB], W=[MTB], out=MT[:, :], in0=pg1[:, :], in1=GT[:, :],
                         op=ALU.mult)
                    k.op("pool", POOL.affine_select, R=[MTB], W=[MTB], out=v3(MT[:, :]), in_=v3(MT[:, :]),
                         pattern=[[0, 4], [1, 128]], compare_op=ALU.is_gt, fill=FILL0, base=0, channel_multiplier=-1)
                    if stop == "A3b":
                        break
                    pA, pAB = psum()
                    for h in range(4):
                        k.op("pe", PE.transpose, R=[MTB, identB], W=[pAB], out=pA[:, h * 128:(h + 1) * 128],
                             in_=MT[:, h * 128:(h + 1) * 128], identity=ident_f[:, :])
                    P, PB = gd["P0"]
                    k.op("dve", V.tensor_tensor, R=[pAB, colB], W=[PB], out=v3(P[:, :]), in0=v3(pA[:, :]),
                         in1=bc(c_beta.unsqueeze(2), [128, 4, 128]), op=ALU.mult)
                    if stop == "A3b1":
                        break
                    pAT, pATB = psum()
                    for h in range(4):
                        k.op("pe", PE.transpose, R=[PB, identB], W=[pATB], out=pAT[:, h * 128:(h + 1) * 128],
                             in_=P[:, h * 128:(h + 1) * 128], identity=ident_f[:, :])
                    if stop == "A3b1a":
                        break
                    PT_, PTB = gd["PT0"]
                    k.op("act", ACT.copy, R=[pATB], W=[PTB], out=PT_[:, :], in_=pAT[:, :])
                    if stop == "A3b1b":
                        break
                    RT, RTB = gd["RT0"]
                    for h in range(4):
                        hs = slice(h * 128, (h + 1) * 128)
                        k.op("dve", V.tensor_tensor, R=[identB, PTB], W=[RTB], out=RT[:, hs], in0=ident_f[:, :],
                             in1=PT_[:, hs], op=ALU.subtract)
                    if stop == "A3b2":
                        break
                    for lvl in range(6):
                        if stop == "A3b3" and lvl == 1:
                            break
                        p2, p2B = psum()
                        for h in range(4):
                            hs = slice(h * 128, (h + 1) * 128)
                            k.op("pe", PE.matmul, R=[PTB, PB], W=[p2B], out=p2[:, hs], lhsT=PT_[:, hs], rhs=P[:, hs],
                                 start=True, stop=True)
                        if lvl < 5:
                            p2t, p2tB = psum()
                            for h in range(4):
                                hs = slice(h * 128, (h + 1) * 128)
                                k.op("pe", PE.matmul, R=[PTB, PB], W=[p2tB], out=p2t[:, hs], lhsT=P[:, hs],
                                     rhs=PT_[:, hs], start=True, stop=True)
                        Pn, PnB = gd["P%d" % ((lvl + 1) % 2)]
                        k.op("act", ACT.copy, R=[p2B], W=[PnB], out=Pn[:, :], in_=p2[:, :])
                        if lvl < 5:
                            PTn, PTnB = gd["PT%d" % ((lvl + 1) % 2)]
                            k.op("act", ACT.copy, R=[p2tB], W=[PTnB], out=PTn[:, :], in_=p2t[:, :])
                        p3, p3B = psum()
                        for h in range(4):
                            hs = slice(h * 128, (h + 1) * 128)
                            k.op("pe", PE.matmul, R=[PnB, RTB], W=[p3B], out=p3[:, hs], lhsT=Pn[:, hs], rhs=RT[:, hs],
                                 start=True, stop=True)
                        RTn, RTnB = gd["RT%d" % ((lvl + 1) % 2)]
                        k.op("dve", V.tensor_tensor, R=[p3B, RTB], W=[RTnB], out=RTn[:, :], in0=p3[:, :], in1=RT[:, :],
                             op=ALU.add)
                        RT, RTB = RTn, RTnB
                        P, PB = Pn, PnB
                        if lvl < 5:
                            PT_, PTB = PTn, PTnB
                    if stop in ("A3c", "A3b3"):
                        break
                    pw, pwB = psum()
                    pu_, puB_ = psum()
                    for h in range(4):
                        hs = slice(h * 128, (h + 1) * 128)
                        k.op("pe", PE.matmul, R=[KbdB, RTB], W=[pwB], out=pw[:, hs], lhsT=Kbd[:, hs], rhs=RT[:, hs],
                             start=True, stop=True)
                    for h in range(4):
                        hs = slice(h * 128, (h + 1) * 128)
                        k.op("pe", PE.matmul, R=[VbB, RTB], W=[puB_], out=pu_[:, hs], lhsT=RT[:, hs], rhs=Vb[:, hs],
                             start=True, stop=True)
                    WT, WTB = scratch()
                    U, UB = scratch()
                    k.op("act", ACT.copy, R=[pwB], W=[WTB], out=WT[:, :], in_=pw[:, :])
                    k.op("act", ACT.copy, R=[puB_], W=[UB], out=U[:, :], in_=pu_[:, :])
                    tg = NTS * s_i + ch
                    Sc, ScB = Sst[tg % 2]
                    Sn, SnB = Sst[(tg + 1) % 2]
                    pws, pwsB = psum()
                    for h in range(4):
                        hs = slice(h * 128, (h + 1) * 128)
                        k.op("pe", PE.matmul, R=[WTB, ScB], W=[pwsB], out=pws[:, hs], lhsT=WT[:, hs], rhs=Sc[:, h, :],
                             start=True, stop=True)
                    Vn, VnB = scratch()
                    k.op("dve", V.scalar_tensor_tensor, R=[UB, pwsB], W=[VnB], out=Vn[:, :], in0=pws[:, :], scalar=-1.0,
                         in1=U[:, :], op0=ALU.mult, op1=ALU.add)
                    po, poB = psum()
                    for h in range(4):
                        hs = slice(h * 128, (h + 1) * 128)
                        k.op("pe", PE.matmul, R=[ScB, qdB], W=[poB], out=po[:, hs], lhsT=Sc[:, h, :], rhs=qd[:, hs],
                             start=True, stop=False)
                        k.op("pe", PE.matmul, R=[VnB, QKTB], W=[poB], out=po[:, hs], lhsT=Vn[:, hs], rhs=QKT[:, hs],
                             start=False, stop=True)
                    ps_, psB_ = psum()
                    for h in range(4):
                        hs = slice(h * 128, (h + 1) * 128)
                        k.op("pe", PE.matmul, R=[KdecB, VnB], W=[psB_], out=ps_[:, hs], lhsT=Kdec[:, hs], rhs=Vn[:, hs],
                             start=True, stop=True)
                    k.op("dve", V.tensor_tensor, R=[ScB, eblB], W=[SnB], out=Sn[:, :, :], in0=Sc[:, :, :],
                         in1=bc(ebl[:, 0:4].unsqueeze(2), [128, 4, 128]), op=ALU.mult)
                    k.op("dve", V.tensor_tensor, R=[SnB, psB_], W=[SnB], out=Sn[:, :, :], in0=Sn[:, :, :],
                         in1=v3(ps_[:, :]), op=ALU.add)
                    o32, o32B = scratch()
                    osq, osqB = scratch()
                    k.op("act", ACT.copy, R=[poB], W=[o32B], out=o32[:, :], in_=po[:, :])
                    k.op("act", ACT.activation, R=[poB], W=[osqB], out=osq[:, :], in_=po[:, :], func=AF.Square)
                    pm_, pmB_ = psum()
                    k.op("pe", PE.matmul, R=[o128B, osqB], W=[pmB_], out=pm_[:, :], lhsT=ones128[:, :], rhs=osq[:, :],
                         start=True, stop=True)
                    k.op("act", ACT.activation, R=[pmB_, epsB], W=[osqB], out=osq[:, :], in_=pm_[:, :], func=AF.Sqrt,
                         bias=epsc[:, 0:1])
                    k.op("dve", V.reciprocal, R=[osqB], W=[osqB], out=osq[:, :], in_=osq[:, :])
                    k.op("dve", V.scalar_tensor_tensor, R=[o32B, osqB, gncB], W=[o32B], out=o32[:, :], in0=o32[:, :],
                         scalar=gnc[:, 0:1], in1=osq[:, :], op0=ALU.mult, op1=ALU.mult)
                    k.op("dve", V.tensor_tensor, R=[o32B, zsB], W=[yTB], out=yT[:, 0:4, cs], in0=v3(o32[:, :]),
                         in1=zs[:, :, cs], op=ALU.mult)

                if stop in ("A3", "A3a", "A3b", "A3c", "A3b1", "A3b2", "A3b3", "A3b1a", "A3b1b"):
                    break
                for j in range(NTS):
                    t = tiles[j]
                    cs = slice(j * 128, (j + 1) * 128)
                    pq, pqB = psum()
                    for c in range(2):
                        k.op("pe", PE.matmul, R=[cqB, WAB], W=[pqB], out=pq[:, 0:256], lhsT=cqT[:, c, cs],
                             rhs=w_qb[:, c, :], start=(c == 0), stop=(c == 1))
                    for c in range(2):
                        k.op("pe", PE.matmul, R=[cqB, WAB], W=[pqB], out=pq[:, 256:512], lhsT=cqT[:, c, cs],
                             rhs=w_qi[:, c, :], start=(c == 0), stop=(c == 1))
                    pkv, pkvB = psum()
                    k.op("pe", PE.matmul, R=[ckvB, WAB], W=[pkvB], out=pkv[:, 0:128], lhsT=ckvT[:, cs], rhs=w_kvb,
                         start=True, stop=True)
                    sq, sqB = scratch()
                    k.op("act", ACT.activation, R=[pqB], W=[sqB], out=sq[:, 0:256], in_=pq[:, 0:256], func=AF.Square)
                    k.op("act", ACT.activation, R=[pkvB], W=[sqB], out=sq[:, 256:320], in_=pkv[:, 0:64],
                         func=AF.Square)
                    ss, ssB = small()
                    k.op("dve", V.tensor_reduce, R=[sqB], W=[ssB], out=ss[:, 0:5],
                         in_=sq[:, 0:320].rearrange("p (a b) -> p a b", a=5), axis=AX.X, op=ALU.add)
                    k.op("dve", V.tensor_scalar, R=[ssB], W=[ssB], out=ss[:, 5:10], in0=ss[:, 0:5], scalar1=1.0 / 64,
                         scalar2=EPS, op0=ALU.mult, op1=ALU.add)
                    k.op("act", ACT.activation, R=[ssB], W=[ssB], out=ss[:, 5:10], in_=ss[:, 5:10], func=AF.Sqrt)
                    k.op("dve", V.reciprocal, R=[ssB], W=[ssB], out=ss[:, 10:15], in_=ss[:, 5:10])
                    tq, tqB = scratch()
                    k.op("dve", V.tensor_tensor, R=[pqB, ssB], W=[tqB], out=v3(tq[:, 0:256]), in0=v3(pq[:, 0:256]),
                         in1=bc(ss[:, 10:14].unsqueeze(2), [128, 4, 64]), op=ALU.mult)
                    QI, QIB = scratchb()
                    QI3 = v3(QI[:, :])
                    k.op("dve", V.tensor_tensor, R=[tqB, gq8B], W=[QIB], out=QI3[:, :, 0:64], in0=v3(tq[:, 0:256]),
                         in1=bc(gq8[:, :].unsqueeze(1), [128, 4, 64]), op=ALU.mult)
                    k.op("dve", V.tensor_tensor, R=[pqB, wabB, ssB], W=[QIB], out=QI3[:, :, 64:128], in0=v3(pq[:, 256:512]),
                         in1=bc(wab[:, j, 0:4].unsqueeze(2), [128, 4, 64]), op=ALU.mult)
                    k.op("dve", V.scalar_tensor_tensor, R=[pkvB, ssB, gkbB], W=[kktB], out=kkt[:, j, 0:64],
                         in0=pkv[:, 0:64], scalar=ss[:, 14:15], in1=gkb[:, :], op0=ALU.mult, op1=ALU.mult)
                    k.op("dve", V.tensor_copy, R=[pkvB, ssB], W=[vcB[t]], out=Vc[:, t, 0:64], in_=pkv[:, 64:128])
                    ptq, ptqB = psum()
                    for h in range(4):
                        k.op("pe", PE.transpose, R=[QIB, identbB], W=[ptqB], out=bfv(ptq)[:, h * 128:(h + 1) * 128],
                             in_=QI3[:, h, :], identity=ident_b[:, :])
                    QQ, QQB = QQs[t % 2]
                    k.op("act", ACT.copy, R=[ptqB], W=[QQB], out=QQ[:, :], in_=bfv(ptq)[:, 0:512])
                    ptk, ptkB = psum()
                    k.op("pe", PE.transpose, R=[kktB, identbB], W=[ptkB], out=bfv(ptk)[:, 0:128], in_=kkt[:, j, :],
                         identity=ident_b[:, :])
                    k.op("act", ACT.copy, R=[ptkB], W=[kkB[t]], out=KKT[:, t * 128:(t + 1) * 128],
                         in_=bfv(ptk)[:, 0:128])
                    Sw = (t + 1) * 128
                    for kc0 in range(0, Sw, 512):
                        wd = min(512, Sw - kc0)
                        kts = list(range(kc0 // 128, (kc0 + wd) // 128))
                        rr = []
                        for h in range(4):
                            ph, phB = psum()
                            k.op("pe", PE.matmul, R=[QQB] + [kkB[u] for u in kts], W=[phB], out=ph[:, :wd],
                                 lhsT=QQ[64:128, h * 128:(h + 1) * 128], rhs=KKT[64:128, kc0:kc0 + wd],
                                 start=True, stop=True)
                            rh, rhB = scratch()
                            k.op("act", ACT.activation, R=[phB], W=[rhB], out=rh[:, :wd], in_=ph[:, :wd], func=AF.Relu)
                            rr.append((rh, rhB))
                        k.op("dve", V.tensor_scalar, R=[rr[0][1], wabB], W=[workB], out=work[:, kc0:kc0 + wd],
                             in0=rr[0][0][:, :wd], scalar1=wab[:, j, 4:5], scalar2=None, op0=ALU.mult)
                        for h in range(1, 4):
                            k.op("dve", V.scalar_tensor_tensor, R=[rr[h][1], wabB, workB], W=[workB],
                                 out=work[:, kc0:kc0 + wd], in0=rr[h][0][:, :wd], scalar=wab[:, j, 4 + h:5 + h],
                                 in1=work[:, kc0:kc0 + wd], op0=ALU.mult, op1=ALU.add)
                    k.op("pool", POOL.affine_select, R=[workB], W=[workB], out=work[:, t * 128:(t + 1) * 128],
                         in_=work[:, t * 128:(t + 1) * 128], pattern=[[-1, 128]], compare_op=ALU.is_ge, fill=FILLNC,
                         base=0, channel_multiplier=1)
                    if t >= 2:
                        for it in range(32):
                            m8, m8B = m8s[it % 2]
                            k.op("dve", V.max, R=[workB], W=[m8B], out=m8[:, :], in_=work[:, :Sw])
                            k.op("dve", V.match_replace, R=[m8B, workB], W=[workB], out=work[:, :Sw],
                                 in_to_replace=m8[:, :], in_values=work[:, :Sw], imm_value=NEG_SEL)
                    pso, psoB = psb[7]
                    for kc0 in range(0, Sw, 512):
                        wd = min(512, Sw - kc0)
                        mk, mkB = mks[mkc[0] % 2]
                        mkc[0] += 1
                        if t >= 2:
                            k.op("dve", V.tensor_single_scalar, R=[workB], W=[mkB], out=mk[:, :wd],
                                 in_=work[:, kc0:kc0 + wd], scalar=-2.5e38, op=ALU.is_le)
                        else:
                            k.op("dve", V.tensor_single_scalar, R=[workB], W=[mkB], out=mk[:, :wd],
                                 in_=work[:, kc0:kc0 + wd], scalar=-1.0e38, op=ALU.is_gt)
                        for u in range(wd // 128):
                            kt = kc0 // 128 + u
                            pmk, pmkB = psum()
                            k.op("pe", PE.transpose, R=[mkB, identbB], W=[pmkB], out=bfv(pmk)[:, 0:128],
                                 in_=mk[:, u * 128:(u + 1) * 128], identity=ident_b[:, :])
                            pl, plB = psum()
                            k.op("pe", PE.matmul, R=[kkB[kt], QQB], W=[plB], out=pl[:, :],
                                 lhsT=KKT[0:64, kt * 128:(kt + 1) * 128], rhs=QQ[0:64, :], start=True, stop=True)
                            pT_, pTB = scratchb()
                            k.op("act", ACT.activation, R=[plB], W=[pTB], out=pT_[:, :], in_=pl[:, :], func=AF.Exp)
                            pmm, pmmB = scratchb()
                            k.op("dve", V.tensor_tensor, R=[pTB, pmkB], W=[pmmB], out=v3(pmm[:, :]), in0=v3(pT_[:, :]),
                                 in1=bc(bfv(pmk)[:, 0:128].unsqueeze(1), [128, 4, 128]), op=ALU.mult)
                            for h in range(4):
                                k.op("pe", PE.matmul, R=[pmmB, vcB[kt]], W=[psoB], out=pso[:, h * 65:(h + 1) * 65],
                                     lhsT=pmm[:, h * 128:(h + 1) * 128], rhs=Vc[:, kt, :],
                                     start=(kt == 0 and h == 0), stop=(kt == t and h == 3))
                    pso3 = pso[:, 0:260].rearrange("p (a b) -> p a b", a=4)
                    rd, rdB = small()
                    k.op("dve", V.reciprocal, R=[psoB], W=[rdB], out=rd[:, 0:4], in_=pso3[:, :, 64])
                    yb, ybB = scratchb()
                    k.op("dve", V.tensor_tensor, R=[psoB, rdB], W=[ybB], out=v3(yb[:, 0:256]), in0=pso3[:, :, 0:64],
                         in1=bc(rd[:, 0:4].unsqueeze(2), [128, 4, 64]), op=ALU.mult)
                    pty, ptyB = psum()
                    for c in range(2):
                        k.op("pe", PE.transpose, R=[ybB, identbB], W=[ptyB], out=bfv(pty)[:, c * 128:(c + 1) * 128],
                             in_=yb[:, c * 128:(c + 1) * 128], identity=ident_b[:, :])
                    k.op("act", ACT.copy, R=[ptyB], W=[yTB], out=yT[:, 4:6, cs],
                         in_=bfv(pty)[:, 0:256].rearrange("p (a b) -> p a b", a=2))

                if stop == "A4":
                    break
                yc, ycB = scratch()
                ysq, ysqB = scratch()
                for i in range(2):
                    pc, pcB = psum()
                    for jj in range(31):
                        k.op("pe", PE.matmul, R=[dgcB, glB], W=[pcB], out=pc[:, :ST], lhsT=dgc[:, i, jj, :],
                             rhs=gl[:, i, jj:jj + ST], start=(jj == 0), stop=(jj == 30))
                    k.op("act", ACT.activation, R=[pcB, cvecB], W=[ycB], out=yc[:, i * ST:(i + 1) * ST], in_=pc[:, :ST],
                         func=AF.Identity, bias=cvec[:, 0, i:i + 1])
                    k.op("act", ACT.activation, R=[pcB, cvecB], W=[ysqB], out=ysq[:, i * ST:(i + 1) * ST],
                         in_=pc[:, :ST], func=AF.Square, bias=cvec[:, 0, i:i + 1])
                k.op("dve", V.tensor_copy, R=[glB], W=[glB], out=gl[:, :, 0:30], in_=gl[:, :, ST:ST + 30])
                pmu, pmuB = psum()
                pms, pmsB = psum()
                for i in range(2):
                    k.op("pe", PE.matmul, R=[o256B, ycB], W=[pmuB], out=pmu[:, :ST], lhsT=ones256[:, :],
                         rhs=yc[:, i * ST:(i + 1) * ST], start=(i == 0), stop=(i == 1))
                for i in range(2):
                    k.op("pe", PE.matmul, R=[o256B, ysqB], W=[pmsB], out=pms[:, :ST], lhsT=ones256[:, :],
                         rhs=ysq[:, i * ST:(i + 1) * ST], start=(i == 0), stop=(i == 1))
                mu, muB = scratch()
                k.op("act", ACT.copy, R=[pmuB], W=[muB], out=mu[:, 0:ST], in_=pmu[:, :ST])
                k.op("act", ACT.activation, R=[pmuB], W=[muB], out=mu[:, ST:2 * ST], in_=pmu[:, :ST], func=AF.Square)
                k.op("dve", V.tensor_tensor, R=[pmsB, muB], W=[muB], out=mu[:, ST:2 * ST], in0=pms[:, :ST],
                     in1=mu[:, ST:2 * ST], op=ALU.subtract)
                k.op("act", ACT.activation, R=[muB, epsB], W=[muB], out=mu[:, ST:2 * ST], in_=mu[:, ST:2 * ST],
                     func=AF.Sqrt, bias=epsc[:, 0:1])
                k.op("dve", V.reciprocal, R=[muB], W=[muB], out=mu[:, ST:2 * ST], in_=mu[:, ST:2 * ST])
                for i in range(2):
                    k.op("dve", V.tensor_tensor, R=[ycB, muB], W=[ycB], out=yc[:, i * ST:(i + 1) * ST],
                         in0=yc[:, i * ST:(i + 1) * ST], in1=mu[:, 0:ST], op=ALU.subtract)
                    k.op("dve", V.tensor_tensor, R=[ycB, muB], W=[ycB], out=yc[:, i * ST:(i + 1) * ST],
                         in0=yc[:, i * ST:(i + 1) * ST], in1=mu[:, ST:2 * ST], op=ALU.mult)
                    k.op("act", ACT.activation, R=[ycB, cvecB], W=[yTB], out=yT[:, 6 + i, :],
                         in_=yc[:, i * ST:(i + 1) * ST], func=AF.Silu, scale=cvec[:, 1, i:i + 1],
                         bias=cvec[:, 2, i:i + 1])

                for j in range(NTS):
                    xt, xtB = xts[j]
                    t = tiles[j]
                    for n in range(2):
                        pt, pB = psum()
                        for c in range(8):
                            k.op("pe", PE.matmul, R=[yTB, WAB], W=[pB], out=pt[:, :],
                                 lhsT=yT[:, c, j * 128:(j + 1) * 128], rhs=w_out[:, c, n * 512:(n + 1) * 512],
                                 start=(c == 0), stop=(c == 7))
                        k.op("dve", V.tensor_tensor, R=[pB, xtB], W=[xtB], out=xt[:, n * 512:(n + 1) * 512],
                             in0=pt[:, :], in1=xt[:, n * 512:(n + 1) * 512], op=ALU.add)
                    k.dma("sp", out_d[t * 128:(t + 1) * 128, :], xt[:, :], xtB, R=[xtB], W=[xdB[t]])
        k.barrier()
        if stop in ("A", "W", "A1", "A2", "A3", "A3a", "A3b", "A3c", "A4", "A3b1", "A3b2", "A3b3", "A3b1a", "A3b1b"):
            break

        with contextlib.ExitStack() as pa:
            def sba(name, shape, dt=F32):
                t = pa.enter_context(nc.sbuf_tensor("%s_x%d" % (name, l), list(shape), dt))
                return t, Buf(name)
            WA, WAB = sba("WA", [128, 16384], BF16)
            for i in range(2):
                stage[i] = sba("stgX%d" % i, [128, 2048])
            wq = WA[:, 0:8 * 512].rearrange("p (c n) -> p c n", c=8)
            wkv = WA[:, 4096:4096 + 8 * 1024].rearrange("p (c n) -> p c n", c=8)
            wo = WA[:, 12288:12288 + 4 * 1024].rearrange("p (c n) -> p c n", c=4)
            load_gcol(norm_cross_d[l])
            for c in range(8):
                cast_rows(wq[:, c, :], WAB, wq_d[l, c * 128:(c + 1) * 128, :], 512, gcol[:, c:c + 1])
            load_gcol(norm_mem_d)
            for c in range(8):
                cast_rows(wkv[:, c, :], WAB, wkv_d[l, c * 128:(c + 1) * 128, :], 1024, gcol[:, c:c + 1])
            for c in range(4):
                cast_rows(wo[:, c, :], WAB, wo_d[l, c * 128:(c + 1) * 128, :], D)
            gxq, gxqB = sba("gxq", [128, 128])
            gxk, gxkB = sba("gxk", [128, 128])
            k.dma("sp", gxq[:, :], xq_norm_d[l].partition_broadcast(128), gxqB, W=[gxqB])
            k.dma("sp", gxk[:, :], xk_norm_d[l].partition_broadcast(128), gxkB, W=[gxkB])
            k.op("dve", V.tensor_scalar, R=[gxqB], W=[gxqB], out=gxq[:, :], in0=gxq[:, :], scalar1=128 ** -0.5,
                 scalar2=None, op0=ALU.mult)
            kmT, kmTB = sba("kmT", [128, 4, MEM], BF16)
            vm1, vm1B = sba("vm1", [128, 2, 4, 129], BF16)
            k.op("pool", POOL.memset, W=[vm1B], ap=vm1[:, :, :, :], constant=1.0)
            scr = [sba("xscr%d" % i, [128, 512]) for i in range(4)]
            scb = [sba("xscb%d" % i, [128, 1024], BF16) for i in range(4)]
            sm = [sba("xsm%d" % i, [128, 16]) for i in range(4)]
            cx = [0, 0, 0]

            def scratch():
                cx[0] += 1
                return scr[cx[0] % 4]

            def scratchb():
                cx[1] += 1
                return scb[cx[1] % 4]

            def small():
                cx[2] += 1
                return sm[cx[2] % 4]

            def v3(ap, a=4):
                return ap.rearrange("p (a b) -> p a b", a=a)

            def head_rms(pt, pB, gtile, gB, dst3, dstB):
                sq, sqB = scratch()
                k.op("act", ACT.activation, R=[pB], W=[sqB], out=sq[:, :], in_=pt[:, :], func=AF.Square)
                ss, ssB = small()
                k.op("dve", V.tensor_reduce, R=[sqB], W=[ssB], out=ss[:, 0:4], in_=v3(sq[:, :]), axis=AX.X, op=ALU.add)
                k.op("dve", V.tensor_scalar, R=[ssB], W=[ssB], out=ss[:, 4:8], in0=ss[:, 0:4], scalar1=1.0 / 128,
                     scalar2=EPS, op0=ALU.mult, op1=ALU.add)
                k.op("act", ACT.activation, R=[ssB], W=[ssB], out=ss[:, 4:8], in_=ss[:, 4:8], func=AF.Sqrt)
                k.op("dve", V.reciprocal, R=[ssB], W=[ssB], out=ss[:, 8:12], in_=ss[:, 4:8])
                k.op("dve", V.tensor_tensor, R=[pB, ssB], W=[sqB], out=v3(sq[:, :]), in0=v3(pt[:, :]),
                     in1=bc(ss[:, 8:12].unsqueeze(2), [128, 4, 128]), op=ALU.mult)
                k.op("dve", V.tensor_tensor, R=[sqB, gB], W=[dstB], out=dst3, in0=v3(sq[:, :]),
                     in1=bc(gtile[:, :].unsqueeze(1), [128, 4, 128]), op=ALU.mult)

            for mt in range(2):
                xt, xtB = xts[mt]
                k.dma("sp", xt[:, :], mem_d[mt * 128:(mt + 1) * 128, :], xtB, W=[xtB])
                norm_T(xt, xtB, 0)
                pk_, pkB_ = psum()
                pv_, pvB_ = psum()
                for c in range(8):
                    k.op("pe", PE.matmul, R=[hTB, WAB], W=[pkB_], out=pk_[:, :], lhsT=hT[:, c, 0:128],
                         rhs=wkv[:, c, 0:512], start=(c == 0), stop=(c == 7))
                for c in range(8):
                    k.op("pe", PE.matmul, R=[hTB, WAB], W=[pvB_], out=pv_[:, :], lhsT=hT[:, c, 0:128],
                         rhs=wkv[:, c, 512:1024], start=(c == 0), stop=(c == 7))
                kn, knB = scratchb()
                head_rms(pk_, pkB_, gxk, gxkB, v3(kn[:, 0:512]), knB)
                ptk, ptkB = psum()
                for h in range(4):
                    k.op("pe", PE.transpose, R=[knB, identbB], W=[ptkB], out=bfv(ptk)[:, h * 128:(h + 1) * 128],
                         in_=kn[:, h * 128:(h + 1) * 128], identity=ident_b[:, :])
                k.op("act", ACT.copy, R=[ptkB], W=[kmTB], out=kmT[:, :, mt * 128:(mt + 1) * 128],
                     in_=v3(bfv(ptk)[:, 0:512]))
                k.op("act", ACT.copy, R=[pvB_], W=[vm1B], out=vm1[:, mt, :, 0:128], in_=v3(pv_[:, :]))

            for t in range(NT):
                xt, xtB = xts[t % 2]
                k.dma("sp", xt[:, :], out_d[t * 128:(t + 1) * 128, :], xtB, R=[xdB[t]], W=[xtB])
                norm_T(xt, xtB, 0)
                pq, pqB = psum()
                for c in range(8):
                    k.op("pe", PE.matmul, R=[hTB, WAB], W=[pqB], out=pq[:, :], lhsT=hT[:, c, 0:128], rhs=wq[:, c, :],
                         start=(c == 0), stop=(c == 7))
                qn, qnB = scratchb()
                head_rms(pq, pqB, gxq, gxqB, v3(qn[:, 0:512]), qnB)
                ptq, ptqB = psum()
                for h in range(4):
                    k.op("pe", PE.transpose, R=[qnB, identbB], W=[ptqB], out=bfv(ptq)[:, h * 128:(h + 1) * 128],
                         in_=qn[:, h * 128:(h + 1) * 128], identity=ident_b[:, :])
                qT, qTB = scratchb()
                k.op("act", ACT.copy, R=[ptqB], W=[qTB], out=qT[:, 0:512], in_=bfv(ptq)[:, 0:512])
                pT_, pTB = scratchb()
                for mt in range(2):
                    pl, plB = psum()
                    for h in range(4):
                        k.op("pe", PE.matmul, R=[kmTB, qTB], W=[plB], out=pl[:, h * 128:(h + 1) * 128],
                             lhsT=kmT[:, h, mt * 128:(mt + 1) * 128], rhs=qT[:, h * 128:(h + 1) * 128], start=True,
                             stop=True)
                    k.op("act", ACT.activation, R=[plB], W=[pTB], out=pT_[:, mt * 512:(mt + 1) * 512], in_=pl[:, :],
                         func=AF.Exp)
                on, onB = scratchb()
                for hh in range(2):
                    po, poB = psum()
                    for h2 in range(2):
                        h = hh * 2 + h2
                        for mt in range(2):
                            k.op("pe", PE.matmul, R=[pTB, vm1B], W=[poB], out=po[:, h2 * 129:(h2 + 1) * 129],
                                 lhsT=pT_[:, mt * 512 + h * 128:mt * 512 + (h + 1) * 128], rhs=vm1[:, mt, h, :],
                                 start=(mt == 0), stop=(mt == 1))
                    po3 = po[:, 0:258].rearrange("p (a b) -> p a b", a=2)
                    rd, rdB = small()
                    k.op("dve", V.reciprocal, R=[poB], W=[rdB], out=rd[:, 0:2], in_=po3[:, :, 128])
                    k.op("dve", V.tensor_tensor, R=[poB, rdB], W=[onB],
                         out=on[:, hh * 256:(hh + 1) * 256].rearrange("p (a b) -> p a b", a=2), in0=po3[:, :, 0:128],
                         in1=bc(rd[:, 0:2].unsqueeze(2), [128, 2, 128]), op=ALU.mult)
                pto, ptoB = psum()
                for h in range(4):
                    k.op("pe", PE.transpose, R=[onB, identbB], W=[ptoB], out=bfv(pto)[:, h * 128:(h + 1) * 128],
                         in_=on[:, h * 128:(h + 1) * 128], identity=ident_b[:, :])
                oT, oTB = scratchb()
                k.op("act", ACT.copy, R=[ptoB], W=[oTB], out=oT[:, 0:512], in_=bfv(pto)[:, 0:512])
                for n in range(2):
                    pt, pB = psum()
                    for h in range(4):
                        k.op("pe", PE.matmul, R=[oTB, WAB], W=[pB], out=pt[:, :], lhsT=oT[:, h * 128:(h + 1) * 128],
                             rhs=wo[:, h, n * 512:(n + 1) * 512], start=(h == 0), stop=(h == 3))
                    k.op("dve", V.tensor_tensor, R=[pB, xtB], W=[xtB], out=xt[:, n * 512:(n + 1) * 512], in0=pt[:, :],
                         in1=xt[:, n * 512:(n + 1) * 512], op=ALU.add)
                k.dma("sp", out_d[t * 128:(t + 1) * 128, :], xt[:, :], xtB, R=[xtB], W=[xdB[t]])
        k.barrier()
        if stop == "A2":
            break

        with contextlib.ExitStack() as pa:
            def sba(name, shape, dt=F32):
                t = pa.enter_context(nc.sbuf_tensor("%s_m%d" % (name, l), list(shape), dt))
                return t, Buf(name)
            ST = ST_B
            NST = S // ST
            WA, WAB = sba("WA", [128, 65536], BF16)
            for i in range(2):
                stage[i] = sba("stgM%d" % i, [128, 2048])
            w1 = WA[:, 0:8 * 4096].rearrange("p (c n) -> p c n", c=8)
            w2 = WA[:, 32768:32768 + 32 * 1024].rearrange("p (c n) -> p c n", c=32)
            load_gcol(norm_mlp_d[l])
            for c in range(8):
                cast_rows(w1[:, c, :], WAB, w1_d[l, c * 128:(c + 1) * 128, :], 4096, gcol[:, c:c + 1])
            for c in range(32):
                cast_rows(w2[:, c, :], WAB, w2_d[l, c * 128:(c + 1) * 128, :], D)
            aT, aTB = sba("aT", [128, 32, ST], BF16)
            rrs = [sba("rr%d" % i, [128, ST]) for i in range(3)]
            for s_i in range(NST):
                t0 = s_i * ST
                for j in range(2):
                    xt, xtB = xts[j]
                    k.dma("sp", xt[:, :], out_d[t0 + j * 128:t0 + (j + 1) * 128, :], xtB, R=[xdB[2 * s_i + j]],
                          W=[xtB])
                    norm_T(xt, xtB, j * 128)
                for f in range(32):
                    pt, pB = psum()
                    for c in range(8):
                        k.op("pe", PE.matmul, R=[WAB, hTB], W=[pB], out=pt[:, :ST], lhsT=w1[:, c, f * 128:(f + 1) * 128],
                             rhs=hT[:, c, :], start=(c == 0), stop=(c == 7))
                    rr_, rrB = rrs[f % 3]
                    k.op("act", ACT.activation, R=[pB], W=[rrB], out=rr_[:, :], in_=pt[:, :ST], func=AF.Relu)
                    e = "pool" if f % 2 == 0 else "dve"
                    k.op(e, k.eng[e].tensor_tensor, R=[rrB], W=[aTB], out=aT[:, f, :], in0=rr_[:, :], in1=rr_[:, :],
                         op=ALU.mult)
                for j in range(2):
                    xt, xtB = xts[j]
                    t = 2 * s_i + j
                    for n in range(2):
                        pt, pB = psum()
                        for f in range(32):
                            k.op("pe", PE.matmul, R=[aTB, WAB], W=[pB], out=pt[:, :],
                                 lhsT=aT[:, f, j * 128:(j + 1) * 128], rhs=w2[:, f, n * 512:(n + 1) * 512],
                                 start=(f == 0), stop=(f == 31))
                        k.op("dve", V.tensor_tensor, R=[pB, xtB], W=[xtB], out=xt[:, n * 512:(n + 1) * 512],
                             in0=pt[:, :], in1=xt[:, n * 512:(n + 1) * 512], op=ALU.add)
                    k.dma("sp", out_d[t * 128:(t + 1) * 128, :], xt[:, :], xtB, R=[xtB], W=[xdB[t]])
        k.barrier()
    k.finish()
    es.close()
    return nc, k


_INPUT_NAMES = ["x", "mem", "norm_mix", "w_in", "gdn_conv", "gdn_a_log", "gdn_dt_bias", "gdn_norm", "dsa_w_qb",
                "dsa_w_qi", "dsa_w_kvb", "dsa_q_norm", "dsa_k_norm", "conv_dw", "conv_dw_b", "conv_ln_g",
                "conv_ln_b", "w_out", "norm_mem", "norm_cross", "xa_wq", "xa_wkv", "xa_q_norm", "xa_k_norm",
                "xa_wo", "norm_mlp", "mlp_w1", "mlp_w2"]


def run(inputs, S=4096, NL=2, stop=None, ncores=8, trace=False):
    nc, kk = build(S=S, NL=NL, stop=stop)
    shared = {n: np.ascontiguousarray(np.asarray(inputs[n], dtype=np.float32)) for n in _INPUT_NAMES
              if n not in ("x", "mem")}
    x = np.asarray(inputs["x"], dtype=np.float32)
    mem = np.asarray(inputs["mem"], dtype=np.float32)
    in_maps = []
    for b in range(ncores):
        m = dict(shared)
        m["x"] = np.ascontiguousarray(x[b, :S])
        m["mem"] = np.ascontiguousarray(mem[b])
        in_maps.append(m)
    res = run_bass_kernel_spmd(nc, in_maps, core_ids=list(range(ncores)), trace=trace)
    out = np.stack([np.asarray(r["out"]) for r in res.results], axis=0)
    return out, res


def kernel(**inputs):
    out, _ = run(inputs)
    return out.astype(np.float32)
```

```python
import contextlib
import numpy as np
import concourse.bass as bass
import concourse.mybir as mybir
from concourse.bass_utils import run_bass_kernel_spmd

F32 = mybir.dt.float32
BF16 = mybir.dt.bfloat16
AF = mybir.ActivationFunctionType
ALU = mybir.AluOpType
AX = mybir.AxisListType

D = 1024
NIN = 3020
MEM = 256
EPS = 1e-6
C_A, C_B, C_CQ, C_CKV, C_KI, C_WI, C_UG = 2048, 2052, 2056, 2312, 2440, 2504, 2508
NEG_NC = -2.0e38
NEG_SEL = -3.0e38
ST_A = 128
ST_B = 256


class Buf:
    __slots__ = ("w", "r", "dsem", "dcnt", "name")

    def __init__(self, name=""):
        self.w = {}
        self.r = {}
        self.dsem = None
        self.dcnt = 0
        self.name = name


class K:
    def __init__(self, nc, es):
        self.nc = nc
        self.es = es
        self.eng = {"pe": nc.tensor, "act": nc.scalar, "dve": nc.vector, "pool": nc.gpsimd, "sp": nc.sync}
        self.sem = {e: es.enter_context(nc.semaphore("s_" + e)) for e in ("pe", "act", "dve", "pool")}
        self.cnt = {e: 0 for e in self.sem}
        self.waited = {e: {} for e in self.eng}
        self.nsem = 0
        self.dbufs = []
        self.ninst = 0

    def _wait(self, e, deps):
        need = {}
        for key, (sem, val) in deps:
            if key == "pe" and e == "pe":
                continue
            if need.get(key, (None, 0))[1] < val:
                need[key] = (sem, val)
        wd = self.waited[e]
        for key, (sem, val) in need.items():
            if wd.get(key, 0) >= val:
                continue
            self.eng[e].wait_ge(sem, val)
            wd[key] = val
            self.ninst += 1

    @staticmethod
    def _deps(reads, writes):
        deps = []
        for b in reads:
            deps.extend(b.w.items())
        for b in writes:
            deps.extend(b.w.items())
            deps.extend(b.r.items())
        return deps

    @staticmethod
    def _mark(tokkey, tok, reads, writes):
        for b in reads:
            b.r[tokkey] = tok
        for b in writes:
            if b.r:
                b.w = {}
                b.r = {}
            b.w[tokkey] = tok

    def op(self, e, fn, R=(), W=(), **kw):
        self._wait(e, self._deps(R, W))
        ins = fn(**kw)
        self.cnt[e] += 1
        ins.then_inc(self.sem[e], 1)
        self.ninst += 1
        self._mark(e, (self.sem[e], self.cnt[e]), R, W)

    def dma(self, q, out, in_, sb, R=(), W=(), group=False, **kw):
        if sb.dsem is None:
            sb.dsem = self.es.enter_context(self.nc.semaphore("d%d" % self.nsem))
            self.nsem += 1
            self.dbufs.append(sb)
        key = "d%d" % id(sb)
        deps = self._deps(R, W)
        if group:
            deps = [d for d in deps if d[0] != key]
        self._wait(q, deps)
        ins = self.eng[q].dma_start(out=out, in_=in_, **kw)
        sb.dcnt += 16
        ins.then_inc(sb.dsem, 16)
        self.ninst += 1
        self._mark(key, (sb.dsem, sb.dcnt), R, W)

    def barrier(self):
        deps = [("d%d" % id(b), (b.dsem, b.dcnt)) for b in self.dbufs]
        for e in self.sem:
            deps.append((e, (self.sem[e], self.cnt[e])))
        for e in self.eng:
            self._wait(e, [d for d in deps if d[0] != e])

    def finish(self):
        deps = [("d%d" % id(b), (b.dsem, b.dcnt)) for b in self.dbufs]
        for e in self.sem:
            deps.append((e, (self.sem[e], self.cnt[e])))
        self._wait("sp", deps)


def bc(ap, shape):
    return ap.to_broadcast(list(shape))


def build(S=4096, NL=2, stop=None):
    NT = S // 128
    nc = bass.Bass("TRN2", target_bir_lowering=False)
    es = contextlib.ExitStack()

    def din(name, shape):
        return nc.dram_tensor(name, list(shape), F32, kind="ExternalInput").ap()

    x_d = din("x", [S, D])
    mem_d = din("mem", [MEM, D])
    norm_mix_d = din("norm_mix", [2, D])
    w_in_d = din("w_in", [2, D, NIN])
    gdn_conv_d = din("gdn_conv", [2, 4, 1536])
    a_log_d = din("gdn_a_log", [2, 4])
    dt_bias_d = din("gdn_dt_bias", [2, 4])
    gdn_norm_d = din("gdn_norm", [2, 128])
    w_qb_d = din("dsa_w_qb", [2, 256, 256])
    w_qi_d = din("dsa_w_qi", [2, 256, 256])
    w_kvb_d = din("dsa_w_kvb", [2, 128, 128])
    dq_norm_d = din("dsa_q_norm", [2, 64])
    dk_norm_d = din("dsa_k_norm", [2, 64])
    conv_dw_d = din("conv_dw", [2, 31, 256])
    conv_b_d = din("conv_dw_b", [2, 256])
    ln_g_d = din("conv_ln_g", [2, 256])
    ln_b_d = din("conv_ln_b", [2, 256])
    w_out_d = din("w_out", [2, D, D])
    norm_mem_d = din("norm_mem", [D])
    norm_cross_d = din("norm_cross", [2, D])
    wq_d = din("xa_wq", [2, D, 512])
    wkv_d = din("xa_wkv", [2, D, 1024])
    xq_norm_d = din("xa_q_norm", [2, 128])
    xk_norm_d = din("xa_k_norm", [2, 128])
    wo_d = din("xa_wo", [2, 512, D])
    norm_mlp_d = din("norm_mlp", [2, D])
    w1_d = din("mlp_w1", [2, D, 4096])
    w2_d = din("mlp_w2", [2, 4096, D])
    out_d = nc.dram_tensor("out", [S, D], F32, kind="ExternalOutput").ap()

    k = K(nc, es)
    V, ACT, PE, POOL = nc.vector, nc.scalar, nc.tensor, nc.gpsimd

    def sb(name, shape, dt=F32):
        t = es.enter_context(nc.sbuf_tensor(name, list(shape), dt))
        return t, Buf(name)

    psb = []
    for i in range(8):
        t = es.enter_context(nc.psum_tensor("ps%d" % i, [128, 512], F32))
        psb.append((t, Buf("ps%d" % i)))
    pctr = [0]

    def psum():
        t, b = psb[pctr[0] % 7]
        pctr[0] += 1
        return t, b

    def bfv(t):
        return t[:, :].bitcast(BF16)

    ones_f, onesB = sb("ones_f", [128, 512])
    ident_f, identB = sb("ident_f", [128, 128])
    ident_b, identbB = sb("ident_b", [128, 128], BF16)
    ones128, o128B = sb("ones128", [128, 128])
    ones256, o256B = sb("ones256", [128, 128])
    onesS, onesSB = sb("onesS", [128, 128])
    sel, selB = sb("sel", [4, 4, 128])
    epsc, epsB = sb("epsc", [128, 1])
    onec, oneB = sb("onec", [128, 1])
    FILL0 = POOL.to_reg(0.0)
    FILLM = POOL.to_reg(-1.0e30)
    FILLNC = POOL.to_reg(NEG_NC)
    k.op("pool", POOL.memset, W=[onesB], ap=ones_f[:, :], constant=1.0)
    k.op("pool", POOL.memset, W=[o128B], ap=ones128[:, :], constant=1.0 / 128)
    k.op("pool", POOL.memset, W=[o256B], ap=ones256[:, :], constant=1.0 / 256)
    k.op("pool", POOL.memset, W=[onesSB], ap=onesS[:, :], constant=1.0)
    k.op("pool", POOL.memset, W=[epsB], ap=epsc[:, :], constant=EPS)
    k.op("pool", POOL.memset, W=[oneB], ap=onec[:, :], constant=1.0)
    k.op("pool", POOL.affine_select, R=[onesB], W=[identB], out=ident_f[:, :], in_=ones_f[:, 0:128],
         pattern=[[-1, 128]], compare_op=ALU.is_equal, fill=FILL0, base=0, channel_multiplier=1)
    k.op("pool", POOL.affine_select, R=[onesB], W=[identbB], out=ident_b[:, :], in_=ones_f[:, 0:128],
         pattern=[[-1, 128]], compare_op=ALU.is_equal, fill=FILL0, base=0, channel_multiplier=1)
    k.op("pool", POOL.affine_select, R=[onesB], W=[selB], out=sel[:, :, :],
         in_=ones_f[0:4, :].rearrange("p (a b) -> p a b", a=4),
         pattern=[[-1, 4], [0, 128]], compare_op=ALU.is_equal, fill=FILL0, base=0, channel_multiplier=1)

    NXT = 2
    xts = [sb("xt%d" % i, [128, D]) for i in range(NXT)]
    hb, hbB = sb("hb", [128, D], BF16)
    hT, hTB = sb("hT", [128, 8, ST_B], BF16)
    st5 = [sb("st5_%d" % i, [128, 8]) for i in range(4)]
    stage = [None, None]
    sctr = [0]
    gcol, gcolB = sb("gcol", [128, 8])

    def load_gcol(vec_ap):
        k.dma("sp", gcol[:, :], vec_ap.rearrange("(c p) -> p c", p=128), gcolB, W=[gcolB],
              allow_slow_non_contiguous=True)

    cast_rr = [0]

    def cast_rows(dst_ap, dstB, src_rows_ap, ncols, scol=None):
        for c0 in range(0, ncols, 2048):
            cw = min(2048, ncols - c0)
            stg, stgB = stage[sctr[0] % 2]
            sctr[0] += 1
            k.dma("sp", stg[:, :cw], src_rows_ap[:, c0:c0 + cw], stgB, W=[stgB])
            e = ("pool", "act")[cast_rr[0] % 2]
            cast_rr[0] += 1
            R = [stgB] + ([gcolB] if scol is not None else [])
            if e == "pool":
                if scol is None:
                    k.op("pool", POOL.tensor_copy, R=R, W=[dstB], out=dst_ap[:, c0:c0 + cw], in_=stg[:, :cw])
                else:
                    k.op("pool", POOL.tensor_scalar, R=R, W=[dstB], out=dst_ap[:, c0:c0 + cw], in0=stg[:, :cw],
                         scalar1=scol, scalar2=1.0, op0=ALU.mult, op1=ALU.mult)
            else:
                if scol is None:
                    k.op("act", ACT.copy, R=R, W=[dstB], out=dst_ap[:, c0:c0 + cw], in_=stg[:, :cw])
                else:
                    k.op("act", ACT.activation, R=R, W=[dstB], out=dst_ap[:, c0:c0 + cw], in_=stg[:, :cw],
                         func=AF.Copy, scale=scol)

    def norm_T(xt, xtB, ncol0):
        s5, s5B = st5[ncol0 // 128 % 4]
        k.op("act", ACT.activation, R=[xtB], W=[hbB, s5B], out=hb[:, :], in_=xt[:, :], func=AF.Square,
             accum_out=s5[:, 0:1])
        k.op("dve", V.tensor_scalar, R=[s5B], W=[s5B], out=s5[:, 1:2], in0=s5[:, 0:1], scalar1=1.0 / D,
             scalar2=EPS, op0=ALU.mult, op1=ALU.add)
        k.op("act", ACT.activation, R=[s5B], W=[s5B], out=s5[:, 2:3], in_=s5[:, 1:2], func=AF.Sqrt)
        k.op("dve", V.reciprocal, R=[s5B], W=[s5B], out=s5[:, 3:4], in_=s5[:, 2:3])
        k.op("act", ACT.activation, R=[xtB, s5B], W=[hbB], out=hb[:, :], in_=xt[:, :], func=AF.Copy,
             scale=s5[:, 3:4])
        pt, pB = psum()
        for c in range(8):
            k.op("pe", PE.transpose, R=[hbB, identbB], W=[pB], out=bfv(pt)[:, c * 128:(c + 1) * 128],
                 in_=hb[:, c * 128:(c + 1) * 128], identity=ident_b[:, :])
        k.op("dve", V.tensor_copy, R=[pB], W=[hTB], out=hT[:, :, ncol0:ncol0 + 128],
             in_=bfv(pt).rearrange("p (c t) -> p c t", c=8))

    xdB = [Buf("xd%d" % t) for t in range(NT)]

    for l in range(NL):
        xsrc = x_d if l == 0 else out_d
        with contextlib.ExitStack() as pa:
            def sba(name, shape, dt=F32):
                t = pa.enter_context(nc.sbuf_tensor("%s_l%d" % (name, l), list(shape), dt))
                return t, Buf(name)

            ST = ST_A
            NTS = ST // 128
            NST = S // ST
            WA, WAB = sba("WA", [128, 33536], BF16)
            prep = contextlib.ExitStack()
            for i in range(2):
                stage[i] = (prep.enter_context(nc.sbuf_tensor("stgA%d_%d" % (l, i), [128, 2048], F32)), Buf("stg"))
            w_in = WA[:, 0:8 * NIN].rearrange("p (c n) -> p c n", c=8)
            o1 = 8 * NIN
            w_out = WA[:, o1:o1 + 8 * D].rearrange("p (c n) -> p c n", c=8)
            o2 = o1 + 8 * D
            w_qb = WA[:, o2:o2 + 512].rearrange("p (c n) -> p c n", c=2)
            w_qi = WA[:, o2 + 512:o2 + 1024].rearrange("p (c n) -> p c n", c=2)
            w_kvb = WA[:, o2 + 1024:o2 + 1152]
            load_gcol(norm_mix_d[l])
            for c in range(8):
                cast_rows(w_in[:, c, :], WAB, w_in_d[l, c * 128:(c + 1) * 128, :], NIN, gcol[:, c:c + 1])
            for c in range(8):
                cast_rows(w_out[:, c, :], WAB, w_out_d[l, c * 128:(c + 1) * 128, :], D)
            for c in range(2):
                cast_rows(w_qb[:, c, :], WAB, w_qb_d[l, c * 128:(c + 1) * 128, :], 256)
                cast_rows(w_qi[:, c, :], WAB, w_qi_d[l, c * 128:(c + 1) * 128, :], 256)
            cast_rows(w_kvb, WAB, w_kvb_d[l, :, :], 128)
            k.barrier()
            prep.close()
            cw, cwB = sba("cw", [128, 12, 4])
            for c in range(12):
                k.dma("sp", cw[:, c, :], gdn_conv_d[l][:, c * 128:(c + 1) * 128].rearrange("j p -> p j"), cwB,
                      W=[cwB], group=True, allow_slow_non_contiguous=True)
            cdw, cdwB = sba("cdw", [128, 2, 31])
            for c in range(2):
                k.dma("sp", cdw[:, c, :], conv_dw_d[l][:, c * 128:(c + 1) * 128].rearrange("j p -> p j"), cdwB,
                      W=[cdwB], group=True, allow_slow_non_contiguous=True)
            cvec, cvecB = sba("cvec", [128, 3, 2])
            for i, v in enumerate((conv_b_d, ln_g_d, ln_b_d)):
                k.dma("sp", cvec[:, i, :], v[l].rearrange("(c p) -> p c", p=128), cvecB, W=[cvecB], group=True,
                      allow_slow_non_contiguous=True)
            gnc, gncB = sba("gnc", [128, 1])
            k.dma("sp", gnc[:, :], gdn_norm_d[l].rearrange("(p o) -> p o", o=1), gncB, W=[gncB])
            gv, gvB = sba("gv", [4, 4])
            k.dma("sp", gv[:, 0:1], a_log_d[l].rearrange("(p o) -> p o", o=1), gvB, W=[gvB])
            k.dma("sp", gv[:, 1:2], dt_bias_d[l].rearrange("(p o) -> p o", o=1), gvB, W=[gvB], group=True)
            k.op("act", ACT.activation, R=[gvB], W=[gvB], out=gv[:, 2:3], in_=gv[:, 0:1], func=AF.Exp)
            k.op("dve", V.tensor_scalar, R=[gvB], W=[gvB], out=gv[:, 3:4], in0=gv[:, 2:3], scalar1=-1.0,
                 scalar2=None, op0=ALU.mult)
            gq8, gq8B = sba("gq8", [128, 64])
            gkb, gkbB = sba("gkb", [128, 64])
            k.dma("sp", gq8[:, :], dq_norm_d[l].partition_broadcast(128), gq8B, W=[gq8B])
            k.dma("sp", gkb[:, :], dk_norm_d[l].partition_broadcast(128), gkbB, W=[gkbB])
            k.op("dve", V.tensor_scalar, R=[gq8B], W=[gq8B], out=gq8[:, :], in0=gq8[:, :], scalar1=0.125,
                 scalar2=None, op0=ALU.mult)
            dgc, dgcB = sba("dgc", [128, 2, 31, 128], BF16)
            for i in range(2):
                for j in range(31):
                    k.op("pool", POOL.tensor_scalar, R=[identB, cdwB], W=[dgcB], out=dgc[:, i, j, :],
                         in0=ident_f[:, :], scalar1=cdw[:, i, j:j + 1], scalar2=1.0, op0=ALU.mult, op1=ALU.mult)
            pre, preB = sba("pre", [128, 12, 3 + ST])
            gl, glB = sba("gl", [128, 2, 30 + ST], BF16)
            k.op("pool", POOL.memset, W=[preB], ap=pre[:, :, :], constant=0.0)
            k.op("pool", POOL.memset, W=[glB], ap=gl[:, :, :], constant=0.0)
            qkvT, _ = sba("qkvT", [128, 12, ST])
            qkvB = [Buf("qkv%d" % i) for i in range(12)]
            zs, zsB = sba("zs", [128, 4, ST])
            cqT, cqB = sba("cqT", [128, 2, ST], BF16)
            ckvT, ckvB = sba("ckvT", [128, ST], BF16)
            yT, yTB = sba("yT", [128, 8, ST], BF16)
            Sst = [sba("S%d" % i, [128, 4, 128]) for i in range(2)]
            k.op("pool", POOL.memset, W=[Sst[0][1]], ap=Sst[0][0][:, :, :], constant=0.0)
            KKT, _ = sba("KKT", [128, S], BF16)
            kkB = [Buf("kk%d" % t) for t in range(NT)]
            Vc, _ = sba("Vc", [128, NT, 65], BF16)
            vcB = [Buf("vc%d" % t) for t in range(NT)]
            VcI, VcIB = sba("VcI", [128, 1], BF16)
            work, workB = sba("work", [128, S])
            rows, rowsB = sba("rows", [4, 8, ST])
            cols = [sba("cols%d" % i, [128, 16]) for i in range(2)]
            kkt, kktB = sba("kkt", [128, 2, 128], BF16)
            wab, wabB = sba("wab", [128, 2, 8])
            gd = {n: sba("gd_" + n, [128, 512]) for n in
                  ("Kbd", "Kdec", "Vb", "QKT", "qd", "P0", "P1", "PT0", "PT1", "RT0", "RT1")}
            QQs = [sba("QQ%d" % i, [128, 512], BF16) for i in range(2)]
            mks = [sba("mk%d" % i, [128, 512], BF16) for i in range(2)]
            mkc = [0]
            print("sbuf remaining (phase A)", nc.sbuf_bytes_remaining)
            NSC = 8
            scr = [sba("scr%d" % i, [128, 512]) for i in range(NSC)]
            scrc = [0]

            def scratch():
                t, b = scr[scrc[0] % NSC]
                scrc[0] += 1
                return t, b
            NSB = 6
            scb = [sba("scb%d" % i, [128, 512], BF16) for i in range(NSB)]
            scbc = [0]

            def scratchb():
                t, b = scb[scbc[0] % NSB]
                scbc[0] += 1
                return t, b
            m8s = [sba("m8_%d" % i, [128, 8]) for i in range(2)]
            KB = 26
            pw2, pw2B = sba("pw2", [128, KB])
            for kk_ in range(KB):
                k.op("pool", POOL.memset, W=[pw2B], ap=pw2[:, kk_:kk_ + 1], constant=-(2.0 ** -(kk_ + 1)))
            bst, bstB = sba("bst", [128, 16])
            nst, nstB = sba("nst", [128, KB])
            junk8 = xts[1][0][:, :].bitcast(mybir.dt.int8)
            junkB = xts[1][1]
            sm = [sba("sm%d" % i, [128, 16]) for i in range(4)]
            smc = [0]

            def small():
                t, b = sm[smc[0] % 4]
                smc[0] += 1
                return t, b

            for t in range(NT):
                k.op("pool", POOL.memset, W=[vcB[t]], ap=Vc[:, t, 64:65], constant=1.0)

            def v3(ap, a=4):
                return ap.rearrange("p (a b) -> p a b", a=a)

            for s_i in range(NST):
                if stop == "W":
                    break
                t0 = s_i * ST
                tiles = [s_i * NTS + j for j in range(NTS)]
                for j in range(NTS):
                    xt, xtB = xts[j]
                    k.dma("sp", xt[:, :], xsrc[t0 + j * 128:t0 + (j + 1) * 128, :], xtB, R=[xdB[tiles[j]]],
                          W=[xtB])
                    norm_T(xt, xtB, j * 128)

                def featmajor(col0, M):
                    pt, pB = psum()
                    for c in range(8):
                        k.op("pe", PE.matmul, R=[WAB, hTB], W=[pB], out=pt[:M, :ST],
                             lhsT=w_in[:, c, col0:col0 + M], rhs=hT[:, c, 0:ST], start=(c == 0), stop=(c == 7))
                    return pt, pB

                for i in range(12):
                    pt, pB = featmajor(i * 128, 128)
                    k.op("act", ACT.copy, R=[pB], W=[preB], out=pre[:, i, 3:3 + ST], in_=pt[:, :ST])
                    k.op("dve", V.tensor_scalar, R=[preB, cwB], W=[qkvB[i]], out=qkvT[:, i, :], in0=pre[:, i, 0:ST],
                         scalar1=cw[:, i, 0:1], scalar2=None, op0=ALU.mult)
                    for j in range(1, 4):
                        k.op("dve", V.scalar_tensor_tensor, R=[preB, cwB, qkvB[i]], W=[qkvB[i]], out=qkvT[:, i, :],
                             in0=pre[:, i, j:j + ST], scalar=cw[:, i, j:j + 1], in1=qkvT[:, i, :], op0=ALU.mult,
                             op1=ALU.add)
                    k.op("act", ACT.activation, R=[qkvB[i]], W=[qkvB[i]], out=qkvT[:, i, :], in_=qkvT[:, i, :],
                         func=AF.Silu)
                k.op("dve", V.tensor_copy, R=[preB], W=[preB], out=pre[:, :, 0:3], in_=pre[:, :, ST:ST + 3])
                for i in range(8):
                    sq, sqB = scratch()
                    k.op("act", ACT.activation, R=[qkvB[i]], W=[sqB], out=sq[:, :ST], in_=qkvT[:, i, :],
                         func=AF.Square)
                    pt, pB = psum()
                    k.op("pe", PE.matmul, R=[onesSB, sqB], W=[pB], out=pt[:, :ST], lhsT=onesS[:, :],
                         rhs=sq[:, :ST], start=True, stop=True)
                    k.op("act", ACT.activation, R=[pB, epsB], W=[sqB], out=sq[:, :ST], in_=pt[:, :ST],
                         func=AF.Sqrt, bias=epsc[:, 0:1])
                    k.op("dve", V.reciprocal, R=[sqB], W=[sqB], out=sq[:, :ST], in_=sq[:, :ST])
                    k.op("dve", V.scalar_tensor_tensor, R=[qkvB[i], sqB], W=[qkvB[i]], out=qkvT[:, i, :],
                         in0=qkvT[:, i, :], scalar=(128 ** -0.5 if i < 4 else 1.0), in1=sq[:, :ST],
                         op0=ALU.mult, op1=ALU.mult)
                for i in range(4):
                    pt, pB = featmajor(1536 + i * 128, 128)
                    k.op("act", ACT.activation, R=[pB], W=[zsB], out=zs[:, i, :], in_=pt[:, :ST], func=AF.Silu)
                for i in range(2):
                    pt, pB = featmajor(C_CQ + i * 128, 128)
                    k.op("act", ACT.copy, R=[pB], W=[cqB], out=cqT[:, i, :], in_=pt[:, :ST])
                pt, pB = featmajor(C_CKV, 128)
                k.op("act", ACT.copy, R=[pB], W=[ckvB], out=ckvT[:, :], in_=pt[:, :ST])
                for i in range(2):
                    pu, puB = featmajor(C_UG + i * 128, 128)
                    pg, pgB = featmajor(C_UG + 256 + i * 128, 128)
                    sg, sgB = scratch()
                    k.op("act", ACT.activation, R=[pgB], W=[sgB], out=sg[:, :ST], in_=pg[:, :ST], func=AF.Sigmoid)
                    k.op("dve", V.tensor_tensor, R=[puB, sgB], W=[glB], out=gl[:, i, 30:30 + ST], in0=pu[:, :ST],
                         in1=sg[:, :ST], op=ALU.mult)
                for j in range(NTS):
                    pt, pB = psum()
                    for c in range(8):
                        k.op("pe", PE.matmul, R=[WAB, hTB], W=[pB], out=pt[:, :68],
                             lhsT=hT[:, c, j * 128:(j + 1) * 128], rhs=w_in[:, c, C_KI:C_KI + 68],
                             start=(c == 0), stop=(c == 7))
                    k.op("act", ACT.copy, R=[pB], W=[kktB], out=kkt[:, j, 64:128], in_=pt[:, 0:64])
                    k.op("act", ACT.activation, R=[pB], W=[wabB], out=wab[:, j, 0:4], in_=pt[:, 64:68],
                         func=AF.Abs, scale=1.0 / 16)
                    k.op("act", ACT.activation, R=[pB], W=[wabB], out=wab[:, j, 4:8], in_=pt[:, 64:68],
                         func=AF.Sign)
                if stop == "A1":
                    break
                pa_, paB = featmajor(C_A, 4)
                pb_, pbB = featmajor(C_B, 4)
                r = rows
                k.op("act", ACT.activation, R=[paB, gvB], W=[rowsB], out=r[:, 0, :], in_=pa_[:4, :ST],
                     func=AF.Abs, bias=gv[:, 1:2])
                k.op("act", ACT.activation, R=[rowsB], W=[rowsB], out=r[:, 1, :], in_=r[:, 0, :], func=AF.Exp,
                     scale=-1.0)
                k.op("act", ACT.activation, R=[rowsB, oneB], W=[rowsB], out=r[:, 1, :], in_=r[:, 1, :], func=AF.Ln,
                     bias=onec[0:4, 0:1])
                k.op("dve", V.tensor_scalar, R=[paB, gvB], W=[rowsB], out=r[:, 2, :], in0=pa_[:4, :ST],
                     scalar1=gv[:, 1:2], scalar2=0.0, op0=ALU.add, op1=ALU.max)
                k.op("dve", V.tensor_tensor, R=[rowsB], W=[rowsB], out=r[:, 2, :], in0=r[:, 2, :], in1=r[:, 1, :],
                     op=ALU.add)
                k.op("dve", V.tensor_scalar, R=[rowsB, gvB], W=[rowsB], out=r[:, 2, :], in0=r[:, 2, :],
                     scalar1=gv[:, 3:4], scalar2=None, op0=ALU.mult)
                for ch in range(NTS):
                    cs = slice(ch * 128, (ch + 1) * 128)
                    k.op("dve", V.tensor_tensor_scan, R=[rowsB, onesB], W=[rowsB], out=r[:, 3, cs],
                         data0=ones_f[0:4, 0:128], data1=r[:, 2, cs], initial=0.0, op0=ALU.mult, op1=ALU.add)
                k.op("act", ACT.activation, R=[pbB], W=[rowsB], out=r[:, 4, :], in_=pb_[:4, :ST], func=AF.Sigmoid)
                k.op("act", ACT.activation, R=[rowsB], W=[rowsB], out=r[:, 5, :], in_=r[:, 3, :], func=AF.Exp)
                for ch in range(NTS):
                    cs = slice(ch * 128, (ch + 1) * 128)
                    k.op("dve", V.tensor_scalar, R=[rowsB], W=[rowsB], out=r[:, 6, cs], in0=r[:, 3, cs],
                         scalar1=r[:, 3, ch * 128 + 127:ch * 128 + 128], scalar2=-1.0, op0=ALU.subtract,
                         op1=ALU.mult)
                k.op("act", ACT.activation, R=[rowsB], W=[rowsB], out=r[:, 6, :], in_=r[:, 6, :], func=AF.Exp)
                k.op("dve", V.tensor_tensor, R=[rowsB], W=[rowsB], out=r[:, 7, :], in0=r[:, 4, :], in1=r[:, 5, :],
                     op=ALU.mult)

                if stop == "A2":
                    break
                for ch in range(NTS):
                    cs = slice(ch * 128, (ch + 1) * 128)
                    col, colB = cols[ch]
                    pt, pB = psum()
                    for qi_, rq in enumerate((3, 4, 7, 6)):
                        k.op("pe", PE.matmul, R=[rowsB, identB], W=[pB], out=pt[:, qi_ * 4:(qi_ + 1) * 4],
                             lhsT=r[:, rq, cs], rhs=ident_f[0:4, 0:4], start=True, stop=True)
                    k.op("act", ACT.copy, R=[pB], W=[colB], out=col[:, :], in_=pt[:, 0:16])
                    c_b, c_beta, c_be, c_elb = (col[:, 0:4], col[:, 4:8], col[:, 8:12], col[:, 12:16])
                    prb, prbB = psum()
                    pre_, preB_ = psum()
                    for h in range(4):
                        k.op("pe", PE.matmul, R=[selB, rowsB], W=[prbB], out=prb[:, h * 128:(h + 1) * 128],
                             lhsT=sel[:, h, :], rhs=r[:, 3, cs], start=True, stop=True)
                    for h in range(4):
                        k.op("pe", PE.matmul, R=[selB, rowsB], W=[preB_], out=pre_[:, h * 128:(h + 1) * 128],
                             lhsT=sel[:, h, :], rhs=r[:, 5, cs], start=True, stop=True)
                    qd, qdB = gd["qd"]
                    k.op("dve", V.tensor_tensor, R=[preB_] + qkvB[0:4], W=[qdB], out=v3(qd[:, :]),
                         in0=qkvT[:, 0:4, cs], in1=v3(pre_[:, :]), op=ALU.mult)
                    ebl, eblB = small()
                    k.op("dve", V.tensor_copy, R=[preB_], W=[eblB], out=ebl[:, 0:4], in_=v3(pre_[:, :])[:, :, 127])
                    pk, pkB = psum()
                    pv, pvB = psum()
                    for h in range(4):
                        k.op("pe", PE.transpose, R=[qkvB[4 + h], identB], W=[pkB], out=pk[:, h * 128:(h + 1) * 128],
                             in_=qkvT[:, 4 + h, cs], identity=ident_f[:, :])
                    for h in range(4):
                        k.op("pe", PE.transpose, R=[qkvB[8 + h], identB], W=[pvB], out=pv[:, h * 128:(h + 1) * 128],
                             in_=qkvT[:, 8 + h, cs], identity=ident_f[:, :])
                    Kbd, KbdB = gd["Kbd"]
                    Kdec, KdecB = gd["Kdec"]
                    Vb, VbB = gd["Vb"]
                    k.op("dve", V.tensor_tensor, R=[pkB, colB], W=[KbdB], out=v3(Kbd[:, :]), in0=v3(pk[:, :]),
                         in1=bc(c_be.unsqueeze(2), [128, 4, 128]), op=ALU.mult)
                    k.op("dve", V.tensor_tensor, R=[pkB, colB], W=[KdecB], out=v3(Kdec[:, :]), in0=v3(pk[:, :]),
                         in1=bc(c_elb.unsqueeze(2), [128, 4, 128]), op=ALU.mult)
                    k.op("dve", V.tensor_tensor, R=[pvB, colB], W=[VbB], out=v3(Vb[:, :]), in0=v3(pv[:, :]),
                         in1=bc(c_beta.unsqueeze(2), [128, 4, 128]), op=ALU.mult)
                    if stop == "A3a":
                        break
                    pg1, pg1B = psum()
                    pg2, pg2B = psum()
                    for h in range(4):
                        k.op("pe", PE.matmul, R=[qkvB[4 + h]], W=[pg1B], out=pg1[:, h * 128:(h + 1) * 128],
                             lhsT=qkvT[:, 4 + h, cs], rhs=qkvT[:, 4 + h, cs], start=True, stop=True)
                    for h in range(4):
                        k.op("pe", PE.matmul, R=[qkvB[4 + h], qkvB[h]], W=[pg2B], out=pg2[:, h * 128:(h + 1) * 128],
                             lhsT=qkvT[:, 4 + h, cs], rhs=qkvT[:, h, cs], start=True, stop=True)
                    GT, GTB = scratch()
                    k.op("dve", V.tensor_tensor, R=[prbB, colB], W=[GTB], out=v3(GT[:, :]), in0=v3(prb[:, :]),
                         in1=bc(c_b.unsqueeze(2), [128, 4, 128]), op=ALU.subtract)
                    k.op("pool", POOL.affine_select, R=[GTB], W=[GTB], out=v3(GT[:, :]), in_=v3(GT[:, :]),
                         pattern=[[0, 4], [1, 128]], compare_op=ALU.is_ge, fill=FILLM, base=0,
                         channel_multiplier=-1)
                    k.op("act", ACT.activation, R=[GTB], W=[GTB], out=GT[:, :], in_=GT[:, :], func=AF.Exp)
                    QKT, QKTB = gd["QKT"]
                    k.op("dve", V.tensor_tensor, R=[pg2B, GTB], W=[QKTB], out=QKT[:, :], in0=pg2[:, :], in1=GT[:, :],
                         op=ALU.mult)
                    MT, MTB = scratch()
                    k.op("dve", V.tensor_tensor, R=[pg1B, GTB], W=[MTB], out=MT[:, :], in0=pg1[:, :], in1=GT[:, :],
                         op=ALU.mult)
                    k.op("pool", POOL.affine_select, R=[MTB], W=[MTB], out=v3(MT[:, :]), in_=v3(MT[:, :]),
                         pattern=[[0, 4], [1, 128]], compare_op=ALU.is_gt, fill=FILL0, base=0, channel_multiplier=-1)
                    if stop == "A3b":
                        break
                    pA, pAB = psum()
                    for h in range(4):
                        k.op("pe", PE.transpose, R=[MTB, identB], W=[pAB], out=pA[:, h * 128:(h + 1) * 128],
                             in_=MT[:, h * 128:(h + 1) * 128], identity=ident_f[:, :])
                    P, PB = gd["P0"]
                    k.op("dve", V.tensor_tensor, R=[pAB, colB], W=[PB], out=v3(P[:, :]), in0=v3(pA[:, :]),
                         in1=bc(c_beta.unsqueeze(2), [128, 4, 128]), op=ALU.mult)
                    if stop == "A3b1":
                        break
                    pAT, pATB = psum()
                    for h in range(4):
                        k.op("pe", PE.transpose, R=[PB, identB], W=[pATB], out=pAT[:, h * 128:(h + 1) * 128],
                             in_=P[:, h * 128:(h + 1) * 128], identity=ident_f[:, :])
                    if stop == "A3b1a":
                        break
                    PT_, PTB = gd["PT0"]
                    k.op("act", ACT.copy, R=[pATB], W=[PTB], out=PT_[:, :], in_=pAT[:, :])
                    if stop == "A3b1b":
                        break
                    RT, RTB = gd["RT0"]
                    for h in range(4):
                        hs = slice(h * 128, (h + 1) * 128)
                        k.op("dve", V.tensor_tensor, R=[identB, PTB], W=[RTB], out=RT[:, hs], in0=ident_f[:, :],
                             in1=PT_[:, hs], op=ALU.subtract)
                    if stop == "A3b2":
                        break
                    for lvl in range(6):
                        if stop == "A3b3" and lvl == 1:
                            break
                        p2, p2B = psum()
                        for h in range(4):
                            hs = slice(h * 128, (h + 1) * 128)
                            k.op("pe", PE.matmul, R=[PTB, PB], W=[p2B], out=p2[:, hs], lhsT=PT_[:, hs], rhs=P[:, hs],
                                 start=True, stop=True)
                        if lvl < 5:
                            p2t, p2tB = psum()
                            for h in range(4):
                                hs = slice(h * 128, (h + 1) * 128)
                                k.op("pe", PE.matmul, R=[PTB, PB], W=[p2tB], out=p2t[:, hs], lhsT=P[:, hs],
                                     rhs=PT_[:, hs], start=True, stop=True)
                        Pn, PnB = gd["P%d" % ((lvl + 1) % 2)]
                        k.op("act", ACT.copy, R=[p2B], W=[PnB], out=Pn[:, :], in_=p2[:, :])
                        if lvl < 5:
                            PTn, PTnB = gd["PT%d" % ((lvl + 1) % 2)]
                            k.op("act", ACT.copy, R=[p2tB], W=[PTnB], out=PTn[:, :], in_=p2t[:, :])
                        p3, p3B = psum()
                        for h in range(4):
                            hs = slice(h * 128, (h + 1) * 128)
                            k.op("pe", PE.matmul, R=[PnB, RTB], W=[p3B], out=p3[:, hs], lhsT=Pn[:, hs], rhs=RT[:, hs],
                                 start=True, stop=True)
                        RTn, RTnB = gd["RT%d" % ((lvl + 1) % 2)]
                        k.op("dve", V.tensor_tensor, R=[p3B, RTB], W=[RTnB], out=RTn[:, :], in0=p3[:, :], in1=RT[:, :],
                             op=ALU.add)
                        RT, RTB = RTn, RTnB
                        P, PB = Pn, PnB
                        if lvl < 5:
                            PT_, PTB = PTn, PTnB
                    if stop in ("A3c", "A3b3"):
                        break
                    pw, pwB = psum()
                    pu_, puB_ = psum()
                    for h in range(4):
                        hs = slice(h * 128, (h + 1) * 128)
                        k.op("pe", PE.matmul, R=[KbdB, RTB], W=[pwB], out=pw[:, hs], lhsT=Kbd[:, hs], rhs=RT[:, hs],
                             start=True, stop=True)
                    for h in range(4):
                        hs = slice(h * 128, (h + 1) * 128)
                        k.op("pe", PE.matmul, R=[VbB, RTB], W=[puB_], out=pu_[:, hs], lhsT=RT[:, hs], rhs=Vb[:, hs],
                             start=True, stop=True)
                    WT, WTB = scratch()
                    U, UB = scratch()
                    k.op("act", ACT.copy, R=[pwB], W=[WTB], out=WT[:, :], in_=pw[:, :])
                    k.op("act", ACT.copy, R=[puB_], W=[UB], out=U[:, :], in_=pu_[:, :])
                    tg = NTS * s_i + ch
                    Sc, ScB = Sst[tg % 2]
                    Sn, SnB = Sst[(tg + 1) % 2]
                    pws, pwsB = psum()
                    for h in range(4):
                        hs = slice(h * 128, (h + 1) * 128)
                        k.op("pe", PE.matmul, R=[WTB, ScB], W=[pwsB], out=pws[:, hs], lhsT=WT[:, hs], rhs=Sc[:, h, :],
                             start=True, stop=True)
                    Vn, VnB = scratch()
                    k.op("dve", V.scalar_tensor_tensor, R=[UB, pwsB], W=[VnB], out=Vn[:, :], in0=pws[:, :], scalar=-1.0,
                         in1=U[:, :], op0=ALU.mult, op1=ALU.add)
                    po, poB = psum()
                    for h in range(4):
                        hs = slice(h * 128, (h + 1) * 128)
                        k.op("pe", PE.matmul, R=[ScB, qdB], W=[poB], out=po[:, hs], lhsT=Sc[:, h, :], rhs=qd[:, hs],
                             start=True, stop=False)
                        k.op("pe", PE.matmul, R=[VnB, QKTB], W=[poB], out=po[:, hs], lhsT=Vn[:, hs], rhs=QKT[:, hs],
                             start=False, stop=True)
                    ps_, psB_ = psum()
                    for h in range(4):
                        hs = slice(h * 128, (h + 1) * 128)
                        k.op("pe", PE.matmul, R=[KdecB, VnB], W=[psB_], out=ps_[:, hs], lhsT=Kdec[:, hs], rhs=Vn[:, hs],
                             start=True, stop=True)
                    k.op("dve", V.tensor_tensor, R=[ScB, eblB], W=[SnB], out=Sn[:, :, :], in0=Sc[:, :, :],
                         in1=bc(ebl[:, 0:4].unsqueeze(2), [128, 4, 128]), op=ALU.mult)
                    k.op("dve", V.tensor_tensor, R=[SnB, psB_], W=[SnB], out=Sn[:, :, :], in0=Sn[:, :, :],
                         in1=v3(ps_[:, :]), op=ALU.add)
                    o32, o32B = scratch()
                    osq, osqB = scratch()
                    k.op("act", ACT.copy, R=[poB], W=[o32B], out=o32[:, :], in_=po[:, :])
                    k.op("act", ACT.activation, R=[poB], W=[osqB], out=osq[:, :], in_=po[:, :], func=AF.Square)
                    pm_, pmB_ = psum()
                    k.op("pe", PE.matmul, R=[o128B, osqB], W=[pmB_], out=pm_[:, :], lhsT=ones128[:, :], rhs=osq[:, :],
                         start=True, stop=True)
                    k.op("act", ACT.activation, R=[pmB_, epsB], W=[osqB], out=osq[:, :], in_=pm_[:, :], func=AF.Sqrt,
                         bias=epsc[:, 0:1])
                    k.op("dve", V.reciprocal, R=[osqB], W=[osqB], out=osq[:, :], in_=osq[:, :])
                    k.op("dve", V.scalar_tensor_tensor, R=[o32B, osqB, gncB], W=[o32B], out=o32[:, :], in0=o32[:, :],
                         scalar=gnc[:, 0:1], in1=osq[:, :], op0=ALU.mult, op1=ALU.mult)
                    k.op("dve", V.tensor_tensor, R=[o32B, zsB], W=[yTB], out=yT[:, 0:4, cs], in0=v3(o32[:, :]),
                         in1=zs[:, :, cs], op=ALU.mult)

                if stop in ("A3", "A3a", "A3b", "A3c", "A3b1", "A3b2", "A3b3", "A3b1a", "A3b1b"):
                    break
                for j in range(NTS):
                    t = tiles[j]
                    cs = slice(j * 128, (j + 1) * 128)
                    pq, pqB = psum()
                    for c in range(2):
                        k.op("pe", PE.matmul, R=[cqB, WAB], W=[pqB], out=pq[:, 0:256], lhsT=cqT[:, c, cs],
                             rhs=w_qb[:, c, :], start=(c == 0), stop=(c == 1))
                    for c in range(2):
                        k.op("pe", PE.matmul, R=[cqB, WAB], W=[pqB], out=pq[:, 256:512], lhsT=cqT[:, c, cs],
                             rhs=w_qi[:, c, :], start=(c == 0), stop=(c == 1))
                    pkv, pkvB = psum()
                    k.op("pe", PE.matmul, R=[ckvB, WAB], W=[pkvB], out=pkv[:, 0:128], lhsT=ckvT[:, cs], rhs=w_kvb,
                         start=True, stop=True)
                    sq, sqB = scratch()
                    k.op("act", ACT.activation, R=[pqB], W=[sqB], out=sq[:, 0:256], in_=pq[:, 0:256], func=AF.Square)
                    k.op("act", ACT.activation, R=[pkvB], W=[sqB], out=sq[:, 256:320], in_=pkv[:, 0:64],
                         func=AF.Square)
                    ss, ssB = small()
                    k.op("dve", V.tensor_reduce, R=[sqB], W=[ssB], out=ss[:, 0:5],
                         in_=sq[:, 0:320].rearrange("p (a b) -> p a b", a=5), axis=AX.X, op=ALU.add)
                    k.op("dve", V.tensor_scalar, R=[ssB], W=[ssB], out=ss[:, 5:10], in0=ss[:, 0:5], scalar1=1.0 / 64,
                         scalar2=EPS, op0=ALU.mult, op1=ALU.add)
                    k.op("act", ACT.activation, R=[ssB], W=[ssB], out=ss[:, 5:10], in_=ss[:, 5:10], func=AF.Sqrt)
                    k.op("dve", V.reciprocal, R=[ssB], W=[ssB], out=ss[:, 10:15], in_=ss[:, 5:10])
                    tq, tqB = scratch()
                    k.op("dve", V.tensor_tensor, R=[pqB, ssB], W=[tqB], out=v3(tq[:, 0:256]), in0=v3(pq[:, 0:256]),
                         in1=bc(ss[:, 10:14].unsqueeze(2), [128, 4, 64]), op=ALU.mult)
                    QI, QIB = scratchb()
                    QI3 = v3(QI[:, :])
                    k.op("dve", V.tensor_tensor, R=[tqB, gq8B], W=[QIB], out=QI3[:, :, 0:64], in0=v3(tq[:, 0:256]),
                         in1=bc(gq8[:, :].unsqueeze(1), [128, 4, 64]), op=ALU.mult)
                    k.op("dve", V.tensor_tensor, R=[pqB, wabB, ssB], W=[QIB], out=QI3[:, :, 64:128], in0=v3(pq[:, 256:512]),
                         in1=bc(wab[:, j, 0:4].unsqueeze(2), [128, 4, 64]), op=ALU.mult)
                    k.op("dve", V.scalar_tensor_tensor, R=[pkvB, ssB, gkbB], W=[kktB], out=kkt[:, j, 0:64],
                         in0=pkv[:, 0:64], scalar=ss[:, 14:15], in1=gkb[:, :], op0=ALU.mult, op1=ALU.mult)
                    k.op("dve", V.tensor_copy, R=[pkvB, ssB], W=[vcB[t]], out=Vc[:, t, 0:64], in_=pkv[:, 64:128])
                    ptq, ptqB = psum()
                    for h in range(4):
                        k.op("pe", PE.transpose, R=[QIB, identbB], W=[ptqB], out=bfv(ptq)[:, h * 128:(h + 1) * 128],
                             in_=QI3[:, h, :], identity=ident_b[:, :])
                    QQ, QQB = QQs[t % 2]
                    k.op("act", ACT.copy, R=[ptqB], W=[QQB], out=QQ[:, :], in_=bfv(ptq)[:, 0:512])
                    ptk, ptkB = psum()
                    k.op("pe", PE.transpose, R=[kktB, identbB], W=[ptkB], out=bfv(ptk)[:, 0:128], in_=kkt[:, j, :],
                         identity=ident_b[:, :])
                    k.op("act", ACT.copy, R=[ptkB], W=[kkB[t]], out=KKT[:, t * 128:(t + 1) * 128],
                         in_=bfv(ptk)[:, 0:128])
                    Sw = (t + 1) * 128
                    for kc0 in range(0, Sw, 512):
                        wd = min(512, Sw - kc0)
                        kts = list(range(kc0 // 128, (kc0 + wd) // 128))
                        rr = []
                        for h in range(4):
                            ph, phB = psum()
                            k.op("pe", PE.matmul, R=[QQB] + [kkB[u] for u in kts], W=[phB], out=ph[:, :wd],
                                 lhsT=QQ[64:128, h * 128:(h + 1) * 128], rhs=KKT[64:128, kc0:kc0 + wd],
                                 start=True, stop=True)
                            rh, rhB = scratch()
                            k.op("act", ACT.activation, R=[phB], W=[rhB], out=rh[:, :wd], in_=ph[:, :wd], func=AF.Relu)
                            rr.append((rh, rhB))
                        k.op("dve", V.tensor_scalar, R=[rr[0][1], wabB], W=[workB], out=work[:, kc0:kc0 + wd],
                             in0=rr[0][0][:, :wd], scalar1=wab[:, j, 4:5], scalar2=None, op0=ALU.mult)
                        for h in range(1, 4):
                            k.op("dve", V.scalar_tensor_tensor, R=[rr[h][1], wabB, workB], W=[workB],
                                 out=work[:, kc0:kc0 + wd], in0=rr[h][0][:, :wd], scalar=wab[:, j, 4 + h:5 + h],
                                 in1=work[:, kc0:kc0 + wd], op0=ALU.mult, op1=ALU.add)
                    if t >= 2:
                        k.op("dve", V.tensor_reduce, R=[workB], W=[bstB], out=bst[:, 0:1], in_=work[:, :Sw], axis=AX.X,
                             op=ALU.min)
                        m8, m8B = m8s[0]
                        k.op("dve", V.max, R=[workB], W=[m8B], out=m8[:, :], in_=work[:, :Sw])
                    k.op("pool", POOL.affine_select, R=[workB], W=[workB], out=work[:, t * 128:(t + 1) * 128],
                         in_=work[:, t * 128:(t + 1) * 128], pattern=[[-1, 128]], compare_op=ALU.is_ge, fill=FILLNC,
                         base=0, channel_multiplier=1)
                    if t >= 2:
                        k.op("dve", V.tensor_tensor, R=[m8B, bstB], W=[bstB], out=bst[:, 2:3], in0=m8[:, 0:1],
                             in1=bst[:, 0:1], op=ALU.subtract)
                        k.op("dve", V.tensor_scalar, R=[bstB, pw2B], W=[nstB], out=nst[:, :], in0=pw2[:, :],
                             scalar1=bst[:, 2:3], scalar2=None, op0=ALU.mult)
                        k.op("dve", V.tensor_scalar, R=[bstB, m8B], W=[bstB], out=bst[:, 4:5], in0=bst[:, 0:1],
                             scalar1=m8[:, 0:1], scalar2=-0.5, op0=ALU.add, op1=ALU.mult)
                        k.op("dve", V.tensor_copy, R=[bstB], W=[bstB], out=bst[:, 3:4], in_=bst[:, 0:1])
                        thr = float(511 - Sw)
                        for it in range(KB):
                            nm = bst[:, 4 + it % 2:5 + it % 2]
                            nmn = bst[:, 4 + (it + 1) % 2:5 + (it + 1) % 2]
                            cn = bst[:, 6 + it % 2:7 + it % 2]
                            k.op("act", ACT.activation, R=[workB, bstB], W=[junkB, bstB], out=junk8[:, :Sw],
                                 in_=work[:, :Sw], func=AF.Sign, bias=nm, accum_out=cn)
                            k.op("dve", V.tensor_scalar, R=[bstB], W=[bstB], out=bst[:, 8:9], in0=cn, scalar1=thr,
                                 scalar2=0.5, op0=ALU.is_ge, op1=ALU.subtract)
                            k.op("dve", V.scalar_tensor_tensor, R=[bstB, nstB], W=[bstB], out=nmn, in0=bst[:, 8:9],
                                 scalar=nst[:, it:it + 1], in1=nm, op0=ALU.mult, op1=ALU.add)
                            k.op("dve", V.tensor_scalar, R=[bstB], W=[bstB], out=bst[:, 9:10], in0=bst[:, 8:9],
                                 scalar1=0.5, scalar2=1.0e30, op0=ALU.subtract, op1=ALU.mult)
                            k.op("dve", V.scalar_tensor_tensor, R=[bstB], W=[bstB], out=bst[:, 3:4], in0=bst[:, 9:10],
                                 scalar=nm, in1=bst[:, 3:4], op0=ALU.subtract, op1=ALU.max)
                    pso, psoB = psb[7]
                    for kc0 in range(0, Sw, 512):
                        wd = min(512, Sw - kc0)
                        mk, mkB = mks[mkc[0] % 2]
                        mkc[0] += 1
                        if t >= 2:
                            k.op("dve", V.tensor_scalar, R=[workB, bstB], W=[mkB], out=mk[:, :wd],
                                 in0=work[:, kc0:kc0 + wd], scalar1=bst[:, 3:4], scalar2=None, op0=ALU.is_ge)
                        else:
                            k.op("dve", V.tensor_single_scalar, R=[workB], W=[mkB], out=mk[:, :wd],
                                 in_=work[:, kc0:kc0 + wd], scalar=-1.0e38, op=ALU.is_gt)
                        for u in range(wd // 128):
                            kt = kc0 // 128 + u
                            pmk, pmkB = psum()
                            k.op("pe", PE.transpose, R=[mkB, identbB], W=[pmkB], out=bfv(pmk)[:, 0:128],
                                 in_=mk[:, u * 128:(u + 1) * 128], identity=ident_b[:, :])
                            pl, plB = psum()
                            k.op("pe", PE.matmul, R=[kkB[kt], QQB], W=[plB], out=pl[:, :],
                                 lhsT=KKT[0:64, kt * 128:(kt + 1) * 128], rhs=QQ[0:64, :], start=True, stop=True)
                            pT_, pTB = scratchb()
                            k.op("act", ACT.activation, R=[plB], W=[pTB], out=pT_[:, :], in_=pl[:, :], func=AF.Exp)
                            pmm, pmmB = scratchb()
                            k.op("dve", V.tensor_tensor, R=[pTB, pmkB], W=[pmmB], out=v3(pmm[:, :]), in0=v3(pT_[:, :]),
                                 in1=bc(bfv(pmk)[:, 0:128].unsqueeze(1), [128, 4, 128]), op=ALU.mult)
                            for h in range(4):
                                k.op("pe", PE.matmul, R=[pmmB, vcB[kt]], W=[psoB], out=pso[:, h * 65:(h + 1) * 65],
                                     lhsT=pmm[:, h * 128:(h + 1) * 128], rhs=Vc[:, kt, :],
                                     start=(kt == 0 and h == 0), stop=(kt == t and h == 3))
                    pso3 = pso[:, 0:260].rearrange("p (a b) -> p a b", a=4)
                    rd, rdB = small()
                    k.op("dve", V.reciprocal, R=[psoB], W=[rdB], out=rd[:, 0:4], in_=pso3[:, :, 64])
                    yb, ybB = scratchb()
                    k.op("dve", V.tensor_tensor, R=[psoB, rdB], W=[ybB], out=v3(yb[:, 0:256]), in0=pso3[:, :, 0:64],
                         in1=bc(rd[:, 0:4].unsqueeze(2), [128, 4, 64]), op=ALU.mult)
                    pty, ptyB = psum()
                    for c in range(2):
                        k.op("pe", PE.transpose, R=[ybB, identbB], W=[ptyB], out=bfv(pty)[:, c * 128:(c + 1) * 128],
                             in_=yb[:, c * 128:(c + 1) * 128], identity=ident_b[:, :])
                    k.op("act", ACT.copy, R=[ptyB], W=[yTB], out=yT[:, 4:6, cs],
                         in_=bfv(pty)[:, 0:256].rearrange("p (a b) -> p a b", a=2))

                if stop == "A4":
                    break
                yc, ycB = scratch()
                ysq, ysqB = scratch()
                for i in range(2):
                    pc, pcB = psum()
                    for jj in range(31):
                        k.op("pe", PE.matmul, R=[dgcB, glB], W=[pcB], out=pc[:, :ST], lhsT=dgc[:, i, jj, :],
                             rhs=gl[:, i, jj:jj + ST], start=(jj == 0), stop=(jj == 30))
                    k.op("act", ACT.activation, R=[pcB, cvecB], W=[ycB], out=yc[:, i * ST:(i + 1) * ST], in_=pc[:, :ST],
                         func=AF.Identity, bias=cvec[:, 0, i:i + 1])
                    k.op("act", ACT.activation, R=[pcB, cvecB], W=[ysqB], out=ysq[:, i * ST:(i + 1) * ST],
                         in_=pc[:, :ST], func=AF.Square, bias=cvec[:, 0, i:i + 1])
                k.op("dve", V.tensor_copy, R=[glB], W=[glB], out=gl[:, :, 0:30], in_=gl[:, :, ST:ST + 30])
                pmu, pmuB = psum()
                pms, pmsB = psum()
                for i in range(2):
                    k.op("pe", PE.matmul, R=[o256B, ycB], W=[pmuB], out=pmu[:, :ST], lhsT=ones256[:, :],
                         rhs=yc[:, i * ST:(i + 1) * ST], start=(i == 0), stop=(i == 1))
                for i in range(2):
                    k.op("pe", PE.matmul, R=[o256B, ysqB], W=[pmsB], out=pms[:, :ST], lhsT=ones256[:, :],
                         rhs=ysq[:, i * ST:(i + 1) * ST], start=(i == 0), stop=(i == 1))
                mu, muB = scratch()
                k.op("act", ACT.copy, R=[pmuB], W=[muB], out=mu[:, 0:ST], in_=pmu[:, :ST])
                k.op("act", ACT.activation, R=[pmuB], W=[muB], out=mu[:, ST:2 * ST], in_=pmu[:, :ST], func=AF.Square)
                k.op("dve", V.tensor_tensor, R=[pmsB, muB], W=[muB], out=mu[:, ST:2 * ST], in0=pms[:, :ST],
                     in1=mu[:, ST:2 * ST], op=ALU.subtract)
                k.op("act", ACT.activation, R=[muB, epsB], W=[muB], out=mu[:, ST:2 * ST], in_=mu[:, ST:2 * ST],
                     func=AF.Sqrt, bias=epsc[:, 0:1])
                k.op("dve", V.reciprocal, R=[muB], W=[muB], out=mu[:, ST:2 * ST], in_=mu[:, ST:2 * ST])
                for i in range(2):
                    k.op("dve", V.tensor_tensor, R=[ycB, muB], W=[ycB], out=yc[:, i * ST:(i + 1) * ST],
                         in0=yc[:, i * ST:(i + 1) * ST], in1=mu[:, 0:ST], op=ALU.subtract)
                    k.op("dve", V.tensor_tensor, R=[ycB, muB], W=[ycB], out=yc[:, i * ST:(i + 1) * ST],
                         in0=yc[:, i * ST:(i + 1) * ST], in1=mu[:, ST:2 * ST], op=ALU.mult)
                    k.op("act", ACT.activation, R=[ycB, cvecB], W=[yTB], out=yT[:, 6 + i, :],
                         in_=yc[:, i * ST:(i + 1) * ST], func=AF.Silu, scale=cvec[:, 1, i:i + 1],
                         bias=cvec[:, 2, i:i + 1])

                for j in range(NTS):
                    xt, xtB = xts[j]
                    t = tiles[j]
                    for n in range(2):
                        pt, pB = psum()
                        for c in range(8):
                            k.op("pe", PE.matmul, R=[yTB, WAB], W=[pB], out=pt[:, :],
                                 lhsT=yT[:, c, j * 128:(j + 1) * 128], rhs=w_out[:, c, n * 512:(n + 1) * 512],
                                 start=(c == 0), stop=(c == 7))
                        k.op("dve", V.tensor_tensor, R=[pB, xtB], W=[xtB], out=xt[:, n * 512:(n + 1) * 512],
                             in0=pt[:, :], in1=xt[:, n * 512:(n + 1) * 512], op=ALU.add)
                    k.dma("sp", out_d[t * 128:(t + 1) * 128, :], xt[:, :], xtB, R=[xtB], W=[xdB[t]])
        k.barrier()
        if stop in ("A", "W", "A1", "A2", "A3", "A3a", "A3b", "A3c", "A4", "A3b1", "A3b2", "A3b3", "A3b1a", "A3b1b"):
            break

        with contextlib.ExitStack() as pa:
            def sba(name, shape, dt=F32):
                t = pa.enter_context(nc.sbuf_tensor("%s_x%d" % (name, l), list(shape), dt))
                return t, Buf(name)
            WA, WAB = sba("WA", [128, 16384], BF16)
            for i in range(2):
                stage[i] = sba("stgX%d" % i, [128, 2048])
            wq = WA[:, 0:8 * 512].rearrange("p (c n) -> p c n", c=8)
            wkv = WA[:, 4096:4096 + 8 * 1024].rearrange("p (c n) -> p c n", c=8)
            wo = WA[:, 12288:12288 + 4 * 1024].rearrange("p (c n) -> p c n", c=4)
            load_gcol(norm_cross_d[l])
            for c in range(8):
                cast_rows(wq[:, c, :], WAB, wq_d[l, c * 128:(c + 1) * 128, :], 512, gcol[:, c:c + 1])
            load_gcol(norm_mem_d)
            for c in range(8):
                cast_rows(wkv[:, c, :], WAB, wkv_d[l, c * 128:(c + 1) * 128, :], 1024, gcol[:, c:c + 1])
            for c in range(4):
                cast_rows(wo[:, c, :], WAB, wo_d[l, c * 128:(c + 1) * 128, :], D)
            gxq, gxqB = sba("gxq", [128, 128])
            gxk, gxkB = sba("gxk", [128, 128])
            k.dma("sp", gxq[:, :], xq_norm_d[l].partition_broadcast(128), gxqB, W=[gxqB])
            k.dma("sp", gxk[:, :], xk_norm_d[l].partition_broadcast(128), gxkB, W=[gxkB])
            k.op("dve", V.tensor_scalar, R=[gxqB], W=[gxqB], out=gxq[:, :], in0=gxq[:, :], scalar1=128 ** -0.5,
                 scalar2=None, op0=ALU.mult)
            kmT, kmTB = sba("kmT", [128, 4, MEM], BF16)
            vm1, vm1B = sba("vm1", [128, 2, 4, 129], BF16)
            k.op("pool", POOL.memset, W=[vm1B], ap=vm1[:, :, :, :], constant=1.0)
            scr = [sba("xscr%d" % i, [128, 512]) for i in range(4)]
            scb = [sba("xscb%d" % i, [128, 1024], BF16) for i in range(4)]
            sm = [sba("xsm%d" % i, [128, 16]) for i in range(4)]
            cx = [0, 0, 0]

            def scratch():
                cx[0] += 1
                return scr[cx[0] % 4]

            def scratchb():
                cx[1] += 1
                return scb[cx[1] % 4]

            def small():
                cx[2] += 1
                return sm[cx[2] % 4]

            def v3(ap, a=4):
                return ap.rearrange("p (a b) -> p a b", a=a)

            def head_rms(pt, pB, gtile, gB, dst3, dstB):
                sq, sqB = scratch()
                k.op("act", ACT.activation, R=[pB], W=[sqB], out=sq[:, :], in_=pt[:, :], func=AF.Square)
                ss, ssB = small()
                k.op("dve", V.tensor_reduce, R=[sqB], W=[ssB], out=ss[:, 0:4], in_=v3(sq[:, :]), axis=AX.X, op=ALU.add)
                k.op("dve", V.tensor_scalar, R=[ssB], W=[ssB], out=ss[:, 4:8], in0=ss[:, 0:4], scalar1=1.0 / 128,
                     scalar2=EPS, op0=ALU.mult, op1=ALU.add)
                k.op("act", ACT.activation, R=[ssB], W=[ssB], out=ss[:, 4:8], in_=ss[:, 4:8], func=AF.Sqrt)
                k.op("dve", V.reciprocal, R=[ssB], W=[ssB], out=ss[:, 8:12], in_=ss[:, 4:8])
                k.op("dve", V.tensor_tensor, R=[pB, ssB], W=[sqB], out=v3(sq[:, :]), in0=v3(pt[:, :]),
                     in1=bc(ss[:, 8:12].unsqueeze(2), [128, 4, 128]), op=ALU.mult)
                k.op("dve", V.tensor_tensor, R=[sqB, gB], W=[dstB], out=dst3, in0=v3(sq[:, :]),
                     in1=bc(gtile[:, :].unsqueeze(1), [128, 4, 128]), op=ALU.mult)

            for mt in range(2):
                xt, xtB = xts[mt]
                k.dma("sp", xt[:, :], mem_d[mt * 128:(mt + 1) * 128, :], xtB, W=[xtB])
                norm_T(xt, xtB, 0)
                pk_, pkB_ = psum()
                pv_, pvB_ = psum()
                for c in range(8):
                    k.op("pe", PE.matmul, R=[hTB, WAB], W=[pkB_], out=pk_[:, :], lhsT=hT[:, c, 0:128],
                         rhs=wkv[:, c, 0:512], start=(c == 0), stop=(c == 7))
                for c in range(8):
                    k.op("pe", PE.matmul, R=[hTB, WAB], W=[pvB_], out=pv_[:, :], lhsT=hT[:, c, 0:128],
                         rhs=wkv[:, c, 512:1024], start=(c == 0), stop=(c == 7))
                kn, knB = scratchb()
                head_rms(pk_, pkB_, gxk, gxkB, v3(kn[:, 0:512]), knB)
                ptk, ptkB = psum()
                for h in range(4):
                    k.op("pe", PE.transpose, R=[knB, identbB], W=[ptkB], out=bfv(ptk)[:, h * 128:(h + 1) * 128],
                         in_=kn[:, h * 128:(h + 1) * 128], identity=ident_b[:, :])
                k.op("act", ACT.copy, R=[ptkB], W=[kmTB], out=kmT[:, :, mt * 128:(mt + 1) * 128],
                     in_=v3(bfv(ptk)[:, 0:512]))
                k.op("act", ACT.copy, R=[pvB_], W=[vm1B], out=vm1[:, mt, :, 0:128], in_=v3(pv_[:, :]))

            for t in range(NT):
                xt, xtB = xts[t % 2]
                k.dma("sp", xt[:, :], out_d[t * 128:(t + 1) * 128, :], xtB, R=[xdB[t]], W=[xtB])
                norm_T(xt, xtB, 0)
                pq, pqB = psum()
                for c in range(8):
                    k.op("pe", PE.matmul, R=[hTB, WAB], W=[pqB], out=pq[:, :], lhsT=hT[:, c, 0:128], rhs=wq[:, c, :],
                         start=(c == 0), stop=(c == 7))
                qn, qnB = scratchb()
                head_rms(pq, pqB, gxq, gxqB, v3(qn[:, 0:512]), qnB)
                ptq, ptqB = psum()
                for h in range(4):
                    k.op("pe", PE.transpose, R=[qnB, identbB], W=[ptqB], out=bfv(ptq)[:, h * 128:(h + 1) * 128],
                         in_=qn[:, h * 128:(h + 1) * 128], identity=ident_b[:, :])
                qT, qTB = scratchb()
                k.op("act", ACT.copy, R=[ptqB], W=[qTB], out=qT[:, 0:512], in_=bfv(ptq)[:, 0:512])
                pT_, pTB = scratchb()
                for mt in range(2):
                    pl, plB = psum()
                    for h in range(4):
                        k.op("pe", PE.matmul, R=[kmTB, qTB], W=[plB], out=pl[:, h * 128:(h + 1) * 128],
                             lhsT=kmT[:, h, mt * 128:(mt + 1) * 128], rhs=qT[:, h * 128:(h + 1) * 128], start=True,
                             stop=True)
                    k.op("act", ACT.activation, R=[plB], W=[pTB], out=pT_[:, mt * 512:(mt + 1) * 512], in_=pl[:, :],
                         func=AF.Exp)
                on, onB = scratchb()
                for hh in range(2):
                    po, poB = psum()
                    for h2 in range(2):
                        h = hh * 2 + h2
                        for mt in range(2):
                            k.op("pe", PE.matmul, R=[pTB, vm1B], W=[poB], out=po[:, h2 * 129:(h2 + 1) * 129],
                                 lhsT=pT_[:, mt * 512 + h * 128:mt * 512 + (h + 1) * 128], rhs=vm1[:, mt, h, :],
                                 start=(mt == 0), stop=(mt == 1))
                    po3 = po[:, 0:258].rearrange("p (a b) -> p a b", a=2)
                    rd, rdB = small()
                    k.op("dve", V.reciprocal, R=[poB], W=[rdB], out=rd[:, 0:2], in_=po3[:, :, 128])
                    k.op("dve", V.tensor_tensor, R=[poB, rdB], W=[onB],
                         out=on[:, hh * 256:(hh + 1) * 256].rearrange("p (a b) -> p a b", a=2), in0=po3[:, :, 0:128],
                         in1=bc(rd[:, 0:2].unsqueeze(2), [128, 2, 128]), op=ALU.mult)
                pto, ptoB = psum()
                for h in range(4):
                    k.op("pe", PE.transpose, R=[onB, identbB], W=[ptoB], out=bfv(pto)[:, h * 128:(h + 1) * 128],
                         in_=on[:, h * 128:(h + 1) * 128], identity=ident_b[:, :])
                oT, oTB = scratchb()
                k.op("act", ACT.copy, R=[ptoB], W=[oTB], out=oT[:, 0:512], in_=bfv(pto)[:, 0:512])
                for n in range(2):
                    pt, pB = psum()
                    for h in range(4):
                        k.op("pe", PE.matmul, R=[oTB, WAB], W=[pB], out=pt[:, :], lhsT=oT[:, h * 128:(h + 1) * 128],
                             rhs=wo[:, h, n * 512:(n + 1) * 512], start=(h == 0), stop=(h == 3))
                    k.op("dve", V.tensor_tensor, R=[pB, xtB], W=[xtB], out=xt[:, n * 512:(n + 1) * 512], in0=pt[:, :],
                         in1=xt[:, n * 512:(n + 1) * 512], op=ALU.add)
                k.dma("sp", out_d[t * 128:(t + 1) * 128, :], xt[:, :], xtB, R=[xtB], W=[xdB[t]])
        k.barrier()
        if stop == "A2":
            break

        with contextlib.ExitStack() as pa:
            def sba(name, shape, dt=F32):
                t = pa.enter_context(nc.sbuf_tensor("%s_m%d" % (name, l), list(shape), dt))
                return t, Buf(name)
            ST = ST_B
            NST = S // ST
            WA, WAB = sba("WA", [128, 65536], BF16)
            for i in range(2):
                stage[i] = sba("stgM%d" % i, [128, 2048])
            w1 = WA[:, 0:8 * 4096].rearrange("p (c n) -> p c n", c=8)
            w2 = WA[:, 32768:32768 + 32 * 1024].rearrange("p (c n) -> p c n", c=32)
            load_gcol(norm_mlp_d[l])
            for c in range(8):
                cast_rows(w1[:, c, :], WAB, w1_d[l, c * 128:(c + 1) * 128, :], 4096, gcol[:, c:c + 1])
            for c in range(32):
                cast_rows(w2[:, c, :], WAB, w2_d[l, c * 128:(c + 1) * 128, :], D)
            aT, aTB = sba("aT", [128, 32, ST], BF16)
            rrs = [sba("rr%d" % i, [128, ST]) for i in range(3)]
            for s_i in range(NST):
                t0 = s_i * ST
                for j in range(2):
                    xt, xtB = xts[j]
                    k.dma("sp", xt[:, :], out_d[t0 + j * 128:t0 + (j + 1) * 128, :], xtB, R=[xdB[2 * s_i + j]],
                          W=[xtB])
                    norm_T(xt, xtB, j * 128)
                for f in range(32):
                    pt, pB = psum()
                    for c in range(8):
                        k.op("pe", PE.matmul, R=[WAB, hTB], W=[pB], out=pt[:, :ST], lhsT=w1[:, c, f * 128:(f + 1) * 128],
                             rhs=hT[:, c, :], start=(c == 0), stop=(c == 7))
                    rr_, rrB = rrs[f % 3]
                    k.op("act", ACT.activation, R=[pB], W=[rrB], out=rr_[:, :], in_=pt[:, :ST], func=AF.Relu)
                    e = "pool" if f % 2 == 0 else "dve"
                    k.op(e, k.eng[e].tensor_tensor, R=[rrB], W=[aTB], out=aT[:, f, :], in0=rr_[:, :], in1=rr_[:, :],
                         op=ALU.mult)
                for j in range(2):
                    xt, xtB = xts[j]
                    t = 2 * s_i + j
                    for n in range(2):
                        pt, pB = psum()
                        for f in range(32):
                            k.op("pe", PE.matmul, R=[aTB, WAB], W=[pB], out=pt[:, :],
                                 lhsT=aT[:, f, j * 128:(j + 1) * 128], rhs=w2[:, f, n * 512:(n + 1) * 512],
                                 start=(f == 0), stop=(f == 31))
                        k.op("dve", V.tensor_tensor, R=[pB, xtB], W=[xtB], out=xt[:, n * 512:(n + 1) * 512],
                             in0=pt[:, :], in1=xt[:, n * 512:(n + 1) * 512], op=ALU.add)
                    k.dma("sp", out_d[t * 128:(t + 1) * 128, :], xt[:, :], xtB, R=[xtB], W=[xdB[t]])
        k.barrier()
    k.finish()
    es.close()
    return nc, k


_INPUT_NAMES = ["x", "mem", "norm_mix", "w_in", "gdn_conv", "gdn_a_log", "gdn_dt_bias", "gdn_norm", "dsa_w_qb",
                "dsa_w_qi", "dsa_w_kvb", "dsa_q_norm", "dsa_k_norm", "conv_dw", "conv_dw_b", "conv_ln_g",
                "conv_ln_b", "w_out", "norm_mem", "norm_cross", "xa_wq", "xa_wkv", "xa_q_norm", "xa_k_norm",
                "xa_wo", "norm_mlp", "mlp_w1", "mlp_w2"]


def run(inputs, S=4096, NL=2, stop=None, ncores=8, trace=False):
    nc, kk = build(S=S, NL=NL, stop=stop)
    shared = {n: np.ascontiguousarray(np.asarray(inputs[n], dtype=np.float32)) for n in _INPUT_NAMES
              if n not in ("x", "mem")}
    x = np.asarray(inputs["x"], dtype=np.float32)
    mem = np.asarray(inputs["mem"], dtype=np.float32)
    in_maps = []
    for b in range(ncores):
        m = dict(shared)
        m["x"] = np.ascontiguousarray(x[b, :S])
        m["mem"] = np.ascontiguousarray(mem[b])
        in_maps.append(m)
    res = run_bass_kernel_spmd(nc, in_maps, core_ids=list(range(ncores)), trace=trace)
    out = np.stack([np.asarray(r["out"]) for r in res.results], axis=0)
    return out, res


def kernel(**inputs):
    out, _ = run(inputs)
    return out.astype(np.float32)
```

```python
import contextlib
import numpy as np
import concourse.bass as bass
import concourse.mybir as mybir
from concourse.bass_utils import run_bass_kernel_spmd

F32 = mybir.dt.float32
BF16 = mybir.dt.bfloat16
AF = mybir.ActivationFunctionType
ALU = mybir.AluOpType
AX = mybir.AxisListType

D = 1024
NIN = 3020
MEM = 256
EPS = 1e-6
C_A, C_B, C_CQ, C_CKV, C_KI, C_WI, C_UG = 2048, 2052, 2056, 2312, 2440, 2504, 2508
NEG_NC = -2.0e38
NEG_SEL = -3.0e38
ST_A = 128
ST_B = 256


class Buf:
    __slots__ = ("w", "r", "dsem", "dcnt", "name")

    def __init__(self, name=""):
        self.w = {}
        self.r = {}
        self.dsem = None
        self.dcnt = 0
        self.name = name


class K:
    def __init__(self, nc, es):
        self.nc = nc
        self.es = es
        self.eng = {"pe": nc.tensor, "act": nc.scalar, "dve": nc.vector, "pool": nc.gpsimd, "sp": nc.sync}
        self.sem = {e: es.enter_context(nc.semaphore("s_" + e)) for e in ("pe", "act", "dve", "pool")}
        self.cnt = {e: 0 for e in self.sem}
        self.waited = {e: {} for e in self.eng}
        self.nsem = 0
        self.dbufs = []
        self.ninst = 0

    def _wait(self, e, deps):
        need = {}
        for key, (sem, val) in deps:
            if key == "pe" and e == "pe":
                continue
            if need.get(key, (None, 0))[1] < val:
                need[key] = (sem, val)
        wd = self.waited[e]
        for key, (sem, val) in need.items():
            if wd.get(key, 0) >= val:
                continue
            self.eng[e].wait_ge(sem, val)
            wd[key] = val
            self.ninst += 1

    @staticmethod
    def _deps(reads, writes):
        deps = []
        for b in reads:
            deps.extend(b.w.items())
        for b in writes:
            deps.extend(b.w.items())
            deps.extend(b.r.items())
        return deps

    @staticmethod
    def _mark(tokkey, tok, reads, writes):
        for b in reads:
            b.r[tokkey] = tok
        for b in writes:
            if b.r:
                b.w = {}
                b.r = {}
            b.w[tokkey] = tok

    def op(self, e, fn, R=(), W=(), **kw):
        self._wait(e, self._deps(R, W))
        ins = fn(**kw)
        self.cnt[e] += 1
        ins.then_inc(self.sem[e], 1)
        self.ninst += 1
        self._mark(e, (self.sem[e], self.cnt[e]), R, W)

    def dma(self, q, out, in_, sb, R=(), W=(), group=False, **kw):
        if sb.dsem is None:
            sb.dsem = self.es.enter_context(self.nc.semaphore("d%d" % self.nsem))
            self.nsem += 1
            self.dbufs.append(sb)
        key = "d%d" % id(sb)
        deps = self._deps(R, W)
        if group:
            deps = [d for d in deps if d[0] != key]
        self._wait(q, deps)
        ins = self.eng[q].dma_start(out=out, in_=in_, **kw)
        sb.dcnt += 16
        ins.then_inc(sb.dsem, 16)
        self.ninst += 1
        self._mark(key, (sb.dsem, sb.dcnt), R, W)

    def barrier(self):
        deps = [("d%d" % id(b), (b.dsem, b.dcnt)) for b in self.dbufs]
        for e in self.sem:
            deps.append((e, (self.sem[e], self.cnt[e])))
        for e in self.eng:
            self._wait(e, [d for d in deps if d[0] != e])

    def finish(self):
        deps = [("d%d" % id(b), (b.dsem, b.dcnt)) for b in self.dbufs]
        for e in self.sem:
            deps.append((e, (self.sem[e], self.cnt[e])))
        self._wait("sp", deps)


def bc(ap, shape):
    return ap.to_broadcast(list(shape))


def build(S=4096, NL=2, stop=None):
    NT = S // 128
    nc = bass.Bass("TRN2", target_bir_lowering=False)
    es = contextlib.ExitStack()

    def din(name, shape):
        return nc.dram_tensor(name, list(shape), F32, kind="ExternalInput").ap()

    x_d = din("x", [S, D])
    mem_d = din("mem", [MEM, D])
    norm_mix_d = din("norm_mix", [2, D])
    w_in_d = din("w_in", [2, D, NIN])
    gdn_conv_d = din("gdn_conv", [2, 4, 1536])
    a_log_d = din("gdn_a_log", [2, 4])
    dt_bias_d = din("gdn_dt_bias", [2, 4])
    gdn_norm_d = din("gdn_norm", [2, 128])
    w_qb_d = din("dsa_w_qb", [2, 256, 256])
    w_qi_d = din("dsa_w_qi", [2, 256, 256])
    w_kvb_d = din("dsa_w_kvb", [2, 128, 128])
    dq_norm_d = din("dsa_q_norm", [2, 64])
    dk_norm_d = din("dsa_k_norm", [2, 64])
    conv_dw_d = din("conv_dw", [2, 31, 256])
    conv_b_d = din("conv_dw_b", [2, 256])
    ln_g_d = din("conv_ln_g", [2, 256])
    ln_b_d = din("conv_ln_b", [2, 256])
    w_out_d = din("w_out", [2, D, D])
    norm_mem_d = din("norm_mem", [D])
    norm_cross_d = din("norm_cross", [2, D])
    wq_d = din("xa_wq", [2, D, 512])
    wkv_d = din("xa_wkv", [2, D, 1024])
    xq_norm_d = din("xa_q_norm", [2, 128])
    xk_norm_d = din("xa_k_norm", [2, 128])
    wo_d = din("xa_wo", [2, 512, D])
    norm_mlp_d = din("norm_mlp", [2, D])
    w1_d = din("mlp_w1", [2, D, 4096])
    w2_d = din("mlp_w2", [2, 4096, D])
    out_d = nc.dram_tensor("out", [S, D], F32, kind="ExternalOutput").ap()

    k = K(nc, es)
    V, ACT, PE, POOL = nc.vector, nc.scalar, nc.tensor, nc.gpsimd

    def sb(name, shape, dt=F32):
        t = es.enter_context(nc.sbuf_tensor(name, list(shape), dt))
        return t, Buf(name)

    psb = []
    for i in range(8):
        t = es.enter_context(nc.psum_tensor("ps%d" % i, [128, 512], F32))
        psb.append((t, Buf("ps%d" % i)))
    pctr = [0]

    def psum():
        t, b = psb[pctr[0] % 7]
        pctr[0] += 1
        return t, b

    def bfv(t):
        return t[:, :].bitcast(BF16)

    ones_f, onesB = sb("ones_f", [128, 512])
    ident_f, identB = sb("ident_f", [128, 128])
    ident_b, identbB = sb("ident_b", [128, 128], BF16)
    ones128, o128B = sb("ones128", [128, 128])
    ones256, o256B = sb("ones256", [128, 128])
    onesS, onesSB = sb("onesS", [128, 128])
    sel, selB = sb("sel", [4, 4, 128])
    epsc, epsB = sb("epsc", [128, 1])
    onec, oneB = sb("onec", [128, 1])
    FILL0 = POOL.to_reg(0.0)
    FILLM = POOL.to_reg(-1.0e30)
    FILLNC = POOL.to_reg(NEG_NC)
    k.op("pool", POOL.memset, W=[onesB], ap=ones_f[:, :], constant=1.0)
    k.op("pool", POOL.memset, W=[o128B], ap=ones128[:, :], constant=1.0 / 128)
    k.op("pool", POOL.memset, W=[o256B], ap=ones256[:, :], constant=1.0 / 256)
    k.op("pool", POOL.memset, W=[onesSB], ap=onesS[:, :], constant=1.0)
    k.op("pool", POOL.memset, W=[epsB], ap=epsc[:, :], constant=EPS)
    k.op("pool", POOL.memset, W=[oneB], ap=onec[:, :], constant=1.0)
    k.op("pool", POOL.affine_select, R=[onesB], W=[identB], out=ident_f[:, :], in_=ones_f[:, 0:128],
         pattern=[[-1, 128]], compare_op=ALU.is_equal, fill=FILL0, base=0, channel_multiplier=1)
    k.op("pool", POOL.affine_select, R=[onesB], W=[identbB], out=ident_b[:, :], in_=ones_f[:, 0:128],
         pattern=[[-1, 128]], compare_op=ALU.is_equal, fill=FILL0, base=0, channel_multiplier=1)
    k.op("pool", POOL.affine_select, R=[onesB], W=[selB], out=sel[:, :, :],
         in_=ones_f[0:4, :].rearrange("p (a b) -> p a b", a=4),
         pattern=[[-1, 4], [0, 128]], compare_op=ALU.is_equal, fill=FILL0, base=0, channel_multiplier=1)

    NXT = 2
    xts = [sb("xt%d" % i, [128, D]) for i in range(NXT)]
    hb, hbB = sb("hb", [128, D], BF16)
    hT, hTB = sb("hT", [128, 8, ST_B], BF16)
    st5 = [sb("st5_%d" % i, [128, 8]) for i in range(4)]
    stage = [None, None]
    sctr = [0]
    gcol, gcolB = sb("gcol", [128, 8])

    def load_gcol(vec_ap):
        k.dma("sp", gcol[:, :], vec_ap.rearrange("(c p) -> p c", p=128), gcolB, W=[gcolB],
              allow_slow_non_contiguous=True)

    cast_rr = [0]

    def cast_rows(dst_ap, dstB, src_rows_ap, ncols, scol=None):
        for c0 in range(0, ncols, 2048):
            cw = min(2048, ncols - c0)
            stg, stgB = stage[sctr[0] % 2]
            sctr[0] += 1
            k.dma("sp", stg[:, :cw], src_rows_ap[:, c0:c0 + cw], stgB, W=[stgB])
            e = ("pool", "act")[cast_rr[0] % 2]
            cast_rr[0] += 1
            R = [stgB] + ([gcolB] if scol is not None else [])
            if e == "pool":
                if scol is None:
                    k.op("pool", POOL.tensor_copy, R=R, W=[dstB], out=dst_ap[:, c0:c0 + cw], in_=stg[:, :cw])
                else:
                    k.op("pool", POOL.tensor_scalar, R=R, W=[dstB], out=dst_ap[:, c0:c0 + cw], in0=stg[:, :cw],
                         scalar1=scol, scalar2=1.0, op0=ALU.mult, op1=ALU.mult)
            else:
                if scol is None:
                    k.op("act", ACT.copy, R=R, W=[dstB], out=dst_ap[:, c0:c0 + cw], in_=stg[:, :cw])
                else:
                    k.op("act", ACT.activation, R=R, W=[dstB], out=dst_ap[:, c0:c0 + cw], in_=stg[:, :cw],
                         func=AF.Copy, scale=scol)

    def norm_T(xt, xtB, ncol0):
        s5, s5B = st5[ncol0 // 128 % 4]
        k.op("act", ACT.activation, R=[xtB], W=[hbB, s5B], out=hb[:, :], in_=xt[:, :], func=AF.Square,
             accum_out=s5[:, 0:1])
        k.op("dve", V.tensor_scalar, R=[s5B], W=[s5B], out=s5[:, 1:2], in0=s5[:, 0:1], scalar1=1.0 / D,
             scalar2=EPS, op0=ALU.mult, op1=ALU.add)
        k.op("act", ACT.activation, R=[s5B], W=[s5B], out=s5[:, 2:3], in_=s5[:, 1:2], func=AF.Sqrt)
        k.op("dve", V.reciprocal, R=[s5B], W=[s5B], out=s5[:, 3:4], in_=s5[:, 2:3])
        k.op("act", ACT.activation, R=[xtB, s5B], W=[hbB], out=hb[:, :], in_=xt[:, :], func=AF.Copy,
             scale=s5[:, 3:4])
        pt, pB = psum()
        for c in range(8):
            k.op("pe", PE.transpose, R=[hbB, identbB], W=[pB], out=bfv(pt)[:, c * 128:(c + 1) * 128],
                 in_=hb[:, c * 128:(c + 1) * 128], identity=ident_b[:, :])
        k.op("dve", V.tensor_copy, R=[pB], W=[hTB], out=hT[:, :, ncol0:ncol0 + 128],
             in_=bfv(pt).rearrange("p (c t) -> p c t", c=8))

    xdB = [Buf("xd%d" % t) for t in range(NT)]

    for l in range(NL):
        xsrc = x_d if l == 0 else out_d
        with contextlib.ExitStack() as pa:
            def sba(name, shape, dt=F32):
                t = pa.enter_context(nc.sbuf_tensor("%s_l%d" % (name, l), list(shape), dt))
                return t, Buf(name)

            ST = ST_A
            NTS = ST // 128
            NST = S // ST
            WA, WAB = sba("WA", [128, 33536], BF16)
            prep = contextlib.ExitStack()
            for i in range(2):
                stage[i] = (prep.enter_context(nc.sbuf_tensor("stgA%d_%d" % (l, i), [128, 2048], F32)), Buf("stg"))
            w_in = WA[:, 0:8 * NIN].rearrange("p (c n) -> p c n", c=8)
            o1 = 8 * NIN
            w_out = WA[:, o1:o1 + 8 * D].rearrange("p (c n) -> p c n", c=8)
            o2 = o1 + 8 * D
            w_qb = WA[:, o2:o2 + 512].rearrange("p (c n) -> p c n", c=2)
            w_qi = WA[:, o2 + 512:o2 + 1024].rearrange("p (c n) -> p c n", c=2)
            w_kvb = WA[:, o2 + 1024:o2 + 1152]
            load_gcol(norm_mix_d[l])
            for c in range(8):
                cast_rows(w_in[:, c, :], WAB, w_in_d[l, c * 128:(c + 1) * 128, :], NIN, gcol[:, c:c + 1])
            for c in range(8):
                cast_rows(w_out[:, c, :], WAB, w_out_d[l, c * 128:(c + 1) * 128, :], D)
            for c in range(2):
                cast_rows(w_qb[:, c, :], WAB, w_qb_d[l, c * 128:(c + 1) * 128, :], 256)
                cast_rows(w_qi[:, c, :], WAB, w_qi_d[l, c * 128:(c + 1) * 128, :], 256)
            cast_rows(w_kvb, WAB, w_kvb_d[l, :, :], 128)
            k.barrier()
            prep.close()
            cw, cwB = sba("cw", [128, 12, 4])
            for c in range(12):
                k.dma("sp", cw[:, c, :], gdn_conv_d[l][:, c * 128:(c + 1) * 128].rearrange("j p -> p j"), cwB,
                      W=[cwB], group=True, allow_slow_non_contiguous=True)
            cdw, cdwB = sba("cdw", [128, 2, 31])
            for c in range(2):
                k.dma("sp", cdw[:, c, :], conv_dw_d[l][:, c * 128:(c + 1) * 128].rearrange("j p -> p j"), cdwB,
                      W=[cdwB], group=True, allow_slow_non_contiguous=True)
            cvec, cvecB = sba("cvec", [128, 3, 2])
            for i, v in enumerate((conv_b_d, ln_g_d, ln_b_d)):
                k.dma("sp", cvec[:, i, :], v[l].rearrange("(c p) -> p c", p=128), cvecB, W=[cvecB], group=True,
                      allow_slow_non_contiguous=True)
            gnc, gncB = sba("gnc", [128, 1])
            k.dma("sp", gnc[:, :], gdn_norm_d[l].rearrange("(p o) -> p o", o=1), gncB, W=[gncB])
            gv, gvB = sba("gv", [4, 4])
            k.dma("sp", gv[:, 0:1], a_log_d[l].rearrange("(p o) -> p o", o=1), gvB, W=[gvB])
            k.dma("sp", gv[:, 1:2], dt_bias_d[l].rearrange("(p o) -> p o", o=1), gvB, W=[gvB], group=True)
            k.op("act", ACT.activation, R=[gvB], W=[gvB], out=gv[:, 2:3], in_=gv[:, 0:1], func=AF.Exp)
            k.op("dve", V.tensor_scalar, R=[gvB], W=[gvB], out=gv[:, 3:4], in0=gv[:, 2:3], scalar1=-1.0,
                 scalar2=None, op0=ALU.mult)
            gq8, gq8B = sba("gq8", [128, 64])
            gkb, gkbB = sba("gkb", [128, 64])
            k.dma("sp", gq8[:, :], dq_norm_d[l].partition_broadcast(128), gq8B, W=[gq8B])
            k.dma("sp", gkb[:, :], dk_norm_d[l].partition_broadcast(128), gkbB, W=[gkbB])
            k.op("dve", V.tensor_scalar, R=[gq8B], W=[gq8B], out=gq8[:, :], in0=gq8[:, :], scalar1=0.125,
                 scalar2=None, op0=ALU.mult)
            dgc, dgcB = sba("dgc", [128, 2, 31, 128], BF16)
            for i in range(2):
                for j in range(31):
                    k.op("pool", POOL.tensor_scalar, R=[identB, cdwB], W=[dgcB], out=dgc[:, i, j, :],
                         in0=ident_f[:, :], scalar1=cdw[:, i, j:j + 1], scalar2=1.0, op0=ALU.mult, op1=ALU.mult)
            pre, preB = sba("pre", [128, 12, 3 + ST])
            gl, glB = sba("gl", [128, 2, 30 + ST], BF16)
            k.op("pool", POOL.memset, W=[preB], ap=pre[:, :, :], constant=0.0)
            k.op("pool", POOL.memset, W=[glB], ap=gl[:, :, :], constant=0.0)
            qkvT, _ = sba("qkvT", [128, 12, ST])
            qkvB = [Buf("qkv%d" % i) for i in range(12)]
            zs, zsB = sba("zs", [128, 4, ST])
            cqT, cqB = sba("cqT", [128, 2, ST], BF16)
            ckvT, ckvB = sba("ckvT", [128, ST], BF16)
            yT, yTB = sba("yT", [128, 8, ST], BF16)
            Sst = [sba("S%d" % i, [128, 4, 128]) for i in range(2)]
            k.op("pool", POOL.memset, W=[Sst[0][1]], ap=Sst[0][0][:, :, :], constant=0.0)
            KKT, _ = sba("KKT", [128, S], BF16)
            kkB = [Buf("kk%d" % t) for t in range(NT)]
            Vc, _ = sba("Vc", [128, NT, 65], BF16)
            vcB = [Buf("vc%d" % t) for t in range(NT)]
            VcI, VcIB = sba("VcI", [128, 1], BF16)
            work, workB = sba("work", [128, S])
            rows, rowsB = sba("rows", [4, 8, ST])
            cols = [sba("cols%d" % i, [128, 16]) for i in range(2)]
            kkt, kktB = sba("kkt", [128, 2, 128], BF16)
            wab, wabB = sba("wab", [128, 2, 8])
            gd = {n: sba("gd_" + n, [128, 512]) for n in
                  ("Kbd", "Kdec", "Vb", "QKT", "qd", "P0", "P1", "PT0", "PT1", "RT0", "RT1")}
            QQs = [sba("QQ%d" % i, [128, 512], BF16) for i in range(2)]
            mks = [sba("mk%d" % i, [128, 512], BF16) for i in range(2)]
            mkc = [0]
            print("sbuf remaining (phase A)", nc.sbuf_bytes_remaining)
            NSC = 8
            scr = [sba("scr%d" % i, [128, 512]) for i in range(NSC)]
            scrc = [0]

            def scratch():
                t, b = scr[scrc[0] % NSC]
                scrc[0] += 1
                return t, b
            NSB = 6
            scb = [sba("scb%d" % i, [128, 512], BF16) for i in range(NSB)]
            scbc = [0]

            def scratchb():
                t, b = scb[scbc[0] % NSB]
                scbc[0] += 1
                return t, b
            m8s = [sba("m8_%d" % i, [128, 8]) for i in range(2)]
            KB = 26
            pw2, pw2B = sba("pw2", [128, KB])
            for kk_ in range(KB):
                k.op("pool", POOL.memset, W=[pw2B], ap=pw2[:, kk_:kk_ + 1], constant=-(2.0 ** -(kk_ + 1)))
            bst, bstB = sba("bst", [128, 16])
            nst, nstB = sba("nst", [128, KB])
            junk8 = xts[1][0][:, :].bitcast(mybir.dt.int8)
            junkB = xts[1][1]
            sm = [sba("sm%d" % i, [128, 16]) for i in range(4)]
            smc = [0]

            def small():
                t, b = sm[smc[0] % 4]
                smc[0] += 1
                return t, b

            for t in range(NT):
                k.op("pool", POOL.memset, W=[vcB[t]], ap=Vc[:, t, 64:65], constant=1.0)

            def v3(ap, a=4):
                return ap.rearrange("p (a b) -> p a b", a=a)

            for s_i in range(NST):
                if stop == "W":
                    break
                t0 = s_i * ST
                tiles = [s_i * NTS + j for j in range(NTS)]
                for j in range(NTS):
                    xt, xtB = xts[j]
                    k.dma("sp", xt[:, :], xsrc[t0 + j * 128:t0 + (j + 1) * 128, :], xtB, R=[xdB[tiles[j]]],
                          W=[xtB])
                    norm_T(xt, xtB, j * 128)

                def featmajor(col0, M):
                    pt, pB = psum()
                    for c in range(8):
                        k.op("pe", PE.matmul, R=[WAB, hTB], W=[pB], out=pt[:M, :ST],
                             lhsT=w_in[:, c, col0:col0 + M], rhs=hT[:, c, 0:ST], start=(c == 0), stop=(c == 7))
                    return pt, pB

                for i in range(12):
                    pt, pB = featmajor(i * 128, 128)
                    k.op("act", ACT.copy, R=[pB], W=[preB], out=pre[:, i, 3:3 + ST], in_=pt[:, :ST])
                    k.op("dve", V.tensor_scalar, R=[preB, cwB], W=[qkvB[i]], out=qkvT[:, i, :], in0=pre[:, i, 0:ST],
                         scalar1=cw[:, i, 0:1], scalar2=None, op0=ALU.mult)
                    for j in range(1, 4):
                        k.op("dve", V.scalar_tensor_tensor, R=[preB, cwB, qkvB[i]], W=[qkvB[i]], out=qkvT[:, i, :],
                             in0=pre[:, i, j:j + ST], scalar=cw[:, i, j:j + 1], in1=qkvT[:, i, :], op0=ALU.mult,
                             op1=ALU.add)
                    k.op("act", ACT.activation, R=[qkvB[i]], W=[qkvB[i]], out=qkvT[:, i, :], in_=qkvT[:, i, :],
                         func=AF.Silu)
                k.op("dve", V.tensor_copy, R=[preB], W=[preB], out=pre[:, :, 0:3], in_=pre[:, :, ST:ST + 3])
                for i in range(8):
                    sq, sqB = scratch()
                    k.op("act", ACT.activation, R=[qkvB[i]], W=[sqB], out=sq[:, :ST], in_=qkvT[:, i, :],
                         func=AF.Square)
                    pt, pB = psum()
                    k.op("pe", PE.matmul, R=[onesSB, sqB], W=[pB], out=pt[:, :ST], lhsT=onesS[:, :],
                         rhs=sq[:, :ST], start=True, stop=True)
                    k.op("act", ACT.activation, R=[pB, epsB], W=[sqB], out=sq[:, :ST], in_=pt[:, :ST],
                         func=AF.Sqrt, bias=epsc[:, 0:1])
                    k.op("dve", V.reciprocal, R=[sqB], W=[sqB], out=sq[:, :ST], in_=sq[:, :ST])
                    k.op("dve", V.scalar_tensor_tensor, R=[qkvB[i], sqB], W=[qkvB[i]], out=qkvT[:, i, :],
                         in0=qkvT[:, i, :], scalar=(128 ** -0.5 if i < 4 else 1.0), in1=sq[:, :ST],
                         op0=ALU.mult, op1=ALU.mult)
                for i in range(4):
                    pt, pB = featmajor(1536 + i * 128, 128)
                    k.op("act", ACT.activation, R=[pB], W=[zsB], out=zs[:, i, :], in_=pt[:, :ST], func=AF.Silu)
                for i in range(2):
                    pt, pB = featmajor(C_CQ + i * 128, 128)
                    k.op("act", ACT.copy, R=[pB], W=[cqB], out=cqT[:, i, :], in_=pt[:, :ST])
                pt, pB = featmajor(C_CKV, 128)
                k.op("act", ACT.copy, R=[pB], W=[ckvB], out=ckvT[:, :], in_=pt[:, :ST])
                for i in range(2):
                    pu, puB = featmajor(C_UG + i * 128, 128)
                    pg, pgB = featmajor(C_UG + 256 + i * 128, 128)
                    sg, sgB = scratch()
                    k.op("act", ACT.activation, R=[pgB], W=[sgB], out=sg[:, :ST], in_=pg[:, :ST], func=AF.Sigmoid)
                    k.op("dve", V.tensor_tensor, R=[puB, sgB], W=[glB], out=gl[:, i, 30:30 + ST], in0=pu[:, :ST],
                         in1=sg[:, :ST], op=ALU.mult)
                for j in range(NTS):
                    pt, pB = psum()
                    for c in range(8):
                        k.op("pe", PE.matmul, R=[WAB, hTB], W=[pB], out=pt[:, :68],
                             lhsT=hT[:, c, j * 128:(j + 1) * 128], rhs=w_in[:, c, C_KI:C_KI + 68],
                             start=(c == 0), stop=(c == 7))
                    k.op("act", ACT.copy, R=[pB], W=[kktB], out=kkt[:, j, 64:128], in_=pt[:, 0:64])
                    k.op("act", ACT.activation, R=[pB], W=[wabB], out=wab[:, j, 0:4], in_=pt[:, 64:68],
                         func=AF.Abs, scale=1.0 / 16)
                    k.op("act", ACT.activation, R=[pB], W=[wabB], out=wab[:, j, 4:8], in_=pt[:, 64:68],
                         func=AF.Sign)
                if stop == "A1":
                    break
                def dsa_gen(j):
                    t = tiles[j]
                    cs = slice(j * 128, (j + 1) * 128)
                    pq, pqB = psum()
                    for c in range(2):
                        k.op("pe", PE.matmul, R=[cqB, WAB], W=[pqB], out=pq[:, 0:256], lhsT=cqT[:, c, cs],
                             rhs=w_qb[:, c, :], start=(c == 0), stop=(c == 1))
                    for c in range(2):
                        k.op("pe", PE.matmul, R=[cqB, WAB], W=[pqB], out=pq[:, 256:512], lhsT=cqT[:, c, cs],
                             rhs=w_qi[:, c, :], start=(c == 0), stop=(c == 1))
                    pkv, pkvB = psum()
                    k.op("pe", PE.matmul, R=[ckvB, WAB], W=[pkvB], out=pkv[:, 0:128], lhsT=ckvT[:, cs], rhs=w_kvb,
                         start=True, stop=True)
                    sq, sqB = scratch()
                    k.op("act", ACT.activation, R=[pqB], W=[sqB], out=sq[:, 0:256], in_=pq[:, 0:256], func=AF.Square)
                    k.op("act", ACT.activation, R=[pkvB], W=[sqB], out=sq[:, 256:320], in_=pkv[:, 0:64],
                         func=AF.Square)
                    ss, ssB = small()
                    k.op("dve", V.tensor_reduce, R=[sqB], W=[ssB], out=ss[:, 0:5],
                         in_=sq[:, 0:320].rearrange("p (a b) -> p a b", a=5), axis=AX.X, op=ALU.add)
                    k.op("dve", V.tensor_scalar, R=[ssB], W=[ssB], out=ss[:, 5:10], in0=ss[:, 0:5], scalar1=1.0 / 64,
                         scalar2=EPS, op0=ALU.mult, op1=ALU.add)
                    k.op("act", ACT.activation, R=[ssB], W=[ssB], out=ss[:, 5:10], in_=ss[:, 5:10], func=AF.Sqrt)
                    k.op("dve", V.reciprocal, R=[ssB], W=[ssB], out=ss[:, 10:15], in_=ss[:, 5:10])
                    tq, tqB = scratch()
                    k.op("dve", V.tensor_tensor, R=[pqB, ssB], W=[tqB], out=v3(tq[:, 0:256]), in0=v3(pq[:, 0:256]),
                         in1=bc(ss[:, 10:14].unsqueeze(2), [128, 4, 64]), op=ALU.mult)
                    QI, QIB = scratchb()
                    QI3 = v3(QI[:, :])
                    k.op("dve", V.tensor_tensor, R=[tqB, gq8B], W=[QIB], out=QI3[:, :, 0:64], in0=v3(tq[:, 0:256]),
                         in1=bc(gq8[:, :].unsqueeze(1), [128, 4, 64]), op=ALU.mult)
                    k.op("dve", V.tensor_tensor, R=[pqB, wabB, ssB], W=[QIB], out=QI3[:, :, 64:128], in0=v3(pq[:, 256:512]),
                         in1=bc(wab[:, j, 0:4].unsqueeze(2), [128, 4, 64]), op=ALU.mult)
                    k.op("dve", V.scalar_tensor_tensor, R=[pkvB, ssB, gkbB], W=[kktB], out=kkt[:, j, 0:64],
                         in0=pkv[:, 0:64], scalar=ss[:, 14:15], in1=gkb[:, :], op0=ALU.mult, op1=ALU.mult)
                    k.op("dve", V.tensor_copy, R=[pkvB, ssB], W=[vcB[t]], out=Vc[:, t, 0:64], in_=pkv[:, 64:128])
                    ptq, ptqB = psum()
                    for h in range(4):
                        k.op("pe", PE.transpose, R=[QIB, identbB], W=[ptqB], out=bfv(ptq)[:, h * 128:(h + 1) * 128],
                             in_=QI3[:, h, :], identity=ident_b[:, :])
                    QQ, QQB = QQs[t % 2]
                    k.op("act", ACT.copy, R=[ptqB], W=[QQB], out=QQ[:, :], in_=bfv(ptq)[:, 0:512])
                    ptk, ptkB = psum()
                    k.op("pe", PE.transpose, R=[kktB, identbB], W=[ptkB], out=bfv(ptk)[:, 0:128], in_=kkt[:, j, :],
                         identity=ident_b[:, :])
                    k.op("act", ACT.copy, R=[ptkB], W=[kkB[t]], out=KKT[:, t * 128:(t + 1) * 128],
                         in_=bfv(ptk)[:, 0:128])
                    Sw = (t + 1) * 128
                    for kc0 in range(0, Sw, 512):
                        wd = min(512, Sw - kc0)
                        kts = list(range(kc0 // 128, (kc0 + wd) // 128))
                        rr = []
                        for h in range(4):
                            ph, phB = psum()
                            k.op("pe", PE.matmul, R=[QQB] + [kkB[u] for u in kts], W=[phB], out=ph[:, :wd],
                                 lhsT=QQ[64:128, h * 128:(h + 1) * 128], rhs=KKT[64:128, kc0:kc0 + wd],
                                 start=True, stop=True)
                            rh, rhB = scratch()
                            k.op("act", ACT.activation, R=[phB], W=[rhB], out=rh[:, :wd], in_=ph[:, :wd], func=AF.Relu)
                            rr.append((rh, rhB))
                        k.op("dve", V.tensor_scalar, R=[rr[0][1], wabB], W=[workB], out=work[:, kc0:kc0 + wd],
                             in0=rr[0][0][:, :wd], scalar1=wab[:, j, 4:5], scalar2=None, op0=ALU.mult)
                        for h in range(1, 4):
                            k.op("dve", V.scalar_tensor_tensor, R=[rr[h][1], wabB, workB], W=[workB],
                                 out=work[:, kc0:kc0 + wd], in0=rr[h][0][:, :wd], scalar=wab[:, j, 4 + h:5 + h],
                                 in1=work[:, kc0:kc0 + wd], op0=ALU.mult, op1=ALU.add)
                    if t >= 2:
                        k.op("dve", V.tensor_reduce, R=[workB], W=[bstB], out=bst[:, 0:1], in_=work[:, :Sw], axis=AX.X,
                             op=ALU.min)
                        m8, m8B = m8s[0]
                        k.op("dve", V.max, R=[workB], W=[m8B], out=m8[:, :], in_=work[:, :Sw])
                    k.op("pool", POOL.affine_select, R=[workB], W=[workB], out=work[:, t * 128:(t + 1) * 128],
                         in_=work[:, t * 128:(t + 1) * 128], pattern=[[-1, 128]], compare_op=ALU.is_ge, fill=FILLNC,
                         base=0, channel_multiplier=1)
                    if t >= 2:
                        k.op("dve", V.tensor_tensor, R=[m8B, bstB], W=[bstB], out=bst[:, 2:3], in0=m8[:, 0:1],
                             in1=bst[:, 0:1], op=ALU.subtract)
                        k.op("dve", V.tensor_scalar, R=[bstB, pw2B], W=[nstB], out=nst[:, :], in0=pw2[:, :],
                             scalar1=bst[:, 2:3], scalar2=None, op0=ALU.mult)
                        k.op("dve", V.tensor_scalar, R=[bstB, m8B], W=[bstB], out=bst[:, 4:5], in0=bst[:, 0:1],
                             scalar1=m8[:, 0:1], scalar2=-0.5, op0=ALU.add, op1=ALU.mult)
                        k.op("dve", V.tensor_copy, R=[bstB], W=[bstB], out=bst[:, 3:4], in_=bst[:, 0:1])
                        thr = float(511 - Sw)
                        yield
                        for it in range(KB):
                            nm = bst[:, 4 + it % 2:5 + it % 2]
                            nmn = bst[:, 4 + (it + 1) % 2:5 + (it + 1) % 2]
                            cn = bst[:, 6 + it % 2:7 + it % 2]
                            k.op("act", ACT.activation, R=[workB, bstB], W=[junkB, bstB], out=junk8[:, :Sw],
                                 in_=work[:, :Sw], func=AF.Sign, bias=nm, accum_out=cn)
                            k.op("dve", V.tensor_scalar, R=[bstB], W=[bstB], out=bst[:, 8:9], in0=cn, scalar1=thr,
                                 scalar2=0.5, op0=ALU.is_ge, op1=ALU.subtract)
                            k.op("dve", V.scalar_tensor_tensor, R=[bstB, nstB], W=[bstB], out=nmn, in0=bst[:, 8:9],
                                 scalar=nst[:, it:it + 1], in1=nm, op0=ALU.mult, op1=ALU.add)
                            k.op("dve", V.tensor_scalar, R=[bstB], W=[bstB], out=bst[:, 9:10], in0=bst[:, 8:9],
                                 scalar1=0.5, scalar2=1.0e30, op0=ALU.subtract, op1=ALU.mult)
                            k.op("dve", V.scalar_tensor_tensor, R=[bstB], W=[bstB], out=bst[:, 3:4], in0=bst[:, 9:10],
                                 scalar=nm, in1=bst[:, 3:4], op0=ALU.subtract, op1=ALU.max)
                            yield
                    if t < 2:
                        yield
                    pso, psoB = psb[7]
                    for kc0 in range(0, Sw, 512):
                        wd = min(512, Sw - kc0)
                        mk, mkB = mks[mkc[0] % 2]
                        mkc[0] += 1
                        if t >= 2:
                            k.op("dve", V.tensor_scalar, R=[workB, bstB], W=[mkB], out=mk[:, :wd],
                                 in0=work[:, kc0:kc0 + wd], scalar1=bst[:, 3:4], scalar2=None, op0=ALU.is_ge)
                        else:
                            k.op("dve", V.tensor_single_scalar, R=[workB], W=[mkB], out=mk[:, :wd],
                                 in_=work[:, kc0:kc0 + wd], scalar=-1.0e38, op=ALU.is_gt)
                        for u in range(wd // 128):
                            kt = kc0 // 128 + u
                            pmk, pmkB = psum()
                            k.op("pe", PE.transpose, R=[mkB, identbB], W=[pmkB], out=bfv(pmk)[:, 0:128],
                                 in_=mk[:, u * 128:(u + 1) * 128], identity=ident_b[:, :])
                            pl, plB = psum()
                            k.op("pe", PE.matmul, R=[kkB[kt], QQB], W=[plB], out=pl[:, :],
                                 lhsT=KKT[0:64, kt * 128:(kt + 1) * 128], rhs=QQ[0:64, :], start=True, stop=True)
                            pT_, pTB = scratchb()
                            k.op("act", ACT.activation, R=[plB], W=[pTB], out=pT_[:, :], in_=pl[:, :], func=AF.Exp)
                            pmm, pmmB = scratchb()
                            k.op("dve", V.tensor_tensor, R=[pTB, pmkB], W=[pmmB], out=v3(pmm[:, :]), in0=v3(pT_[:, :]),
                                 in1=bc(bfv(pmk)[:, 0:128].unsqueeze(1), [128, 4, 128]), op=ALU.mult)
                            for h in range(4):
                                k.op("pe", PE.matmul, R=[pmmB, vcB[kt]], W=[psoB], out=pso[:, h * 65:(h + 1) * 65],
                                     lhsT=pmm[:, h * 128:(h + 1) * 128], rhs=Vc[:, kt, :],
                                     start=(kt == 0 and h == 0), stop=(kt == t and h == 3))
                    pso3 = pso[:, 0:260].rearrange("p (a b) -> p a b", a=4)
                    rd, rdB = small()
                    k.op("dve", V.reciprocal, R=[psoB], W=[rdB], out=rd[:, 0:4], in_=pso3[:, :, 64])
                    yb, ybB = scratchb()
                    k.op("dve", V.tensor_tensor, R=[psoB, rdB], W=[ybB], out=v3(yb[:, 0:256]), in0=pso3[:, :, 0:64],
                         in1=bc(rd[:, 0:4].unsqueeze(2), [128, 4, 64]), op=ALU.mult)
                    pty, ptyB = psum()
                    for c in range(2):
                        k.op("pe", PE.transpose, R=[ybB, identbB], W=[ptyB], out=bfv(pty)[:, c * 128:(c + 1) * 128],
                             in_=yb[:, c * 128:(c + 1) * 128], identity=ident_b[:, :])
                    k.op("act", ACT.copy, R=[ptyB], W=[yTB], out=yT[:, 4:6, cs],
                         in_=bfv(pty)[:, 0:256].rearrange("p (a b) -> p a b", a=2))

                dsa_g = dsa_gen(0)
                next(dsa_g)
                nticks = [0]

                def tick(n=1):
                    for _ in range(n):
                        if tiles[0] >= 2 and nticks[0] < KB:
                            nticks[0] += 1
                            next(dsa_g)

                pa_, paB = featmajor(C_A, 4)
                pb_, pbB = featmajor(C_B, 4)
                r = rows
                k.op("act", ACT.activation, R=[paB, gvB], W=[rowsB], out=r[:, 0, :], in_=pa_[:4, :ST],
                     func=AF.Abs, bias=gv[:, 1:2])
                k.op("act", ACT.activation, R=[rowsB], W=[rowsB], out=r[:, 1, :], in_=r[:, 0, :], func=AF.Exp,
                     scale=-1.0)
                k.op("act", ACT.activation, R=[rowsB, oneB], W=[rowsB], out=r[:, 1, :], in_=r[:, 1, :], func=AF.Ln,
                     bias=onec[0:4, 0:1])
                k.op("dve", V.tensor_scalar, R=[paB, gvB], W=[rowsB], out=r[:, 2, :], in0=pa_[:4, :ST],
                     scalar1=gv[:, 1:2], scalar2=0.0, op0=ALU.add, op1=ALU.max)
                k.op("dve", V.tensor_tensor, R=[rowsB], W=[rowsB], out=r[:, 2, :], in0=r[:, 2, :], in1=r[:, 1, :],
                     op=ALU.add)
                k.op("dve", V.tensor_scalar, R=[rowsB, gvB], W=[rowsB], out=r[:, 2, :], in0=r[:, 2, :],
                     scalar1=gv[:, 3:4], scalar2=None, op0=ALU.mult)
                for ch in range(NTS):
                    cs = slice(ch * 128, (ch + 1) * 128)
                    k.op("dve", V.tensor_tensor_scan, R=[rowsB, onesB], W=[rowsB], out=r[:, 3, cs],
                         data0=ones_f[0:4, 0:128], data1=r[:, 2, cs], initial=0.0, op0=ALU.mult, op1=ALU.add)
                k.op("act", ACT.activation, R=[pbB], W=[rowsB], out=r[:, 4, :], in_=pb_[:4, :ST], func=AF.Sigmoid)
                k.op("act", ACT.activation, R=[rowsB], W=[rowsB], out=r[:, 5, :], in_=r[:, 3, :], func=AF.Exp)
                for ch in range(NTS):
                    cs = slice(ch * 128, (ch + 1) * 128)
                    k.op("dve", V.tensor_scalar, R=[rowsB], W=[rowsB], out=r[:, 6, cs], in0=r[:, 3, cs],
                         scalar1=r[:, 3, ch * 128 + 127:ch * 128 + 128], scalar2=-1.0, op0=ALU.subtract,
                         op1=ALU.mult)
                k.op("act", ACT.activation, R=[rowsB], W=[rowsB], out=r[:, 6, :], in_=r[:, 6, :], func=AF.Exp)
                k.op("dve", V.tensor_tensor, R=[rowsB], W=[rowsB], out=r[:, 7, :], in0=r[:, 4, :], in1=r[:, 5, :],
                     op=ALU.mult)

                if stop == "A2":
                    break
                for ch in range(NTS):
                    cs = slice(ch * 128, (ch + 1) * 128)
                    col, colB = cols[ch]
                    pt, pB = psum()
                    for qi_, rq in enumerate((3, 4, 7, 6)):
                        k.op("pe", PE.matmul, R=[rowsB, identB], W=[pB], out=pt[:, qi_ * 4:(qi_ + 1) * 4],
                             lhsT=r[:, rq, cs], rhs=ident_f[0:4, 0:4], start=True, stop=True)
                    k.op("act", ACT.copy, R=[pB], W=[colB], out=col[:, :], in_=pt[:, 0:16])
                    c_b, c_beta, c_be, c_elb = (col[:, 0:4], col[:, 4:8], col[:, 8:12], col[:, 12:16])
                    prb, prbB = psum()
                    pre_, preB_ = psum()
                    for h in range(4):
                        k.op("pe", PE.matmul, R=[selB, rowsB], W=[prbB], out=prb[:, h * 128:(h + 1) * 128],
                             lhsT=sel[:, h, :], rhs=r[:, 3, cs], start=True, stop=True)
                    for h in range(4):
                        k.op("pe", PE.matmul, R=[selB, rowsB], W=[preB_], out=pre_[:, h * 128:(h + 1) * 128],
                             lhsT=sel[:, h, :], rhs=r[:, 5, cs], start=True, stop=True)
                    qd, qdB = gd["qd"]
                    k.op("dve", V.tensor_tensor, R=[preB_] + qkvB[0:4], W=[qdB], out=v3(qd[:, :]),
                         in0=qkvT[:, 0:4, cs], in1=v3(pre_[:, :]), op=ALU.mult)
                    ebl, eblB = small()
                    k.op("dve", V.tensor_copy, R=[preB_], W=[eblB], out=ebl[:, 0:4], in_=v3(pre_[:, :])[:, :, 127])
                    pk, pkB = psum()
                    pv, pvB = psum()
                    for h in range(4):
                        k.op("pe", PE.transpose, R=[qkvB[4 + h], identB], W=[pkB], out=pk[:, h * 128:(h + 1) * 128],
                             in_=qkvT[:, 4 + h, cs], identity=ident_f[:, :])
                    for h in range(4):
                        k.op("pe", PE.transpose, R=[qkvB[8 + h], identB], W=[pvB], out=pv[:, h * 128:(h + 1) * 128],
                             in_=qkvT[:, 8 + h, cs], identity=ident_f[:, :])
                    Kbd, KbdB = gd["Kbd"]
                    Kdec, KdecB = gd["Kdec"]
                    Vb, VbB = gd["Vb"]
                    k.op("dve", V.tensor_tensor, R=[pkB, colB], W=[KbdB], out=v3(Kbd[:, :]), in0=v3(pk[:, :]),
                         in1=bc(c_be.unsqueeze(2), [128, 4, 128]), op=ALU.mult)
                    k.op("dve", V.tensor_tensor, R=[pkB, colB], W=[KdecB], out=v3(Kdec[:, :]), in0=v3(pk[:, :]),
                         in1=bc(c_elb.unsqueeze(2), [128, 4, 128]), op=ALU.mult)
                    k.op("dve", V.tensor_tensor, R=[pvB, colB], W=[VbB], out=v3(Vb[:, :]), in0=v3(pv[:, :]),
                         in1=bc(c_beta.unsqueeze(2), [128, 4, 128]), op=ALU.mult)
                    tick()
                    if stop == "A3a":
                        break
                    pg1, pg1B = psum()
                    pg2, pg2B = psum()
                    for h in range(4):
                        k.op("pe", PE.matmul, R=[qkvB[4 + h]], W=[pg1B], out=pg1[:, h * 128:(h + 1) * 128],
                             lhsT=qkvT[:, 4 + h, cs], rhs=qkvT[:, 4 + h, cs], start=True, stop=True)
                    for h in range(4):
                        k.op("pe", PE.matmul, R=[qkvB[4 + h], qkvB[h]], W=[pg2B], out=pg2[:, h * 128:(h + 1) * 128],
                             lhsT=qkvT[:, 4 + h, cs], rhs=qkvT[:, h, cs], start=True, stop=True)
                    GT, GTB = scratch()
                    k.op("dve", V.tensor_tensor, R=[prbB, colB], W=[GTB], out=v3(GT[:, :]), in0=v3(prb[:, :]),
                         in1=bc(c_b.unsqueeze(2), [128, 4, 128]), op=ALU.subtract)
                    k.op("pool", POOL.affine_select, R=[GTB], W=[GTB], out=v3(GT[:, :]), in_=v3(GT[:, :]),
                         pattern=[[0, 4], [1, 128]], compare_op=ALU.is_ge, fill=FILLM, base=0,
                         channel_multiplier=-1)
                    k.op("act", ACT.activation, R=[GTB], W=[GTB], out=GT[:, :], in_=GT[:, :], func=AF.Exp)
                    tick()
                    QKT, QKTB = gd["QKT"]
                    k.op("dve", V.tensor_tensor, R=[pg2B, GTB], W=[QKTB], out=QKT[:, :], in0=pg2[:, :], in1=GT[:, :],
                         op=ALU.mult)
                    MT, MTB = scratch()
                    k.op("dve", V.tensor_tensor, R=[pg1B, GTB], W=[MTB], out=MT[:, :], in0=pg1[:, :], in1=GT[:, :],
                         op=ALU.mult)
                    k.op("pool", POOL.affine_select, R=[MTB], W=[MTB], out=v3(MT[:, :]), in_=v3(MT[:, :]),
                         pattern=[[0, 4], [1, 128]], compare_op=ALU.is_gt, fill=FILL0, base=0, channel_multiplier=-1)
                    tick()
                    if stop == "A3b":
                        break
                    pA, pAB = psum()
                    for h in range(4):
                        k.op("pe", PE.transpose, R=[MTB, identB], W=[pAB], out=pA[:, h * 128:(h + 1) * 128],
                             in_=MT[:, h * 128:(h + 1) * 128], identity=ident_f[:, :])
                    P, PB = gd["P0"]
                    k.op("dve", V.tensor_tensor, R=[pAB, colB], W=[PB], out=v3(P[:, :]), in0=v3(pA[:, :]),
                         in1=bc(c_beta.unsqueeze(2), [128, 4, 128]), op=ALU.mult)
                    tick()
                    if stop == "A3b1":
                        break
                    pAT, pATB = psum()
                    for h in range(4):
                        k.op("pe", PE.transpose, R=[PB, identB], W=[pATB], out=pAT[:, h * 128:(h + 1) * 128],
                             in_=P[:, h * 128:(h + 1) * 128], identity=ident_f[:, :])
                    if stop == "A3b1a":
                        break
                    PT_, PTB = gd["PT0"]
                    k.op("act", ACT.copy, R=[pATB], W=[PTB], out=PT_[:, :], in_=pAT[:, :])
                    if stop == "A3b1b":
                        break
                    RT, RTB = gd["RT0"]
                    for h in range(4):
                        hs = slice(h * 128, (h + 1) * 128)
                        k.op("dve", V.tensor_tensor, R=[identB, PTB], W=[RTB], out=RT[:, hs], in0=ident_f[:, :],
                             in1=PT_[:, hs], op=ALU.subtract)
                    tick()
                    if stop == "A3b2":
                        break
                    for lvl in range(6):
                        if stop == "A3b3" and lvl == 1:
                            break
                        p2, p2B = psum()
                        for h in range(4):
                            hs = slice(h * 128, (h + 1) * 128)
                            k.op("pe", PE.matmul, R=[PTB, PB], W=[p2B], out=p2[:, hs], lhsT=PT_[:, hs], rhs=P[:, hs],
                                 start=True, stop=True)
                        if lvl < 5:
                            p2t, p2tB = psum()
                            for h in range(4):
                                hs = slice(h * 128, (h + 1) * 128)
                                k.op("pe", PE.matmul, R=[PTB, PB], W=[p2tB], out=p2t[:, hs], lhsT=P[:, hs],
                                     rhs=PT_[:, hs], start=True, stop=True)
                        Pn, PnB = gd["P%d" % ((lvl + 1) % 2)]
                        k.op("act", ACT.copy, R=[p2B], W=[PnB], out=Pn[:, :], in_=p2[:, :])
                        tick()
                        if lvl < 5:
                            PTn, PTnB = gd["PT%d" % ((lvl + 1) % 2)]
                            k.op("act", ACT.copy, R=[p2tB], W=[PTnB], out=PTn[:, :], in_=p2t[:, :])
                        p3, p3B = psum()
                        for h in range(4):
                            hs = slice(h * 128, (h + 1) * 128)
                            k.op("pe", PE.matmul, R=[PnB, RTB], W=[p3B], out=p3[:, hs], lhsT=Pn[:, hs], rhs=RT[:, hs],
                                 start=True, stop=True)
                        RTn, RTnB = gd["RT%d" % ((lvl + 1) % 2)]
                        k.op("dve", V.tensor_tensor, R=[p3B, RTB], W=[RTnB], out=RTn[:, :], in0=p3[:, :], in1=RT[:, :],
                             op=ALU.add)
                        tick(2)
                        RT, RTB = RTn, RTnB
                        P, PB = Pn, PnB
                        if lvl < 5:
                            PT_, PTB = PTn, PTnB
                    if stop in ("A3c", "A3b3"):
                        break
                    pw, pwB = psum()
                    pu_, puB_ = psum()
                    for h in range(4):
                        hs = slice(h * 128, (h + 1) * 128)
                        k.op("pe", PE.matmul, R=[KbdB, RTB], W=[pwB], out=pw[:, hs], lhsT=Kbd[:, hs], rhs=RT[:, hs],
                             start=True, stop=True)
                    for h in range(4):
                        hs = slice(h * 128, (h + 1) * 128)
                        k.op("pe", PE.matmul, R=[VbB, RTB], W=[puB_], out=pu_[:, hs], lhsT=RT[:, hs], rhs=Vb[:, hs],
                             start=True, stop=True)
                    WT, WTB = scratch()
                    U, UB = scratch()
                    k.op("act", ACT.copy, R=[pwB], W=[WTB], out=WT[:, :], in_=pw[:, :])
                    k.op("act", ACT.copy, R=[puB_], W=[UB], out=U[:, :], in_=pu_[:, :])
                    tick()
                    tg = NTS * s_i + ch
                    Sc, ScB = Sst[tg % 2]
                    Sn, SnB = Sst[(tg + 1) % 2]
                    pws, pwsB = psum()
                    for h in range(4):
                        hs = slice(h * 128, (h + 1) * 128)
                        k.op("pe", PE.matmul, R=[WTB, ScB], W=[pwsB], out=pws[:, hs], lhsT=WT[:, hs], rhs=Sc[:, h, :],
                             start=True, stop=True)
                    Vn, VnB = scratch()
                    k.op("dve", V.scalar_tensor_tensor, R=[UB, pwsB], W=[VnB], out=Vn[:, :], in0=pws[:, :], scalar=-1.0,
                         in1=U[:, :], op0=ALU.mult, op1=ALU.add)
                    po, poB = psum()
                    for h in range(4):
                        hs = slice(h * 128, (h + 1) * 128)
                        k.op("pe", PE.matmul, R=[ScB, qdB], W=[poB], out=po[:, hs], lhsT=Sc[:, h, :], rhs=qd[:, hs],
                             start=True, stop=False)
                        k.op("pe", PE.matmul, R=[VnB, QKTB], W=[poB], out=po[:, hs], lhsT=Vn[:, hs], rhs=QKT[:, hs],
                             start=False, stop=True)
                    ps_, psB_ = psum()
                    for h in range(4):
                        hs = slice(h * 128, (h + 1) * 128)
                        k.op("pe", PE.matmul, R=[KdecB, VnB], W=[psB_], out=ps_[:, hs], lhsT=Kdec[:, hs], rhs=Vn[:, hs],
                             start=True, stop=True)
                    k.op("dve", V.tensor_tensor, R=[ScB, eblB], W=[SnB], out=Sn[:, :, :], in0=Sc[:, :, :],
                         in1=bc(ebl[:, 0:4].unsqueeze(2), [128, 4, 128]), op=ALU.mult)
                    k.op("dve", V.tensor_tensor, R=[SnB, psB_], W=[SnB], out=Sn[:, :, :], in0=Sn[:, :, :],
                         in1=v3(ps_[:, :]), op=ALU.add)
                    tick()
                    o32, o32B = scratch()
                    osq, osqB = scratch()
                    k.op("act", ACT.copy, R=[poB], W=[o32B], out=o32[:, :], in_=po[:, :])
                    k.op("act", ACT.activation, R=[poB], W=[osqB], out=osq[:, :], in_=po[:, :], func=AF.Square)
                    pm_, pmB_ = psum()
                    k.op("pe", PE.matmul, R=[o128B, osqB], W=[pmB_], out=pm_[:, :], lhsT=ones128[:, :], rhs=osq[:, :],
                         start=True, stop=True)
                    k.op("act", ACT.activation, R=[pmB_, epsB], W=[osqB], out=osq[:, :], in_=pm_[:, :], func=AF.Sqrt,
                         bias=epsc[:, 0:1])
                    k.op("dve", V.reciprocal, R=[osqB], W=[osqB], out=osq[:, :], in_=osq[:, :])
                    k.op("dve", V.scalar_tensor_tensor, R=[o32B, osqB, gncB], W=[o32B], out=o32[:, :], in0=o32[:, :],
                         scalar=gnc[:, 0:1], in1=osq[:, :], op0=ALU.mult, op1=ALU.mult)
                    k.op("dve", V.tensor_tensor, R=[o32B, zsB], W=[yTB], out=yT[:, 0:4, cs], in0=v3(o32[:, :]),
                         in1=zs[:, :, cs], op=ALU.mult)

                if stop in ("A3", "A3a", "A3b", "A3c", "A3b1", "A3b2", "A3b3", "A3b1a", "A3b1b"):
                    break
                for _ in dsa_g:
                    pass
                if stop == "A4":
                    break
                yc, ycB = scratch()
                ysq, ysqB = scratch()
                for i in range(2):
                    pc, pcB = psum()
                    for jj in range(31):
                        k.op("pe", PE.matmul, R=[dgcB, glB], W=[pcB], out=pc[:, :ST], lhsT=dgc[:, i, jj, :],
                             rhs=gl[:, i, jj:jj + ST], start=(jj == 0), stop=(jj == 30))
                    k.op("act", ACT.activation, R=[pcB, cvecB], W=[ycB], out=yc[:, i * ST:(i + 1) * ST], in_=pc[:, :ST],
                         func=AF.Identity, bias=cvec[:, 0, i:i + 1])
                    k.op("act", ACT.activation, R=[pcB, cvecB], W=[ysqB], out=ysq[:, i * ST:(i + 1) * ST],
                         in_=pc[:, :ST], func=AF.Square, bias=cvec[:, 0, i:i + 1])
                k.op("dve", V.tensor_copy, R=[glB], W=[glB], out=gl[:, :, 0:30], in_=gl[:, :, ST:ST + 30])
                pmu, pmuB = psum()
                pms, pmsB = psum()
                for i in range(2):
                    k.op("pe", PE.matmul, R=[o256B, ycB], W=[pmuB], out=pmu[:, :ST], lhsT=ones256[:, :],
                         rhs=yc[:, i * ST:(i + 1) * ST], start=(i == 0), stop=(i == 1))
                for i in range(2):
                    k.op("pe", PE.matmul, R=[o256B, ysqB], W=[pmsB], out=pms[:, :ST], lhsT=ones256[:, :],
                         rhs=ysq[:, i * ST:(i + 1) * ST], start=(i == 0), stop=(i == 1))
                mu, muB = scratch()
                k.op("act", ACT.copy, R=[pmuB], W=[muB], out=mu[:, 0:ST], in_=pmu[:, :ST])
                k.op("act", ACT.activation, R=[pmuB], W=[muB], out=mu[:, ST:2 * ST], in_=pmu[:, :ST], func=AF.Square)
                k.op("dve", V.tensor_tensor, R=[pmsB, muB], W=[muB], out=mu[:, ST:2 * ST], in0=pms[:, :ST],
                     in1=mu[:, ST:2 * ST], op=ALU.subtract)
                k.op("act", ACT.activation, R=[muB, epsB], W=[muB], out=mu[:, ST:2 * ST], in_=mu[:, ST:2 * ST],
                     func=AF.Sqrt, bias=epsc[:, 0:1])
                k.op("dve", V.reciprocal, R=[muB], W=[muB], out=mu[:, ST:2 * ST], in_=mu[:, ST:2 * ST])
                for i in range(2):
                    k.op("dve", V.tensor_tensor, R=[ycB, muB], W=[ycB], out=yc[:, i * ST:(i + 1) * ST],
                         in0=yc[:, i * ST:(i + 1) * ST], in1=mu[:, 0:ST], op=ALU.subtract)
                    k.op("dve", V.tensor_tensor, R=[ycB, muB], W=[ycB], out=yc[:, i * ST:(i + 1) * ST],
                         in0=yc[:, i * ST:(i + 1) * ST], in1=mu[:, ST:2 * ST], op=ALU.mult)
                    k.op("act", ACT.activation, R=[ycB, cvecB], W=[yTB], out=yT[:, 6 + i, :],
                         in_=yc[:, i * ST:(i + 1) * ST], func=AF.Silu, scale=cvec[:, 1, i:i + 1],
                         bias=cvec[:, 2, i:i + 1])

                for j in range(NTS):
                    xt, xtB = xts[j]
                    t = tiles[j]
                    for n in range(2):
                        pt, pB = psum()
                        for c in range(8):
                            k.op("pe", PE.matmul, R=[yTB, WAB], W=[pB], out=pt[:, :],
                                 lhsT=yT[:, c, j * 128:(j + 1) * 128], rhs=w_out[:, c, n * 512:(n + 1) * 512],
                                 start=(c == 0), stop=(c == 7))
                        k.op("dve", V.tensor_tensor, R=[pB, xtB], W=[xtB], out=xt[:, n * 512:(n + 1) * 512],
                             in0=pt[:, :], in1=xt[:, n * 512:(n + 1) * 512], op=ALU.add)
                    k.dma("sp", out_d[t * 128:(t + 1) * 128, :], xt[:, :], xtB, R=[xtB], W=[xdB[t]])
        k.barrier()
        if stop in ("A", "W", "A1", "A2", "A3", "A3a", "A3b", "A3c", "A4", "A3b1", "A3b2", "A3b3", "A3b1a", "A3b1b"):
            break

        with contextlib.ExitStack() as pa:
            def sba(name, shape, dt=F32):
                t = pa.enter_context(nc.sbuf_tensor("%s_x%d" % (name, l), list(shape), dt))
                return t, Buf(name)
            WA, WAB = sba("WA", [128, 16384], BF16)
            for i in range(2):
                stage[i] = sba("stgX%d" % i, [128, 2048])
            wq = WA[:, 0:8 * 512].rearrange("p (c n) -> p c n", c=8)
            wkv = WA[:, 4096:4096 + 8 * 1024].rearrange("p (c n) -> p c n", c=8)
            wo = WA[:, 12288:12288 + 4 * 1024].rearrange("p (c n) -> p c n", c=4)
            load_gcol(norm_cross_d[l])
            for c in range(8):
                cast_rows(wq[:, c, :], WAB, wq_d[l, c * 128:(c + 1) * 128, :], 512, gcol[:, c:c + 1])
            load_gcol(norm_mem_d)
            for c in range(8):
                cast_rows(wkv[:, c, :], WAB, wkv_d[l, c * 128:(c + 1) * 128, :], 1024, gcol[:, c:c + 1])
            for c in range(4):
                cast_rows(wo[:, c, :], WAB, wo_d[l, c * 128:(c + 1) * 128, :], D)
            gxq, gxqB = sba("gxq", [128, 128])
            gxk, gxkB = sba("gxk", [128, 128])
            k.dma("sp", gxq[:, :], xq_norm_d[l].partition_broadcast(128), gxqB, W=[gxqB])
            k.dma("sp", gxk[:, :], xk_norm_d[l].partition_broadcast(128), gxkB, W=[gxkB])
            k.op("dve", V.tensor_scalar, R=[gxqB], W=[gxqB], out=gxq[:, :], in0=gxq[:, :], scalar1=128 ** -0.5,
                 scalar2=None, op0=ALU.mult)
            kmT, kmTB = sba("kmT", [128, 4, MEM], BF16)
            vm1, vm1B = sba("vm1", [128, 2, 4, 129], BF16)
            k.op("pool", POOL.memset, W=[vm1B], ap=vm1[:, :, :, :], constant=1.0)
            scr = [sba("xscr%d" % i, [128, 512]) for i in range(4)]
            scb = [sba("xscb%d" % i, [128, 1024], BF16) for i in range(4)]
            sm = [sba("xsm%d" % i, [128, 16]) for i in range(4)]
            cx = [0, 0, 0]

            def scratch():
                cx[0] += 1
                return scr[cx[0] % 4]

            def scratchb():
                cx[1] += 1
                return scb[cx[1] % 4]

            def small():
                cx[2] += 1
                return sm[cx[2] % 4]

            def v3(ap, a=4):
                return ap.rearrange("p (a b) -> p a b", a=a)

            def head_rms(pt, pB, gtile, gB, dst3, dstB):
                sq, sqB = scratch()
                k.op("act", ACT.activation, R=[pB], W=[sqB], out=sq[:, :], in_=pt[:, :], func=AF.Square)
                ss, ssB = small()
                k.op("dve", V.tensor_reduce, R=[sqB], W=[ssB], out=ss[:, 0:4], in_=v3(sq[:, :]), axis=AX.X, op=ALU.add)
                k.op("dve", V.tensor_scalar, R=[ssB], W=[ssB], out=ss[:, 4:8], in0=ss[:, 0:4], scalar1=1.0 / 128,
                     scalar2=EPS, op0=ALU.mult, op1=ALU.add)
                k.op("act", ACT.activation, R=[ssB], W=[ssB], out=ss[:, 4:8], in_=ss[:, 4:8], func=AF.Sqrt)
                k.op("dve", V.reciprocal, R=[ssB], W=[ssB], out=ss[:, 8:12], in_=ss[:, 4:8])
                k.op("dve", V.tensor_tensor, R=[pB, ssB], W=[sqB], out=v3(sq[:, :]), in0=v3(pt[:, :]),
                     in1=bc(ss[:, 8:12].unsqueeze(2), [128, 4, 128]), op=ALU.mult)
                k.op("dve", V.tensor_tensor, R=[sqB, gB], W=[dstB], out=dst3, in0=v3(sq[:, :]),
                     in1=bc(gtile[:, :].unsqueeze(1), [128, 4, 128]), op=ALU.mult)

            for mt in range(2):
                xt, xtB = xts[mt]
                k.dma("sp", xt[:, :], mem_d[mt * 128:(mt + 1) * 128, :], xtB, W=[xtB])
                norm_T(xt, xtB, 0)
                pk_, pkB_ = psum()
                pv_, pvB_ = psum()
                for c in range(8):
                    k.op("pe", PE.matmul, R=[hTB, WAB], W=[pkB_], out=pk_[:, :], lhsT=hT[:, c, 0:128],
                         rhs=wkv[:, c, 0:512], start=(c == 0), stop=(c == 7))
                for c in range(8):
                    k.op("pe", PE.matmul, R=[hTB, WAB], W=[pvB_], out=pv_[:, :], lhsT=hT[:, c, 0:128],
                         rhs=wkv[:, c, 512:1024], start=(c == 0), stop=(c == 7))
                kn, knB = scratchb()
                head_rms(pk_, pkB_, gxk, gxkB, v3(kn[:, 0:512]), knB)
                ptk, ptkB = psum()
                for h in range(4):
                    k.op("pe", PE.transpose, R=[knB, identbB], W=[ptkB], out=bfv(ptk)[:, h * 128:(h + 1) * 128],
                         in_=kn[:, h * 128:(h + 1) * 128], identity=ident_b[:, :])
                k.op("act", ACT.copy, R=[ptkB], W=[kmTB], out=kmT[:, :, mt * 128:(mt + 1) * 128],
                     in_=v3(bfv(ptk)[:, 0:512]))
                k.op("act", ACT.copy, R=[pvB_], W=[vm1B], out=vm1[:, mt, :, 0:128], in_=v3(pv_[:, :]))

            for t in range(NT):
                xt, xtB = xts[t % 2]
                k.dma("sp", xt[:, :], out_d[t * 128:(t + 1) * 128, :], xtB, R=[xdB[t]], W=[xtB])
                norm_T(xt, xtB, 0)
                pq, pqB = psum()
                for c in range(8):
                    k.op("pe", PE.matmul, R=[hTB, WAB], W=[pqB], out=pq[:, :], lhsT=hT[:, c, 0:128], rhs=wq[:, c, :],
                         start=(c == 0), stop=(c == 7))
                qn, qnB = scratchb()
                head_rms(pq, pqB, gxq, gxqB, v3(qn[:, 0:512]), qnB)
                ptq, ptqB = psum()
                for h in range(4):
                    k.op("pe", PE.transpose, R=[qnB, identbB], W=[ptqB], out=bfv(ptq)[:, h * 128:(h + 1) * 128],
                         in_=qn[:, h * 128:(h + 1) * 128], identity=ident_b[:, :])
                qT, qTB = scratchb()
                k.op("act", ACT.copy, R=[ptqB], W=[qTB], out=qT[:, 0:512], in_=bfv(ptq)[:, 0:512])
                pT_, pTB = scratchb()
                for mt in range(2):
                    pl, plB = psum()
                    for h in range(4):
                        k.op("pe", PE.matmul, R=[kmTB, qTB], W=[plB], out=pl[:, h * 128:(h + 1) * 128],
                             lhsT=kmT[:, h, mt * 128:(mt + 1) * 128], rhs=qT[:, h * 128:(h + 1) * 128], start=True,
                             stop=True)
                    k.op("act", ACT.activation, R=[plB], W=[pTB], out=pT_[:, mt * 512:(mt + 1) * 512], in_=pl[:, :],
                         func=AF.Exp)
                on, onB = scratchb()
                for hh in range(2):
                    po, poB = psum()
                    for h2 in range(2):
                        h = hh * 2 + h2
                        for mt in range(2):
                            k.op("pe", PE.matmul, R=[pTB, vm1B], W=[poB], out=po[:, h2 * 129:(h2 + 1) * 129],
                                 lhsT=pT_[:, mt * 512 + h * 128:mt * 512 + (h + 1) * 128], rhs=vm1[:, mt, h, :],
                                 start=(mt == 0), stop=(mt == 1))
                    po3 = po[:, 0:258].rearrange("p (a b) -> p a b", a=2)
                    rd, rdB = small()
                    k.op("dve", V.reciprocal, R=[poB], W=[rdB], out=rd[:, 0:2], in_=po3[:, :, 128])
                    k.op("dve", V.tensor_tensor, R=[poB, rdB], W=[onB],
                         out=on[:, hh * 256:(hh + 1) * 256].rearrange("p (a b) -> p a b", a=2), in0=po3[:, :, 0:128],
                         in1=bc(rd[:, 0:2].unsqueeze(2), [128, 2, 128]), op=ALU.mult)
                pto, ptoB = psum()
                for h in range(4):
                    k.op("pe", PE.transpose, R=[onB, identbB], W=[ptoB], out=bfv(pto)[:, h * 128:(h + 1) * 128],
                         in_=on[:, h * 128:(h + 1) * 128], identity=ident_b[:, :])
                oT, oTB = scratchb()
                k.op("act", ACT.copy, R=[ptoB], W=[oTB], out=oT[:, 0:512], in_=bfv(pto)[:, 0:512])
                for n in range(2):
                    pt, pB = psum()
                    for h in range(4):
                        k.op("pe", PE.matmul, R=[oTB, WAB], W=[pB], out=pt[:, :], lhsT=oT[:, h * 128:(h + 1) * 128],
                             rhs=wo[:, h, n * 512:(n + 1) * 512], start=(h == 0), stop=(h == 3))
                    k.op("dve", V.tensor_tensor, R=[pB, xtB], W=[xtB], out=xt[:, n * 512:(n + 1) * 512], in0=pt[:, :],
                         in1=xt[:, n * 512:(n + 1) * 512], op=ALU.add)
                k.dma("sp", out_d[t * 128:(t + 1) * 128, :], xt[:, :], xtB, R=[xtB], W=[xdB[t]])
        k.barrier()
        if stop == "A2":
            break

        with contextlib.ExitStack() as pa:
            def sba(name, shape, dt=F32):
                t = pa.enter_context(nc.sbuf_tensor("%s_m%d" % (name, l), list(shape), dt))
                return t, Buf(name)
            ST = ST_B
            NST = S // ST
            WA, WAB = sba("WA", [128, 65536], BF16)
            for i in range(2):
                stage[i] = sba("stgM%d" % i, [128, 2048])
            w1 = WA[:, 0:8 * 4096].rearrange("p (c n) -> p c n", c=8)
            w2 = WA[:, 32768:32768 + 32 * 1024].rearrange("p (c n) -> p c n", c=32)
            load_gcol(norm_mlp_d[l])
            for c in range(8):
                cast_rows(w1[:, c, :], WAB, w1_d[l, c * 128:(c + 1) * 128, :], 4096, gcol[:, c:c + 1])
            for c in range(32):
                cast_rows(w2[:, c, :], WAB, w2_d[l, c * 128:(c + 1) * 128, :], D)
            aT, aTB = sba("aT", [128, 32, ST], BF16)
            rrs = [sba("rr%d" % i, [128, ST]) for i in range(3)]
            for s_i in range(NST):
                t0 = s_i * ST
                for j in range(2):
                    xt, xtB = xts[j]
                    k.dma("sp", xt[:, :], out_d[t0 + j * 128:t0 + (j + 1) * 128, :], xtB, R=[xdB[2 * s_i + j]],
                          W=[xtB])
                    norm_T(xt, xtB, j * 128)
                for f in range(32):
                    pt, pB = psum()
                    for c in range(8):
                        k.op("pe", PE.matmul, R=[WAB, hTB], W=[pB], out=pt[:, :ST], lhsT=w1[:, c, f * 128:(f + 1) * 128],
                             rhs=hT[:, c, :], start=(c == 0), stop=(c == 7))
                    rr_, rrB = rrs[f % 3]
                    k.op("act", ACT.activation, R=[pB], W=[rrB], out=rr_[:, :], in_=pt[:, :ST], func=AF.Relu)
                    e = "pool" if f % 2 == 0 else "dve"
                    k.op(e, k.eng[e].tensor_tensor, R=[rrB], W=[aTB], out=aT[:, f, :], in0=rr_[:, :], in1=rr_[:, :],
                         op=ALU.mult)
                for j in range(2):
                    xt, xtB = xts[j]
                    t = 2 * s_i + j
                    for n in range(2):
                        pt, pB = psum()
                        for f in range(32):
                            k.op("pe", PE.matmul, R=[aTB, WAB], W=[pB], out=pt[:, :],
                                 lhsT=aT[:, f, j * 128:(j + 1) * 128], rhs=w2[:, f, n * 512:(n + 1) * 512],
                                 start=(f == 0), stop=(f == 31))
                        k.op("dve", V.tensor_tensor, R=[pB, xtB], W=[xtB], out=xt[:, n * 512:(n + 1) * 512],
                             in0=pt[:, :], in1=xt[:, n * 512:(n + 1) * 512], op=ALU.add)
                    k.dma("sp", out_d[t * 128:(t + 1) * 128, :], xt[:, :], xtB, R=[xtB], W=[xdB[t]])
        k.barrier()
    k.finish()
    es.close()
    return nc, k


_INPUT_NAMES = ["x", "mem", "norm_mix", "w_in", "gdn_conv", "gdn_a_log", "gdn_dt_bias", "gdn_norm", "dsa_w_qb",
                "dsa_w_qi", "dsa_w_kvb", "dsa_q_norm", "dsa_k_norm", "conv_dw", "conv_dw_b", "conv_ln_g",
                "conv_ln_b", "w_out", "norm_mem", "norm_cross", "xa_wq", "xa_wkv", "xa_q_norm", "xa_k_norm",
                "xa_wo", "norm_mlp", "mlp_w1", "mlp_w2"]


def run(inputs, S=4096, NL=2, stop=None, ncores=8, trace=False):
    nc, kk = build(S=S, NL=NL, stop=stop)
    shared = {n: np.ascontiguousarray(np.asarray(inputs[n], dtype=np.float32)) for n in _INPUT_NAMES
              if n not in ("x", "mem")}
    x = np.asarray(inputs["x"], dtype=np.float32)
    mem = np.asarray(inputs["mem"], dtype=np.float32)
    in_maps = []
    for b in range(ncores):
        m = dict(shared)
        m["x"] = np.ascontiguousarray(x[b, :S])
        m["mem"] = np.ascontiguousarray(mem[b])
        in_maps.append(m)
    res = run_bass_kernel_spmd(nc, in_maps, core_ids=list(range(ncores)), trace=trace)
    out = np.stack([np.asarray(r["out"]) for r in res.results], axis=0)
    return out, res


def kernel(**inputs):
    out, _ = run(inputs)
    return out.astype(np.float32)
```

```python
import contextlib
import numpy as np
import concourse.bass as bass
import concourse.mybir as mybir
from concourse.bass_utils import run_bass_kernel_spmd

F32 = mybir.dt.float32
BF16 = mybir.dt.bfloat16
AF = mybir.ActivationFunctionType
ALU = mybir.AluOpType
AX = mybir.AxisListType

D = 1024
NIN = 3020
MEM = 256
EPS = 1e-6
C_A, C_B, C_CQ, C_CKV, C_KI, C_WI, C_UG = 2048, 2052, 2056, 2312, 2440, 2504, 2508
NEG_NC = -2.0e38
NEG_SEL = -3.0e38
ST_A = 128
ST_B = 256


class Buf:
    __slots__ = ("w", "r", "dsem", "dcnt", "name")

    def __init__(self, name=""):
        self.w = {}
        self.r = {}
        self.dsem = None
        self.dcnt = 0
        self.name = name


class K:
    def __init__(self, nc, es):
        self.nc = nc
        self.es = es
        self.eng = {"pe": nc.tensor, "act": nc.scalar, "dve": nc.vector, "pool": nc.gpsimd, "sp": nc.sync}
        self.sem = {e: es.enter_context(nc.semaphore("s_" + e)) for e in ("pe", "act", "dve", "pool")}
        self.cnt = {e: 0 for e in self.sem}
        self.waited = {e: {} for e in self.eng}
        self.nsem = 0
        self.dbufs = []
        self.ninst = 0

    def _wait(self, e, deps):
        need = {}
        for key, (sem, val) in deps:
            if key == "pe" and e == "pe":
                continue
            if need.get(key, (None, 0))[1] < val:
                need[key] = (sem, val)
        wd = self.waited[e]
        for key, (sem, val) in need.items():
            if wd.get(key, 0) >= val:
                continue
            self.eng[e].wait_ge(sem, val)
            wd[key] = val
            self.ninst += 1

    @staticmethod
    def _deps(reads, writes):
        deps = []
        for b in reads:
            deps.extend(b.w.items())
        for b in writes:
            deps.extend(b.w.items())
            deps.extend(b.r.items())
        return deps

    @staticmethod
    def _mark(tokkey, tok, reads, writes):
        for b in reads:
            b.r[tokkey] = tok
        for b in writes:
            if b.r:
                b.w = {}
                b.r = {}
            b.w[tokkey] = tok

    def op(self, e, fn, R=(), W=(), **kw):
        self._wait(e, self._deps(R, W))
        ins = fn(**kw)
        self.cnt[e] += 1
        ins.then_inc(self.sem[e], 1)
        self.ninst += 1
        self._mark(e, (self.sem[e], self.cnt[e]), R, W)

    def dma(self, q, out, in_, sb, R=(), W=(), group=False, **kw):
        if sb.dsem is None:
            sb.dsem = self.es.enter_context(self.nc.semaphore("d%d" % self.nsem))
            self.nsem += 1
            self.dbufs.append(sb)
        key = "d%d" % id(sb)
        deps = self._deps(R, W)
        if group:
            deps = [d for d in deps if d[0] != key]
        self._wait(q, deps)
        ins = self.eng[q].dma_start(out=out, in_=in_, **kw)
        sb.dcnt += 16
        ins.then_inc(sb.dsem, 16)
        self.ninst += 1
        self._mark(key, (sb.dsem, sb.dcnt), R, W)

    def barrier(self):
        deps = [("d%d" % id(b), (b.dsem, b.dcnt)) for b in self.dbufs]
        for e in self.sem:
            deps.append((e, (self.sem[e], self.cnt[e])))
        for e in self.eng:
            self._wait(e, [d for d in deps if d[0] != e])

    def finish(self):
        deps = [("d%d" % id(b), (b.dsem, b.dcnt)) for b in self.dbufs]
        for e in self.sem:
            deps.append((e, (self.sem[e], self.cnt[e])))
        self._wait("sp", deps)


def bc(ap, shape):
    return ap.to_broadcast(list(shape))


def build(S=4096, NL=2, stop=None):
    NT = S // 128
    nc = bass.Bass("TRN2", target_bir_lowering=False)
    es = contextlib.ExitStack()

    def din(name, shape):
        return nc.dram_tensor(name, list(shape), F32, kind="ExternalInput").ap()

    x_d = din("x", [S, D])
    mem_d = din("mem", [MEM, D])
    norm_mix_d = din("norm_mix", [2, D])
    w_in_d = din("w_in", [2, D, NIN])
    gdn_conv_d = din("gdn_conv", [2, 4, 1536])
    a_log_d = din("gdn_a_log", [2, 4])
    dt_bias_d = din("gdn_dt_bias", [2, 4])
    gdn_norm_d = din("gdn_norm", [2, 128])
    w_qb_d = din("dsa_w_qb", [2, 256, 256])
    w_qi_d = din("dsa_w_qi", [2, 256, 256])
    w_kvb_d = din("dsa_w_kvb", [2, 128, 128])
    dq_norm_d = din("dsa_q_norm", [2, 64])
    dk_norm_d = din("dsa_k_norm", [2, 64])
    conv_dw_d = din("conv_dw", [2, 31, 256])
    conv_b_d = din("conv_dw_b", [2, 256])
    ln_g_d = din("conv_ln_g", [2, 256])
    ln_b_d = din("conv_ln_b", [2, 256])
    w_out_d = din("w_out", [2, D, D])
    norm_mem_d = din("norm_mem", [D])
    norm_cross_d = din("norm_cross", [2, D])
    wq_d = din("xa_wq", [2, D, 512])
    wkv_d = din("xa_wkv", [2, D, 1024])
    xq_norm_d = din("xa_q_norm", [2, 128])
    xk_norm_d = din("xa_k_norm", [2, 128])
    wo_d = din("xa_wo", [2, 512, D])
    norm_mlp_d = din("norm_mlp", [2, D])
    w1_d = din("mlp_w1", [2, D, 4096])
    w2_d = din("mlp_w2", [2, 4096, D])
    out_d = nc.dram_tensor("out", [S, D], F32, kind="ExternalOutput").ap()

    k = K(nc, es)
    V, ACT, PE, POOL = nc.vector, nc.scalar, nc.tensor, nc.gpsimd

    def sb(name, shape, dt=F32):
        t = es.enter_context(nc.sbuf_tensor(name, list(shape), dt))
        return t, Buf(name)

    psb = []
    for i in range(8):
        t = es.enter_context(nc.psum_tensor("ps%d" % i, [128, 512], F32))
        psb.append((t, Buf("ps%d" % i)))
    pctr = [0]

    def psum():
        t, b = psb[pctr[0] % 7]
        pctr[0] += 1
        return t, b

    def bfv(t):
        return t[:, :].bitcast(BF16)

    ones_f, onesB = sb("ones_f", [128, 512])
    ident_f, identB = sb("ident_f", [128, 128])
    ident_b, identbB = sb("ident_b", [128, 128], BF16)
    ones128, o128B = sb("ones128", [128, 128])
    ones256, o256B = sb("ones256", [128, 128])
    onesS, onesSB = sb("onesS", [128, 128])
    sel, selB = sb("sel", [4, 4, 128])
    epsc, epsB = sb("epsc", [128, 1])
    onec, oneB = sb("onec", [128, 1])
    FILL0 = POOL.to_reg(0.0)
    FILLM = POOL.to_reg(-1.0e30)
    FILLNC = POOL.to_reg(NEG_NC)
    k.op("pool", POOL.memset, W=[onesB], ap=ones_f[:, :], constant=1.0)
    k.op("pool", POOL.memset, W=[o128B], ap=ones128[:, :], constant=1.0 / 128)
    k.op("pool", POOL.memset, W=[o256B], ap=ones256[:, :], constant=1.0 / 256)
    k.op("pool", POOL.memset, W=[onesSB], ap=onesS[:, :], constant=1.0)
    k.op("pool", POOL.memset, W=[epsB], ap=epsc[:, :], constant=EPS)
    k.op("pool", POOL.memset, W=[oneB], ap=onec[:, :], constant=1.0)
    k.op("pool", POOL.affine_select, R=[onesB], W=[identB], out=ident_f[:, :], in_=ones_f[:, 0:128],
         pattern=[[-1, 128]], compare_op=ALU.is_equal, fill=FILL0, base=0, channel_multiplier=1)
    k.op("pool", POOL.affine_select, R=[onesB], W=[identbB], out=ident_b[:, :], in_=ones_f[:, 0:128],
         pattern=[[-1, 128]], compare_op=ALU.is_equal, fill=FILL0, base=0, channel_multiplier=1)
    k.op("pool", POOL.affine_select, R=[onesB], W=[selB], out=sel[:, :, :],
         in_=ones_f[0:4, :].rearrange("p (a b) -> p a b", a=4),
         pattern=[[-1, 4], [0, 128]], compare_op=ALU.is_equal, fill=FILL0, base=0, channel_multiplier=1)

    NXT = 2
    xts = [sb("xt%d" % i, [128, D]) for i in range(NXT)]
    hb, hbB = sb("hb", [128, D], BF16)
    hT, hTB = sb("hT", [128, 8, ST_B], BF16)
    st5 = [sb("st5_%d" % i, [128, 8]) for i in range(4)]
    stage = [None, None]
    sctr = [0]
    gcol, gcolB = sb("gcol", [128, 8])

    def load_gcol(vec_ap):
        k.dma("sp", gcol[:, :], vec_ap.rearrange("(c p) -> p c", p=128), gcolB, W=[gcolB],
              allow_slow_non_contiguous=True)

    cast_rr = [0]

    def cast_rows(dst_ap, dstB, src_rows_ap, ncols, scol=None):
        for c0 in range(0, ncols, 2048):
            cw = min(2048, ncols - c0)
            stg, stgB = stage[sctr[0] % 2]
            sctr[0] += 1
            k.dma("sp", stg[:, :cw], src_rows_ap[:, c0:c0 + cw], stgB, W=[stgB])
            e = ("pool", "act")[cast_rr[0] % 2]
            cast_rr[0] += 1
            R = [stgB] + ([gcolB] if scol is not None else [])
            if e == "pool":
                if scol is None:
                    k.op("pool", POOL.tensor_copy, R=R, W=[dstB], out=dst_ap[:, c0:c0 + cw], in_=stg[:, :cw])
                else:
                    k.op("pool", POOL.tensor_scalar, R=R, W=[dstB], out=dst_ap[:, c0:c0 + cw], in0=stg[:, :cw],
                         scalar1=scol, scalar2=1.0, op0=ALU.mult, op1=ALU.mult)
            else:
                if scol is None:
                    k.op("act", ACT.copy, R=R, W=[dstB], out=dst_ap[:, c0:c0 + cw], in_=stg[:, :cw])
                else:
                    k.op("act", ACT.activation, R=R, W=[dstB], out=dst_ap[:, c0:c0 + cw], in_=stg[:, :cw],
                         func=AF.Copy, scale=scol)

    def norm_T(xt, xtB, ncol0):
        s5, s5B = st5[ncol0 // 128 % 4]
        k.op("act", ACT.activation, R=[xtB], W=[hbB, s5B], out=hb[:, :], in_=xt[:, :], func=AF.Square,
             accum_out=s5[:, 0:1])
        k.op("dve", V.tensor_scalar, R=[s5B], W=[s5B], out=s5[:, 1:2], in0=s5[:, 0:1], scalar1=1.0 / D,
             scalar2=EPS, op0=ALU.mult, op1=ALU.add)
        k.op("act", ACT.activation, R=[s5B], W=[s5B], out=s5[:, 2:3], in_=s5[:, 1:2], func=AF.Sqrt)
        k.op("dve", V.reciprocal, R=[s5B], W=[s5B], out=s5[:, 3:4], in_=s5[:, 2:3])
        k.op("act", ACT.activation, R=[xtB, s5B], W=[hbB], out=hb[:, :], in_=xt[:, :], func=AF.Copy,
             scale=s5[:, 3:4])
        pt, pB = psum()
        for c in range(8):
            k.op("pe", PE.transpose, R=[hbB, identbB], W=[pB], out=bfv(pt)[:, c * 128:(c + 1) * 128],
                 in_=hb[:, c * 128:(c + 1) * 128], identity=ident_b[:, :])
        k.op("dve", V.tensor_copy, R=[pB], W=[hTB], out=hT[:, :, ncol0:ncol0 + 128],
             in_=bfv(pt).rearrange("p (c t) -> p c t", c=8))

    xdB = [Buf("xd%d" % t) for t in range(NT)]

    for l in range(NL):
        xsrc = x_d if l == 0 else out_d
        with contextlib.ExitStack() as pa:
            def sba(name, shape, dt=F32):
                t = pa.enter_context(nc.sbuf_tensor("%s_l%d" % (name, l), list(shape), dt))
                return t, Buf(name)

            ST = ST_A
            NTS = ST // 128
            NST = S // ST
            WA, WAB = sba("WA", [128, 33536], BF16)
            prep = contextlib.ExitStack()
            for i in range(2):
                stage[i] = (prep.enter_context(nc.sbuf_tensor("stgA%d_%d" % (l, i), [128, 2048], F32)), Buf("stg"))
            w_in = WA[:, 0:8 * NIN].rearrange("p (c n) -> p c n", c=8)
            o1 = 8 * NIN
            w_out = WA[:, o1:o1 + 8 * D].rearrange("p (c n) -> p c n", c=8)
            o2 = o1 + 8 * D
            w_qb = WA[:, o2:o2 + 512].rearrange("p (c n) -> p c n", c=2)
            w_qi = WA[:, o2 + 512:o2 + 1024].rearrange("p (c n) -> p c n", c=2)
            w_kvb = WA[:, o2 + 1024:o2 + 1152]
            load_gcol(norm_mix_d[l])
            for c in range(8):
                cast_rows(w_in[:, c, :], WAB, w_in_d[l, c * 128:(c + 1) * 128, :], NIN, gcol[:, c:c + 1])
            for c in range(8):
                cast_rows(w_out[:, c, :], WAB, w_out_d[l, c * 128:(c + 1) * 128, :], D)
            for c in range(2):
                cast_rows(w_qb[:, c, :], WAB, w_qb_d[l, c * 128:(c + 1) * 128, :], 256)
                cast_rows(w_qi[:, c, :], WAB, w_qi_d[l, c * 128:(c + 1) * 128, :], 256)
            cast_rows(w_kvb, WAB, w_kvb_d[l, :, :], 128)
            k.barrier()
            prep.close()
            cw, cwB = sba("cw", [128, 12, 4])
            for c in range(12):
                k.dma("sp", cw[:, c, :], gdn_conv_d[l][:, c * 128:(c + 1) * 128].rearrange("j p -> p j"), cwB,
                      W=[cwB], group=True, allow_slow_non_contiguous=True)
            cdw, cdwB = sba("cdw", [128, 2, 31])
            for c in range(2):
                k.dma("sp", cdw[:, c, :], conv_dw_d[l][:, c * 128:(c + 1) * 128].rearrange("j p -> p j"), cdwB,
                      W=[cdwB], group=True, allow_slow_non_contiguous=True)
            cvec, cvecB = sba("cvec", [128, 3, 2])
            for i, v in enumerate((conv_b_d, ln_g_d, ln_b_d)):
                k.dma("sp", cvec[:, i, :], v[l].rearrange("(c p) -> p c", p=128), cvecB, W=[cvecB], group=True,
                      allow_slow_non_contiguous=True)
            gnc, gncB = sba("gnc", [128, 1])
            k.dma("sp", gnc[:, :], gdn_norm_d[l].rearrange("(p o) -> p o", o=1), gncB, W=[gncB])
            gv, gvB = sba("gv", [4, 4])
            k.dma("sp", gv[:, 0:1], a_log_d[l].rearrange("(p o) -> p o", o=1), gvB, W=[gvB])
            k.dma("sp", gv[:, 1:2], dt_bias_d[l].rearrange("(p o) -> p o", o=1), gvB, W=[gvB], group=True)
            k.op("act", ACT.activation, R=[gvB], W=[gvB], out=gv[:, 2:3], in_=gv[:, 0:1], func=AF.Exp)
            k.op("dve", V.tensor_scalar, R=[gvB], W=[gvB], out=gv[:, 3:4], in0=gv[:, 2:3], scalar1=-1.0,
                 scalar2=None, op0=ALU.mult)
            gq8, gq8B = sba("gq8", [128, 64])
            gkb, gkbB = sba("gkb", [128, 64])
            k.dma("sp", gq8[:, :], dq_norm_d[l].partition_broadcast(128), gq8B, W=[gq8B])
            k.dma("sp", gkb[:, :], dk_norm_d[l].partition_broadcast(128), gkbB, W=[gkbB])
            k.op("dve", V.tensor_scalar, R=[gq8B], W=[gq8B], out=gq8[:, :], in0=gq8[:, :], scalar1=0.125,
                 scalar2=None, op0=ALU.mult)
            dgc, dgcB = sba("dgc", [128, 2, 31, 128], BF16)
            for i in range(2):
                for j in range(31):
                    k.op("pool", POOL.tensor_scalar, R=[identB, cdwB], W=[dgcB], out=dgc[:, i, j, :],
                         in0=ident_f[:, :], scalar1=cdw[:, i, j:j + 1], scalar2=1.0, op0=ALU.mult, op1=ALU.mult)
            pre, preB = sba("pre", [128, 12, 3 + ST])
            gl, glB = sba("gl", [128, 2, 30 + ST], BF16)
            k.op("pool", POOL.memset, W=[preB], ap=pre[:, :, :], constant=0.0)
            k.op("pool", POOL.memset, W=[glB], ap=gl[:, :, :], constant=0.0)
            qkvT, _ = sba("qkvT", [128, 12, ST])
            qkvB = [Buf("qkv%d" % i) for i in range(12)]
            zs, zsB = sba("zs", [128, 4, ST])
            cqT, cqB = sba("cqT", [128, 2, ST], BF16)
            ckvT, ckvB = sba("ckvT", [128, ST], BF16)
            yT, yTB = sba("yT", [128, 8, ST], BF16)
            Sst = [sba("S%d" % i, [128, 4, 128]) for i in range(2)]
            k.op("pool", POOL.memset, W=[Sst[0][1]], ap=Sst[0][0][:, :, :], constant=0.0)
            KKT, _ = sba("KKT", [128, S], BF16)
            kkB = [Buf("kk%d" % t) for t in range(NT)]
            Vc, _ = sba("Vc", [128, NT, 65], BF16)
            vcB = [Buf("vc%d" % t) for t in range(NT)]
            VcI, VcIB = sba("VcI", [128, 1], BF16)
            work, workB = sba("work", [128, S])
            rows, rowsB = sba("rows", [4, 8, ST])
            cols = [sba("cols%d" % i, [128, 16]) for i in range(2)]
            kkt, kktB = sba("kkt", [128, 2, 128], BF16)
            wab, wabB = sba("wab", [128, 2, 8])
            gd = {n: sba("gd_" + n, [128, 512]) for n in
                  ("Kbd", "Kdec", "Vb", "QKT", "qd", "P0", "P1", "PT0", "PT1", "RT0", "RT1")}
            QQs = [sba("QQ%d" % i, [128, 512], BF16) for i in range(2)]
            mks = [sba("mk%d" % i, [128, 512], BF16) for i in range(2)]
            mkc = [0]
            print("sbuf remaining (phase A)", nc.sbuf_bytes_remaining)
            NSC = 8
            scr = [sba("scr%d" % i, [128, 512]) for i in range(NSC)]
            scrc = [0]

            def scratch():
                t, b = scr[scrc[0] % NSC]
                scrc[0] += 1
                return t, b
            NSB = 6
            scb = [sba("scb%d" % i, [128, 512], BF16) for i in range(NSB)]
            scbc = [0]

            def scratchb():
                t, b = scb[scbc[0] % NSB]
                scbc[0] += 1
                return t, b
            m8s = [sba("m8_%d" % i, [128, 8]) for i in range(2)]
            KB = 26
            pw2, pw2B = sba("pw2", [128, KB])
            for kk_ in range(KB):
                k.op("pool", POOL.memset, W=[pw2B], ap=pw2[:, kk_:kk_ + 1], constant=-(2.0 ** -(kk_ + 1)))
            bst, bstB = sba("bst", [128, 16])
            nst, nstB = sba("nst", [128, KB])
            junk8 = xts[1][0][:, :].bitcast(mybir.dt.int8)
            junkB = xts[1][1]
            sm = [sba("sm%d" % i, [128, 16]) for i in range(4)]
            smc = [0]

            def small():
                t, b = sm[smc[0] % 4]
                smc[0] += 1
                return t, b

            for t in range(NT):
                k.op("pool", POOL.memset, W=[vcB[t]], ap=Vc[:, t, 64:65], constant=1.0)

            def v3(ap, a=4):
                return ap.rearrange("p (a b) -> p a b", a=a)

            for s_i in range(NST):
                if stop == "W":
                    break
                t0 = s_i * ST
                tiles = [s_i * NTS + j for j in range(NTS)]
                for j in range(NTS):
                    xt, xtB = xts[j]
                    k.dma("sp", xt[:, :], xsrc[t0 + j * 128:t0 + (j + 1) * 128, :], xtB, R=[xdB[tiles[j]]],
                          W=[xtB])
                    norm_T(xt, xtB, j * 128)

                def featmajor(col0, M):
                    pt, pB = psum()
                    for c in range(8):
                        k.op("pe", PE.matmul, R=[WAB, hTB], W=[pB], out=pt[:M, :ST],
                             lhsT=w_in[:, c, col0:col0 + M], rhs=hT[:, c, 0:ST], start=(c == 0), stop=(c == 7))
                    return pt, pB

                for i in range(12):
                    pt, pB = featmajor(i * 128, 128)
                    k.op("act", ACT.copy, R=[pB], W=[preB], out=pre[:, i, 3:3 + ST], in_=pt[:, :ST])
                    k.op("dve", V.tensor_scalar, R=[preB, cwB], W=[qkvB[i]], out=qkvT[:, i, :], in0=pre[:, i, 0:ST],
                         scalar1=cw[:, i, 0:1], scalar2=None, op0=ALU.mult)
                    for j in range(1, 4):
                        k.op("dve", V.scalar_tensor_tensor, R=[preB, cwB, qkvB[i]], W=[qkvB[i]], out=qkvT[:, i, :],
                             in0=pre[:, i, j:j + ST], scalar=cw[:, i, j:j + 1], in1=qkvT[:, i, :], op0=ALU.mult,
                             op1=ALU.add)
                    k.op("act", ACT.activation, R=[qkvB[i]], W=[qkvB[i]], out=qkvT[:, i, :], in_=qkvT[:, i, :],
                         func=AF.Silu)
                k.op("dve", V.tensor_copy, R=[preB], W=[preB], out=pre[:, :, 0:3], in_=pre[:, :, ST:ST + 3])
                for i in range(8):
                    sq, sqB = scratch()
                    k.op("act", ACT.activation, R=[qkvB[i]], W=[sqB], out=sq[:, :ST], in_=qkvT[:, i, :],
                         func=AF.Square)
                    pt, pB = psum()
                    k.op("pe", PE.matmul, R=[onesSB, sqB], W=[pB], out=pt[:, :ST], lhsT=onesS[:, :],
                         rhs=sq[:, :ST], start=True, stop=True)
                    k.op("act", ACT.activation, R=[pB, epsB], W=[sqB], out=sq[:, :ST], in_=pt[:, :ST],
                         func=AF.Sqrt, bias=epsc[:, 0:1])
                    k.op("dve", V.reciprocal, R=[sqB], W=[sqB], out=sq[:, :ST], in_=sq[:, :ST])
                    k.op("dve", V.scalar_tensor_tensor, R=[qkvB[i], sqB], W=[qkvB[i]], out=qkvT[:, i, :],
                         in0=qkvT[:, i, :], scalar=(128 ** -0.5 if i < 4 else 1.0), in1=sq[:, :ST],
                         op0=ALU.mult, op1=ALU.mult)
                for i in range(4):
                    pt, pB = featmajor(1536 + i * 128, 128)
                    k.op("act", ACT.activation, R=[pB], W=[zsB], out=zs[:, i, :], in_=pt[:, :ST], func=AF.Silu)
                for i in range(2):
                    pt, pB = featmajor(C_CQ + i * 128, 128)
                    k.op("act", ACT.copy, R=[pB], W=[cqB], out=cqT[:, i, :], in_=pt[:, :ST])
                pt, pB = featmajor(C_CKV, 128)
                k.op("act", ACT.copy, R=[pB], W=[ckvB], out=ckvT[:, :], in_=pt[:, :ST])
                for i in range(2):
                    pu, puB = featmajor(C_UG + i * 128, 128)
                    pg, pgB = featmajor(C_UG + 256 + i * 128, 128)
                    sg, sgB = scratch()
                    k.op("act", ACT.activation, R=[pgB], W=[sgB], out=sg[:, :ST], in_=pg[:, :ST], func=AF.Sigmoid)
                    k.op("dve", V.tensor_tensor, R=[puB, sgB], W=[glB], out=gl[:, i, 30:30 + ST], in0=pu[:, :ST],
                         in1=sg[:, :ST], op=ALU.mult)
                for j in range(NTS):
                    pt, pB = psum()
                    for c in range(8):
                        k.op("pe", PE.matmul, R=[WAB, hTB], W=[pB], out=pt[:, :68],
                             lhsT=hT[:, c, j * 128:(j + 1) * 128], rhs=w_in[:, c, C_KI:C_KI + 68],
                             start=(c == 0), stop=(c == 7))
                    k.op("act", ACT.copy, R=[pB], W=[kktB], out=kkt[:, j, 64:128], in_=pt[:, 0:64])
                    k.op("act", ACT.activation, R=[pB], W=[wabB], out=wab[:, j, 0:4], in_=pt[:, 64:68],
                         func=AF.Abs, scale=1.0 / 16)
                    k.op("act", ACT.activation, R=[pB], W=[wabB], out=wab[:, j, 4:8], in_=pt[:, 64:68],
                         func=AF.Sign)
                if stop == "A1":
                    break
                yc, ycB = scratch()
                ysq, ysqB = scratch()
                for i in range(2):
                    pc, pcB = psum()
                    for jj in range(31):
                        k.op("pe", PE.matmul, R=[dgcB, glB], W=[pcB], out=pc[:, :ST], lhsT=dgc[:, i, jj, :],
                             rhs=gl[:, i, jj:jj + ST], start=(jj == 0), stop=(jj == 30))
                    k.op("act", ACT.activation, R=[pcB, cvecB], W=[ycB], out=yc[:, i * ST:(i + 1) * ST], in_=pc[:, :ST],
                         func=AF.Identity, bias=cvec[:, 0, i:i + 1])
                    k.op("act", ACT.activation, R=[pcB, cvecB], W=[ysqB], out=ysq[:, i * ST:(i + 1) * ST],
                         in_=pc[:, :ST], func=AF.Square, bias=cvec[:, 0, i:i + 1])
                k.op("dve", V.tensor_copy, R=[glB], W=[glB], out=gl[:, :, 0:30], in_=gl[:, :, ST:ST + 30])
                pmu, pmuB = psum()
                pms, pmsB = psum()
                for i in range(2):
                    k.op("pe", PE.matmul, R=[o256B, ycB], W=[pmuB], out=pmu[:, :ST], lhsT=ones256[:, :],
                         rhs=yc[:, i * ST:(i + 1) * ST], start=(i == 0), stop=(i == 1))
                for i in range(2):
                    k.op("pe", PE.matmul, R=[o256B, ysqB], W=[pmsB], out=pms[:, :ST], lhsT=ones256[:, :],
                         rhs=ysq[:, i * ST:(i + 1) * ST], start=(i == 0), stop=(i == 1))
                mu, muB = scratch()
                k.op("act", ACT.copy, R=[pmuB], W=[muB], out=mu[:, 0:ST], in_=pmu[:, :ST])
                k.op("act", ACT.activation, R=[pmuB], W=[muB], out=mu[:, ST:2 * ST], in_=pmu[:, :ST], func=AF.Square)
                k.op("dve", V.tensor_tensor, R=[pmsB, muB], W=[muB], out=mu[:, ST:2 * ST], in0=pms[:, :ST],
                     in1=mu[:, ST:2 * ST], op=ALU.subtract)
                k.op("act", ACT.activation, R=[muB, epsB], W=[muB], out=mu[:, ST:2 * ST], in_=mu[:, ST:2 * ST],
                     func=AF.Sqrt, bias=epsc[:, 0:1])
                k.op("dve", V.reciprocal, R=[muB], W=[muB], out=mu[:, ST:2 * ST], in_=mu[:, ST:2 * ST])
                for i in range(2):
                    k.op("dve", V.tensor_tensor, R=[ycB, muB], W=[ycB], out=yc[:, i * ST:(i + 1) * ST],
                         in0=yc[:, i * ST:(i + 1) * ST], in1=mu[:, 0:ST], op=ALU.subtract)
                    k.op("dve", V.tensor_tensor, R=[ycB, muB], W=[ycB], out=yc[:, i * ST:(i + 1) * ST],
                         in0=yc[:, i * ST:(i + 1) * ST], in1=mu[:, ST:2 * ST], op=ALU.mult)
                    k.op("act", ACT.activation, R=[ycB, cvecB], W=[yTB], out=yT[:, 6 + i, :],
                         in_=yc[:, i * ST:(i + 1) * ST], func=AF.Silu, scale=cvec[:, 1, i:i + 1],
                         bias=cvec[:, 2, i:i + 1])

                def dsa_gen(j):
                    t = tiles[j]
                    cs = slice(j * 128, (j + 1) * 128)
                    pq, pqB = psum()
                    for c in range(2):
                        k.op("pe", PE.matmul, R=[cqB, WAB], W=[pqB], out=pq[:, 0:256], lhsT=cqT[:, c, cs],
                             rhs=w_qb[:, c, :], start=(c == 0), stop=(c == 1))
                    for c in range(2):
                        k.op("pe", PE.matmul, R=[cqB, WAB], W=[pqB], out=pq[:, 256:512], lhsT=cqT[:, c, cs],
                             rhs=w_qi[:, c, :], start=(c == 0), stop=(c == 1))
                    pkv, pkvB = psum()
                    k.op("pe", PE.matmul, R=[ckvB, WAB], W=[pkvB], out=pkv[:, 0:128], lhsT=ckvT[:, cs], rhs=w_kvb,
                         start=True, stop=True)
                    sq, sqB = scratch()
                    k.op("act", ACT.activation, R=[pqB], W=[sqB], out=sq[:, 0:256], in_=pq[:, 0:256], func=AF.Square)
                    k.op("act", ACT.activation, R=[pkvB], W=[sqB], out=sq[:, 256:320], in_=pkv[:, 0:64],
                         func=AF.Square)
                    ss, ssB = small()
                    k.op("dve", V.tensor_reduce, R=[sqB], W=[ssB], out=ss[:, 0:5],
                         in_=sq[:, 0:320].rearrange("p (a b) -> p a b", a=5), axis=AX.X, op=ALU.add)
                    k.op("dve", V.tensor_scalar, R=[ssB], W=[ssB], out=ss[:, 5:10], in0=ss[:, 0:5], scalar1=1.0 / 64,
                         scalar2=EPS, op0=ALU.mult, op1=ALU.add)
                    k.op("act", ACT.activation, R=[ssB], W=[ssB], out=ss[:, 5:10], in_=ss[:, 5:10], func=AF.Sqrt)
                    k.op("dve", V.reciprocal, R=[ssB], W=[ssB], out=ss[:, 10:15], in_=ss[:, 5:10])
                    tq, tqB = scratch()
                    k.op("dve", V.tensor_tensor, R=[pqB, ssB], W=[tqB], out=v3(tq[:, 0:256]), in0=v3(pq[:, 0:256]),
                         in1=bc(ss[:, 10:14].unsqueeze(2), [128, 4, 64]), op=ALU.mult)
                    QI, QIB = scratchb()
                    QI3 = v3(QI[:, :])
                    k.op("dve", V.tensor_tensor, R=[tqB, gq8B], W=[QIB], out=QI3[:, :, 0:64], in0=v3(tq[:, 0:256]),
                         in1=bc(gq8[:, :].unsqueeze(1), [128, 4, 64]), op=ALU.mult)
                    k.op("dve", V.tensor_tensor, R=[pqB, wabB, ssB], W=[QIB], out=QI3[:, :, 64:128], in0=v3(pq[:, 256:512]),
                         in1=bc(wab[:, j, 0:4].unsqueeze(2), [128, 4, 64]), op=ALU.mult)
                    k.op("dve", V.scalar_tensor_tensor, R=[pkvB, ssB, gkbB], W=[kktB], out=kkt[:, j, 0:64],
                         in0=pkv[:, 0:64], scalar=ss[:, 14:15], in1=gkb[:, :], op0=ALU.mult, op1=ALU.mult)
                    k.op("dve", V.tensor_copy, R=[pkvB, ssB], W=[vcB[t]], out=Vc[:, t, 0:64], in_=pkv[:, 64:128])
                    ptq, ptqB = psum()
                    for h in range(4):
                        k.op("pe", PE.transpose, R=[QIB, identbB], W=[ptqB], out=bfv(ptq)[:, h * 128:(h + 1) * 128],
                             in_=QI3[:, h, :], identity=ident_b[:, :])
                    QQ, QQB = QQs[t % 2]
                    k.op("act", ACT.copy, R=[ptqB], W=[QQB], out=QQ[:, :], in_=bfv(ptq)[:, 0:512])
                    ptk, ptkB = psum()
                    k.op("pe", PE.transpose, R=[kktB, identbB], W=[ptkB], out=bfv(ptk)[:, 0:128], in_=kkt[:, j, :],
                         identity=ident_b[:, :])
                    k.op("act", ACT.copy, R=[ptkB], W=[kkB[t]], out=KKT[:, t * 128:(t + 1) * 128],
                         in_=bfv(ptk)[:, 0:128])
                    Sw = (t + 1) * 128
                    for kc0 in range(0, Sw, 512):
                        wd = min(512, Sw - kc0)
                        kts = list(range(kc0 // 128, (kc0 + wd) // 128))
                        rr = []
                        for h in range(4):
                            ph, phB = psum()
                            k.op("pe", PE.matmul, R=[QQB] + [kkB[u] for u in kts], W=[phB], out=ph[:, :wd],
                                 lhsT=QQ[64:128, h * 128:(h + 1) * 128], rhs=KKT[64:128, kc0:kc0 + wd],
                                 start=True, stop=True)
                            rh, rhB = scratch()
                            k.op("act", ACT.activation, R=[phB], W=[rhB], out=rh[:, :wd], in_=ph[:, :wd], func=AF.Relu)
                            rr.append((rh, rhB))
                        k.op("dve", V.tensor_scalar, R=[rr[0][1], wabB], W=[workB], out=work[:, kc0:kc0 + wd],
                             in0=rr[0][0][:, :wd], scalar1=wab[:, j, 4:5], scalar2=None, op0=ALU.mult)
                        for h in range(1, 4):
                            k.op("dve", V.scalar_tensor_tensor, R=[rr[h][1], wabB, workB], W=[workB],
                                 out=work[:, kc0:kc0 + wd], in0=rr[h][0][:, :wd], scalar=wab[:, j, 4 + h:5 + h],
                                 in1=work[:, kc0:kc0 + wd], op0=ALU.mult, op1=ALU.add)
                    if t >= 2:
                        k.op("dve", V.tensor_reduce, R=[workB], W=[bstB], out=bst[:, 0:1], in_=work[:, :Sw], axis=AX.X,
                             op=ALU.min)
                        m8, m8B = m8s[0]
                        k.op("dve", V.max, R=[workB], W=[m8B], out=m8[:, :], in_=work[:, :Sw])
                    k.op("pool", POOL.affine_select, R=[workB], W=[workB], out=work[:, t * 128:(t + 1) * 128],
                         in_=work[:, t * 128:(t + 1) * 128], pattern=[[-1, 128]], compare_op=ALU.is_ge, fill=FILLNC,
                         base=0, channel_multiplier=1)
                    if t >= 2:
                        k.op("dve", V.tensor_tensor, R=[m8B, bstB], W=[bstB], out=bst[:, 2:3], in0=m8[:, 0:1],
                             in1=bst[:, 0:1], op=ALU.subtract)
                        k.op("dve", V.tensor_scalar, R=[bstB, pw2B], W=[nstB], out=nst[:, :], in0=pw2[:, :],
                             scalar1=bst[:, 2:3], scalar2=None, op0=ALU.mult)
                        k.op("dve", V.tensor_scalar, R=[bstB, m8B], W=[bstB], out=bst[:, 4:5], in0=bst[:, 0:1],
                             scalar1=m8[:, 0:1], scalar2=-0.5, op0=ALU.add, op1=ALU.mult)
                        k.op("dve", V.tensor_copy, R=[bstB], W=[bstB], out=bst[:, 3:4], in_=bst[:, 0:1])
                        thr = float(511 - Sw)
                        yield
                        for it in range(KB):
                            nm = bst[:, 4 + it % 2:5 + it % 2]
                            nmn = bst[:, 4 + (it + 1) % 2:5 + (it + 1) % 2]
                            cn = bst[:, 6 + it % 2:7 + it % 2]
                            k.op("act", ACT.activation, R=[workB, bstB], W=[junkB, bstB], out=junk8[:, :Sw],
                                 in_=work[:, :Sw], func=AF.Sign, bias=nm, accum_out=cn)
                            k.op("dve", V.tensor_scalar, R=[bstB], W=[bstB], out=bst[:, 8:9], in0=cn, scalar1=thr,
                                 scalar2=0.5, op0=ALU.is_ge, op1=ALU.subtract)
                            k.op("dve", V.scalar_tensor_tensor, R=[bstB, nstB], W=[bstB], out=nmn, in0=bst[:, 8:9],
                                 scalar=nst[:, it:it + 1], in1=nm, op0=ALU.mult, op1=ALU.add)
                            k.op("dve", V.tensor_scalar, R=[bstB], W=[bstB], out=bst[:, 9:10], in0=bst[:, 8:9],
                                 scalar1=0.5, scalar2=1.0e30, op0=ALU.subtract, op1=ALU.mult)
                            k.op("dve", V.scalar_tensor_tensor, R=[bstB], W=[bstB], out=bst[:, 3:4], in0=bst[:, 9:10],
                                 scalar=nm, in1=bst[:, 3:4], op0=ALU.subtract, op1=ALU.max)
                            yield
                    if t < 2:
                        yield
                    pso, psoB = psb[7]
                    for kc0 in range(0, Sw, 512):
                        wd = min(512, Sw - kc0)
                        mk, mkB = mks[mkc[0] % 2]
                        mkc[0] += 1
                        if t >= 2:
                            k.op("dve", V.tensor_scalar, R=[workB, bstB], W=[mkB], out=mk[:, :wd],
                                 in0=work[:, kc0:kc0 + wd], scalar1=bst[:, 3:4], scalar2=None, op0=ALU.is_ge)
                        else:
                            k.op("dve", V.tensor_single_scalar, R=[workB], W=[mkB], out=mk[:, :wd],
                                 in_=work[:, kc0:kc0 + wd], scalar=-1.0e38, op=ALU.is_gt)
                        for u in range(wd // 128):
                            kt = kc0 // 128 + u
                            pmk, pmkB = psum()
                            k.op("pe", PE.transpose, R=[mkB, identbB], W=[pmkB], out=bfv(pmk)[:, 0:128],
                                 in_=mk[:, u * 128:(u + 1) * 128], identity=ident_b[:, :])
                            pl, plB = psum()
                            k.op("pe", PE.matmul, R=[kkB[kt], QQB], W=[plB], out=pl[:, :],
                                 lhsT=KKT[0:64, kt * 128:(kt + 1) * 128], rhs=QQ[0:64, :], start=True, stop=True)
                            pT_, pTB = scratchb()
                            k.op("act", ACT.activation, R=[plB], W=[pTB], out=pT_[:, :], in_=pl[:, :], func=AF.Exp)
                            pmm, pmmB = scratchb()
                            k.op("dve", V.tensor_tensor, R=[pTB, pmkB], W=[pmmB], out=v3(pmm[:, :]), in0=v3(pT_[:, :]),
                                 in1=bc(bfv(pmk)[:, 0:128].unsqueeze(1), [128, 4, 128]), op=ALU.mult)
                            for h in range(4):
                                k.op("pe", PE.matmul, R=[pmmB, vcB[kt]], W=[psoB], out=pso[:, h * 65:(h + 1) * 65],
                                     lhsT=pmm[:, h * 128:(h + 1) * 128], rhs=Vc[:, kt, :],
                                     start=(kt == 0 and h == 0), stop=(kt == t and h == 3))
                    pso3 = pso[:, 0:260].rearrange("p (a b) -> p a b", a=4)
                    rd, rdB = small()
                    k.op("dve", V.reciprocal, R=[psoB], W=[rdB], out=rd[:, 0:4], in_=pso3[:, :, 64])
                    yb, ybB = scratchb()
                    k.op("dve", V.tensor_tensor, R=[psoB, rdB], W=[ybB], out=v3(yb[:, 0:256]), in0=pso3[:, :, 0:64],
                         in1=bc(rd[:, 0:4].unsqueeze(2), [128, 4, 64]), op=ALU.mult)
                    pty, ptyB = psum()
                    for c in range(2):
                        k.op("pe", PE.transpose, R=[ybB, identbB], W=[ptyB], out=bfv(pty)[:, c * 128:(c + 1) * 128],
                             in_=yb[:, c * 128:(c + 1) * 128], identity=ident_b[:, :])
                    k.op("act", ACT.copy, R=[ptyB], W=[yTB], out=yT[:, 4:6, cs],
                         in_=bfv(pty)[:, 0:256].rearrange("p (a b) -> p a b", a=2))

                dsa_g = dsa_gen(0)
                next(dsa_g)
                nticks = [0]

                def tick(n=1):
                    for _ in range(n):
                        if tiles[0] >= 2 and nticks[0] < KB:
                            nticks[0] += 1
                            next(dsa_g)

                pa_, paB = featmajor(C_A, 4)
                pb_, pbB = featmajor(C_B, 4)
                r = rows
                k.op("act", ACT.activation, R=[paB, gvB], W=[rowsB], out=r[:, 0, :], in_=pa_[:4, :ST],
                     func=AF.Abs, bias=gv[:, 1:2])
                k.op("act", ACT.activation, R=[rowsB], W=[rowsB], out=r[:, 1, :], in_=r[:, 0, :], func=AF.Exp,
                     scale=-1.0)
                k.op("act", ACT.activation, R=[rowsB, oneB], W=[rowsB], out=r[:, 1, :], in_=r[:, 1, :], func=AF.Ln,
                     bias=onec[0:4, 0:1])
                k.op("dve", V.tensor_scalar, R=[paB, gvB], W=[rowsB], out=r[:, 2, :], in0=pa_[:4, :ST],
                     scalar1=gv[:, 1:2], scalar2=0.0, op0=ALU.add, op1=ALU.max)
                k.op("dve", V.tensor_tensor, R=[rowsB], W=[rowsB], out=r[:, 2, :], in0=r[:, 2, :], in1=r[:, 1, :],
                     op=ALU.add)
                k.op("dve", V.tensor_scalar, R=[rowsB, gvB], W=[rowsB], out=r[:, 2, :], in0=r[:, 2, :],
                     scalar1=gv[:, 3:4], scalar2=None, op0=ALU.mult)
                for ch in range(NTS):
                    cs = slice(ch * 128, (ch + 1) * 128)
                    k.op("dve", V.tensor_tensor_scan, R=[rowsB, onesB], W=[rowsB], out=r[:, 3, cs],
                         data0=ones_f[0:4, 0:128], data1=r[:, 2, cs], initial=0.0, op0=ALU.mult, op1=ALU.add)
                k.op("act", ACT.activation, R=[pbB], W=[rowsB], out=r[:, 4, :], in_=pb_[:4, :ST], func=AF.Sigmoid)
                k.op("act", ACT.activation, R=[rowsB], W=[rowsB], out=r[:, 5, :], in_=r[:, 3, :], func=AF.Exp)
                for ch in range(NTS):
                    cs = slice(ch * 128, (ch + 1) * 128)
                    k.op("dve", V.tensor_scalar, R=[rowsB], W=[rowsB], out=r[:, 6, cs], in0=r[:, 3, cs],
                         scalar1=r[:, 3, ch * 128 + 127:ch * 128 + 128], scalar2=-1.0, op0=ALU.subtract,
                         op1=ALU.mult)
                k.op("act", ACT.activation, R=[rowsB], W=[rowsB], out=r[:, 6, :], in_=r[:, 6, :], func=AF.Exp)
                k.op("dve", V.tensor_tensor, R=[rowsB], W=[rowsB], out=r[:, 7, :], in0=r[:, 4, :], in1=r[:, 5, :],
                     op=ALU.mult)

                if stop == "A2":
                    break
                for ch in range(NTS):
                    cs = slice(ch * 128, (ch + 1) * 128)
                    col, colB = cols[ch]
                    pt, pB = psum()
                    for qi_, rq in enumerate((3, 4, 7, 6)):
                        k.op("pe", PE.matmul, R=[rowsB, identB], W=[pB], out=pt[:, qi_ * 4:(qi_ + 1) * 4],
                             lhsT=r[:, rq, cs], rhs=ident_f[0:4, 0:4], start=True, stop=True)
                    k.op("act", ACT.copy, R=[pB], W=[colB], out=col[:, :], in_=pt[:, 0:16])
                    c_b, c_beta, c_be, c_elb = (col[:, 0:4], col[:, 4:8], col[:, 8:12], col[:, 12:16])
                    prb, prbB = psum()
                    pre_, preB_ = psum()
                    for h in range(4):
                        k.op("pe", PE.matmul, R=[selB, rowsB], W=[prbB], out=prb[:, h * 128:(h + 1) * 128],
                             lhsT=sel[:, h, :], rhs=r[:, 3, cs], start=True, stop=True)
                    for h in range(4):
                        k.op("pe", PE.matmul, R=[selB, rowsB], W=[preB_], out=pre_[:, h * 128:(h + 1) * 128],
                             lhsT=sel[:, h, :], rhs=r[:, 5, cs], start=True, stop=True)
                    qd, qdB = gd["qd"]
                    k.op("dve", V.tensor_tensor, R=[preB_] + qkvB[0:4], W=[qdB], out=v3(qd[:, :]),
                         in0=qkvT[:, 0:4, cs], in1=v3(pre_[:, :]), op=ALU.mult)
                    ebl, eblB = small()
                    k.op("dve", V.tensor_copy, R=[preB_], W=[eblB], out=ebl[:, 0:4], in_=v3(pre_[:, :])[:, :, 127])
                    pk, pkB = psum()
                    pv, pvB = psum()
                    for h in range(4):
                        k.op("pe", PE.transpose, R=[qkvB[4 + h], identB], W=[pkB], out=pk[:, h * 128:(h + 1) * 128],
                             in_=qkvT[:, 4 + h, cs], identity=ident_f[:, :])
                    for h in range(4):
                        k.op("pe", PE.transpose, R=[qkvB[8 + h], identB], W=[pvB], out=pv[:, h * 128:(h + 1) * 128],
                             in_=qkvT[:, 8 + h, cs], identity=ident_f[:, :])
                    Kbd, KbdB = gd["Kbd"]
                    Kdec, KdecB = gd["Kdec"]
                    Vb, VbB = gd["Vb"]
                    k.op("dve", V.tensor_tensor, R=[pkB, colB], W=[KbdB], out=v3(Kbd[:, :]), in0=v3(pk[:, :]),
                         in1=bc(c_be.unsqueeze(2), [128, 4, 128]), op=ALU.mult)
                    k.op("dve", V.tensor_tensor, R=[pkB, colB], W=[KdecB], out=v3(Kdec[:, :]), in0=v3(pk[:, :]),
                         in1=bc(c_elb.unsqueeze(2), [128, 4, 128]), op=ALU.mult)
                    k.op("dve", V.tensor_tensor, R=[pvB, colB], W=[VbB], out=v3(Vb[:, :]), in0=v3(pv[:, :]),
                         in1=bc(c_beta.unsqueeze(2), [128, 4, 128]), op=ALU.mult)
                    tick()
                    if stop == "A3a":
                        break
                    pg1, pg1B = psum()
                    pg2, pg2B = psum()
                    for h in range(4):
                        k.op("pe", PE.matmul, R=[qkvB[4 + h]], W=[pg1B], out=pg1[:, h * 128:(h + 1) * 128],
                             lhsT=qkvT[:, 4 + h, cs], rhs=qkvT[:, 4 + h, cs], start=True, stop=True)
                    for h in range(4):
                        k.op("pe", PE.matmul, R=[qkvB[4 + h], qkvB[h]], W=[pg2B], out=pg2[:, h * 128:(h + 1) * 128],
                             lhsT=qkvT[:, 4 + h, cs], rhs=qkvT[:, h, cs], start=True, stop=True)
                    GT, GTB = scratch()
                    k.op("dve", V.tensor_tensor, R=[prbB, colB], W=[GTB], out=v3(GT[:, :]), in0=v3(prb[:, :]),
                         in1=bc(c_b.unsqueeze(2), [128, 4, 128]), op=ALU.subtract)
                    k.op("pool", POOL.affine_select, R=[GTB], W=[GTB], out=v3(GT[:, :]), in_=v3(GT[:, :]),
                         pattern=[[0, 4], [1, 128]], compare_op=ALU.is_ge, fill=FILLM, base=0,
                         channel_multiplier=-1)
                    k.op("act", ACT.activation, R=[GTB], W=[GTB], out=GT[:, :], in_=GT[:, :], func=AF.Exp)
                    tick()
                    QKT, QKTB = gd["QKT"]
                    k.op("dve", V.tensor_tensor, R=[pg2B, GTB], W=[QKTB], out=QKT[:, :], in0=pg2[:, :], in1=GT[:, :],
                         op=ALU.mult)
                    MT, MTB = scratch()
                    k.op("dve", V.tensor_tensor, R=[pg1B, GTB], W=[MTB], out=MT[:, :], in0=pg1[:, :], in1=GT[:, :],
                         op=ALU.mult)
                    k.op("pool", POOL.affine_select, R=[MTB], W=[MTB], out=v3(MT[:, :]), in_=v3(MT[:, :]),
                         pattern=[[0, 4], [1, 128]], compare_op=ALU.is_gt, fill=FILL0, base=0, channel_multiplier=-1)
                    tick()
                    if stop == "A3b":
                        break
                    pA, pAB = psum()
                    for h in range(4):
                        k.op("pe", PE.transpose, R=[MTB, identB], W=[pAB], out=pA[:, h * 128:(h + 1) * 128],
                             in_=MT[:, h * 128:(h + 1) * 128], identity=ident_f[:, :])
                    P, PB = gd["P0"]
                    k.op("dve", V.tensor_tensor, R=[pAB, colB], W=[PB], out=v3(P[:, :]), in0=v3(pA[:, :]),
                         in1=bc(c_beta.unsqueeze(2), [128, 4, 128]), op=ALU.mult)
                    tick()
                    if stop == "A3b1":
                        break
                    pAT, pATB = psum()
                    for h in range(4):
                        k.op("pe", PE.transpose, R=[PB, identB], W=[pATB], out=pAT[:, h * 128:(h + 1) * 128],
                             in_=P[:, h * 128:(h + 1) * 128], identity=ident_f[:, :])
                    if stop == "A3b1a":
                        break
                    PT_, PTB = gd["PT0"]
                    k.op("act", ACT.copy, R=[pATB], W=[PTB], out=PT_[:, :], in_=pAT[:, :])
                    if stop == "A3b1b":
                        break
                    RT, RTB = gd["RT0"]
                    for h in range(4):
                        hs = slice(h * 128, (h + 1) * 128)
                        k.op("dve", V.tensor_tensor, R=[identB, PTB], W=[RTB], out=RT[:, hs], in0=ident_f[:, :],
                             in1=PT_[:, hs], op=ALU.subtract)
                    tick()
                    if stop == "A3b2":
                        break
                    for lvl in range(6):
                        if stop == "A3b3" and lvl == 1:
                            break
                        p2, p2B = psum()
                        for h in range(4):
                            hs = slice(h * 128, (h + 1) * 128)
                            k.op("pe", PE.matmul, R=[PTB, PB], W=[p2B], out=p2[:, hs], lhsT=PT_[:, hs], rhs=P[:, hs],
                                 start=True, stop=True)
                        if lvl < 5:
                            p2t, p2tB = psum()
                            for h in range(4):
                                hs = slice(h * 128, (h + 1) * 128)
                                k.op("pe", PE.matmul, R=[PTB, PB], W=[p2tB], out=p2t[:, hs], lhsT=P[:, hs],
                                     rhs=PT_[:, hs], start=True, stop=True)
                        Pn, PnB = gd["P%d" % ((lvl + 1) % 2)]
                        k.op("act", ACT.copy, R=[p2B], W=[PnB], out=Pn[:, :], in_=p2[:, :])
                        tick()
                        if lvl < 5:
                            PTn, PTnB = gd["PT%d" % ((lvl + 1) % 2)]
                            k.op("act", ACT.copy, R=[p2tB], W=[PTnB], out=PTn[:, :], in_=p2t[:, :])
                        p3, p3B = psum()
                        for h in range(4):
                            hs = slice(h * 128, (h + 1) * 128)
                            k.op("pe", PE.matmul, R=[PnB, RTB], W=[p3B], out=p3[:, hs], lhsT=Pn[:, hs], rhs=RT[:, hs],
                                 start=True, stop=True)
                        RTn, RTnB = gd["RT%d" % ((lvl + 1) % 2)]
                        k.op("dve", V.tensor_tensor, R=[p3B, RTB], W=[RTnB], out=RTn[:, :], in0=p3[:, :], in1=RT[:, :],
                             op=ALU.add)
                        tick(2)
                        RT, RTB = RTn, RTnB
                        P, PB = Pn, PnB
                        if lvl < 5:
                            PT_, PTB = PTn, PTnB
                    if stop in ("A3c", "A3b3"):
                        break
                    pw, pwB = psum()
                    pu_, puB_ = psum()
                    for h in range(4):
                        hs = slice(h * 128, (h + 1) * 128)
                        k.op("pe", PE.matmul, R=[KbdB, RTB], W=[pwB], out=pw[:, hs], lhsT=Kbd[:, hs], rhs=RT[:, hs],
                             start=True, stop=True)
                    for h in range(4):
                        hs = slice(h * 128, (h + 1) * 128)
                        k.op("pe", PE.matmul, R=[VbB, RTB], W=[puB_], out=pu_[:, hs], lhsT=RT[:, hs], rhs=Vb[:, hs],
                             start=True, stop=True)
                    WT, WTB = scratch()
                    U, UB = scratch()
                    k.op("act", ACT.copy, R=[pwB], W=[WTB], out=WT[:, :], in_=pw[:, :])
                    k.op("act", ACT.copy, R=[puB_], W=[UB], out=U[:, :], in_=pu_[:, :])
                    tick()
                    tg = NTS * s_i + ch
                    Sc, ScB = Sst[tg % 2]
                    Sn, SnB = Sst[(tg + 1) % 2]
                    pws, pwsB = psum()
                    for h in range(4):
                        hs = slice(h * 128, (h + 1) * 128)
                        k.op("pe", PE.matmul, R=[WTB, ScB], W=[pwsB], out=pws[:, hs], lhsT=WT[:, hs], rhs=Sc[:, h, :],
                             start=True, stop=True)
                    Vn, VnB = scratch()
                    k.op("dve", V.scalar_tensor_tensor, R=[UB, pwsB], W=[VnB], out=Vn[:, :], in0=pws[:, :], scalar=-1.0,
                         in1=U[:, :], op0=ALU.mult, op1=ALU.add)
                    po, poB = psum()
                    for h in range(4):
                        hs = slice(h * 128, (h + 1) * 128)
                        k.op("pe", PE.matmul, R=[ScB, qdB], W=[poB], out=po[:, hs], lhsT=Sc[:, h, :], rhs=qd[:, hs],
                             start=True, stop=False)
                        k.op("pe", PE.matmul, R=[VnB, QKTB], W=[poB], out=po[:, hs], lhsT=Vn[:, hs], rhs=QKT[:, hs],
                             start=False, stop=True)
                    ps_, psB_ = psum()
                    for h in range(4):
                        hs = slice(h * 128, (h + 1) * 128)
                        k.op("pe", PE.matmul, R=[KdecB, VnB], W=[psB_], out=ps_[:, hs], lhsT=Kdec[:, hs], rhs=Vn[:, hs],
                             start=True, stop=True)
                    k.op("dve", V.tensor_tensor, R=[ScB, eblB], W=[SnB], out=Sn[:, :, :], in0=Sc[:, :, :],
                         in1=bc(ebl[:, 0:4].unsqueeze(2), [128, 4, 128]), op=ALU.mult)
                    k.op("dve", V.tensor_tensor, R=[SnB, psB_], W=[SnB], out=Sn[:, :, :], in0=Sn[:, :, :],
                         in1=v3(ps_[:, :]), op=ALU.add)
                    tick()
                    o32, o32B = scratch()
                    osq, osqB = scratch()
                    k.op("act", ACT.copy, R=[poB], W=[o32B], out=o32[:, :], in_=po[:, :])
                    k.op("act", ACT.activation, R=[poB], W=[osqB], out=osq[:, :], in_=po[:, :], func=AF.Square)
                    pm_, pmB_ = psum()
                    k.op("pe", PE.matmul, R=[o128B, osqB], W=[pmB_], out=pm_[:, :], lhsT=ones128[:, :], rhs=osq[:, :],
                         start=True, stop=True)
                    k.op("act", ACT.activation, R=[pmB_, epsB], W=[osqB], out=osq[:, :], in_=pm_[:, :], func=AF.Sqrt,
                         bias=epsc[:, 0:1])
                    k.op("dve", V.reciprocal, R=[osqB], W=[osqB], out=osq[:, :], in_=osq[:, :])
                    k.op("dve", V.scalar_tensor_tensor, R=[o32B, osqB, gncB], W=[o32B], out=o32[:, :], in0=o32[:, :],
                         scalar=gnc[:, 0:1], in1=osq[:, :], op0=ALU.mult, op1=ALU.mult)
                    k.op("dve", V.tensor_tensor, R=[o32B, zsB], W=[yTB], out=yT[:, 0:4, cs], in0=v3(o32[:, :]),
                         in1=zs[:, :, cs], op=ALU.mult)

                if stop in ("A3", "A3a", "A3b", "A3c", "A3b1", "A3b2", "A3b3", "A3b1a", "A3b1b"):
                    break
                for _ in dsa_g:
                    pass
                if stop == "A4":
                    break
                for j in range(NTS):
                    xt, xtB = xts[j]
                    t = tiles[j]
                    for n in range(2):
                        pt, pB = psum()
                        for c in range(8):
                            k.op("pe", PE.matmul, R=[yTB, WAB], W=[pB], out=pt[:, :],
                                 lhsT=yT[:, c, j * 128:(j + 1) * 128], rhs=w_out[:, c, n * 512:(n + 1) * 512],
                                 start=(c == 0), stop=(c == 7))
                        k.op("dve", V.tensor_tensor, R=[pB, xtB], W=[xtB], out=xt[:, n * 512:(n + 1) * 512],
                             in0=pt[:, :], in1=xt[:, n * 512:(n + 1) * 512], op=ALU.add)
                    k.dma("sp", out_d[t * 128:(t + 1) * 128, :], xt[:, :], xtB, R=[xtB], W=[xdB[t]])
        k.barrier()
        if stop in ("A", "W", "A1", "A2", "A3", "A3a", "A3b", "A3c", "A4", "A3b1", "A3b2", "A3b3", "A3b1a", "A3b1b"):
            break

        with contextlib.ExitStack() as pa:
            def sba(name, shape, dt=F32):
                t = pa.enter_context(nc.sbuf_tensor("%s_x%d" % (name, l), list(shape), dt))
                return t, Buf(name)
            WA, WAB = sba("WA", [128, 16384], BF16)
            for i in range(2):
                stage[i] = sba("stgX%d" % i, [128, 2048])
            wq = WA[:, 0:8 * 512].rearrange("p (c n) -> p c n", c=8)
            wkv = WA[:, 4096:4096 + 8 * 1024].rearrange("p (c n) -> p c n", c=8)
            wo = WA[:, 12288:12288 + 4 * 1024].rearrange("p (c n) -> p c n", c=4)
            load_gcol(norm_cross_d[l])
            for c in range(8):
                cast_rows(wq[:, c, :], WAB, wq_d[l, c * 128:(c + 1) * 128, :], 512, gcol[:, c:c + 1])
            load_gcol(norm_mem_d)
            for c in range(8):
                cast_rows(wkv[:, c, :], WAB, wkv_d[l, c * 128:(c + 1) * 128, :], 1024, gcol[:, c:c + 1])
            for c in range(4):
                cast_rows(wo[:, c, :], WAB, wo_d[l, c * 128:(c + 1) * 128, :], D)
            gxq, gxqB = sba("gxq", [128, 128])
            gxk, gxkB = sba("gxk", [128, 128])
            k.dma("sp", gxq[:, :], xq_norm_d[l].partition_broadcast(128), gxqB, W=[gxqB])
            k.dma("sp", gxk[:, :], xk_norm_d[l].partition_broadcast(128), gxkB, W=[gxkB])
            k.op("dve", V.tensor_scalar, R=[gxqB], W=[gxqB], out=gxq[:, :], in0=gxq[:, :], scalar1=128 ** -0.5,
                 scalar2=None, op0=ALU.mult)
            kmT, kmTB = sba("kmT", [128, 4, MEM], BF16)
            vm1, vm1B = sba("vm1", [128, 2, 4, 129], BF16)
            k.op("pool", POOL.memset, W=[vm1B], ap=vm1[:, :, :, :], constant=1.0)
            scr = [sba("xscr%d" % i, [128, 512]) for i in range(4)]
            scb = [sba("xscb%d" % i, [128, 1024], BF16) for i in range(4)]
            sm = [sba("xsm%d" % i, [128, 16]) for i in range(4)]
            cx = [0, 0, 0]

            def scratch():
                cx[0] += 1
                return scr[cx[0] % 4]

            def scratchb():
                cx[1] += 1
                return scb[cx[1] % 4]

            def small():
                cx[2] += 1
                return sm[cx[2] % 4]

            def v3(ap, a=4):
                return ap.rearrange("p (a b) -> p a b", a=a)

            def head_rms(pt, pB, gtile, gB, dst3, dstB):
                sq, sqB = scratch()
                k.op("act", ACT.activation, R=[pB], W=[sqB], out=sq[:, :], in_=pt[:, :], func=AF.Square)
                ss, ssB = small()
                k.op("dve", V.tensor_reduce, R=[sqB], W=[ssB], out=ss[:, 0:4], in_=v3(sq[:, :]), axis=AX.X, op=ALU.add)
                k.op("dve", V.tensor_scalar, R=[ssB], W=[ssB], out=ss[:, 4:8], in0=ss[:, 0:4], scalar1=1.0 / 128,
                     scalar2=EPS, op0=ALU.mult, op1=ALU.add)
                k.op("act", ACT.activation, R=[ssB], W=[ssB], out=ss[:, 4:8], in_=ss[:, 4:8], func=AF.Sqrt)
                k.op("dve", V.reciprocal, R=[ssB], W=[ssB], out=ss[:, 8:12], in_=ss[:, 4:8])
                k.op("dve", V.tensor_tensor, R=[pB, ssB], W=[sqB], out=v3(sq[:, :]), in0=v3(pt[:, :]),
                     in1=bc(ss[:, 8:12].unsqueeze(2), [128, 4, 128]), op=ALU.mult)
                k.op("dve", V.tensor_tensor, R=[sqB, gB], W=[dstB], out=dst3, in0=v3(sq[:, :]),
                     in1=bc(gtile[:, :].unsqueeze(1), [128, 4, 128]), op=ALU.mult)

            for mt in range(2):
                xt, xtB = xts[mt]
                k.dma("sp", xt[:, :], mem_d[mt * 128:(mt + 1) * 128, :], xtB, W=[xtB])
                norm_T(xt, xtB, 0)
                pk_, pkB_ = psum()
                pv_, pvB_ = psum()
                for c in range(8):
                    k.op("pe", PE.matmul, R=[hTB, WAB], W=[pkB_], out=pk_[:, :], lhsT=hT[:, c, 0:128],
                         rhs=wkv[:, c, 0:512], start=(c == 0), stop=(c == 7))
                for c in range(8):
                    k.op("pe", PE.matmul, R=[hTB, WAB], W=[pvB_], out=pv_[:, :], lhsT=hT[:, c, 0:128],
                         rhs=wkv[:, c, 512:1024], start=(c == 0), stop=(c == 7))
                kn, knB = scratchb()
                head_rms(pk_, pkB_, gxk, gxkB, v3(kn[:, 0:512]), knB)
                ptk, ptkB = psum()
                for h in range(4):
                    k.op("pe", PE.transpose, R=[knB, identbB], W=[ptkB], out=bfv(ptk)[:, h * 128:(h + 1) * 128],
                         in_=kn[:, h * 128:(h + 1) * 128], identity=ident_b[:, :])
                k.op("act", ACT.copy, R=[ptkB], W=[kmTB], out=kmT[:, :, mt * 128:(mt + 1) * 128],
                     in_=v3(bfv(ptk)[:, 0:512]))
                k.op("act", ACT.copy, R=[pvB_], W=[vm1B], out=vm1[:, mt, :, 0:128], in_=v3(pv_[:, :]))

            for t in range(NT):
                xt, xtB = xts[t % 2]
                k.dma("sp", xt[:, :], out_d[t * 128:(t + 1) * 128, :], xtB, R=[xdB[t]], W=[xtB])
                norm_T(xt, xtB, 0)
                pq, pqB = psum()
                for c in range(8):
                    k.op("pe", PE.matmul, R=[hTB, WAB], W=[pqB], out=pq[:, :], lhsT=hT[:, c, 0:128], rhs=wq[:, c, :],
                         start=(c == 0), stop=(c == 7))
                qn, qnB = scratchb()
                head_rms(pq, pqB, gxq, gxqB, v3(qn[:, 0:512]), qnB)
                ptq, ptqB = psum()
                for h in range(4):
                    k.op("pe", PE.transpose, R=[qnB, identbB], W=[ptqB], out=bfv(ptq)[:, h * 128:(h + 1) * 128],
                         in_=qn[:, h * 128:(h + 1) * 128], identity=ident_b[:, :])
                qT, qTB = scratchb()
                k.op("act", ACT.copy, R=[ptqB], W=[qTB], out=qT[:, 0:512], in_=bfv(ptq)[:, 0:512])
                pT_, pTB = scratchb()
                for mt in range(2):
                    pl, plB = psum()
                    for h in range(4):
                        k.op("pe", PE.matmul, R=[kmTB, qTB], W=[plB], out=pl[:, h * 128:(h + 1) * 128],
                             lhsT=kmT[:, h, mt * 128:(mt + 1) * 128], rhs=qT[:, h * 128:(h + 1) * 128], start=True,
                             stop=True)
                    k.op("act", ACT.activation, R=[plB], W=[pTB], out=pT_[:, mt * 512:(mt + 1) * 512], in_=pl[:, :],
                         func=AF.Exp)
                on, onB = scratchb()
                for hh in range(2):
                    po, poB = psum()
                    for h2 in range(2):
                        h = hh * 2 + h2
                        for mt in range(2):
                            k.op("pe", PE.matmul, R=[pTB, vm1B], W=[poB], out=po[:, h2 * 129:(h2 + 1) * 129],
                                 lhsT=pT_[:, mt * 512 + h * 128:mt * 512 + (h + 1) * 128], rhs=vm1[:, mt, h, :],
                                 start=(mt == 0), stop=(mt == 1))
                    po3 = po[:, 0:258].rearrange("p (a b) -> p a b", a=2)
                    rd, rdB = small()
                    k.op("dve", V.reciprocal, R=[poB], W=[rdB], out=rd[:, 0:2], in_=po3[:, :, 128])
                    k.op("dve", V.tensor_tensor, R=[poB, rdB], W=[onB],
                         out=on[:, hh * 256:(hh + 1) * 256].rearrange("p (a b) -> p a b", a=2), in0=po3[:, :, 0:128],
                         in1=bc(rd[:, 0:2].unsqueeze(2), [128, 2, 128]), op=ALU.mult)
                pto, ptoB = psum()
                for h in range(4):
                    k.op("pe", PE.transpose, R=[onB, identbB], W=[ptoB], out=bfv(pto)[:, h * 128:(h + 1) * 128],
                         in_=on[:, h * 128:(h + 1) * 128], identity=ident_b[:, :])
                oT, oTB = scratchb()
                k.op("act", ACT.copy, R=[ptoB], W=[oTB], out=oT[:, 0:512], in_=bfv(pto)[:, 0:512])
                for n in range(2):
                    pt, pB = psum()
                    for h in range(4):
                        k.op("pe", PE.matmul, R=[oTB, WAB], W=[pB], out=pt[:, :], lhsT=oT[:, h * 128:(h + 1) * 128],
                             rhs=wo[:, h, n * 512:(n + 1) * 512], start=(h == 0), stop=(h == 3))
                    k.op("dve", V.tensor_tensor, R=[pB, xtB], W=[xtB], out=xt[:, n * 512:(n + 1) * 512], in0=pt[:, :],
                         in1=xt[:, n * 512:(n + 1) * 512], op=ALU.add)
                k.dma("sp", out_d[t * 128:(t + 1) * 128, :], xt[:, :], xtB, R=[xtB], W=[xdB[t]])
        k.barrier()
        if stop == "A2":
            break

        with contextlib.ExitStack() as pa:
            def sba(name, shape, dt=F32):
                t = pa.enter_context(nc.sbuf_tensor("%s_m%d" % (name, l), list(shape), dt))
                return t, Buf(name)
            ST = ST_B
            NST = S // ST
            WA, WAB = sba("WA", [128, 65536], BF16)
            for i in range(2):
                stage[i] = sba("stgM%d" % i, [128, 2048])
            w1 = WA[:, 0:8 * 4096].rearrange("p (c n) -> p c n", c=8)
            w2 = WA[:, 32768:32768 + 32 * 1024].rearrange("p (c n) -> p c n", c=32)
            load_gcol(norm_mlp_d[l])
            for c in range(8):
                cast_rows(w1[:, c, :], WAB, w1_d[l, c * 128:(c + 1) * 128, :], 4096, gcol[:, c:c + 1])
            for c in range(32):
                cast_rows(w2[:, c, :], WAB, w2_d[l, c * 128:(c + 1) * 128, :], D)
            aT, aTB = sba("aT", [128, 32, ST], BF16)
            rrs = [sba("rr%d" % i, [128, ST]) for i in range(3)]
            for s_i in range(NST):
                t0 = s_i * ST
                for j in range(2):
                    xt, xtB = xts[j]
                    k.dma("sp", xt[:, :], out_d[t0 + j * 128:t0 + (j + 1) * 128, :], xtB, R=[xdB[2 * s_i + j]],
                          W=[xtB])
                    norm_T(xt, xtB, j * 128)
                for f in range(32):
                    pt, pB = psum()
                    for c in range(8):
                        k.op("pe", PE.matmul, R=[WAB, hTB], W=[pB], out=pt[:, :ST], lhsT=w1[:, c, f * 128:(f + 1) * 128],
                             rhs=hT[:, c, :], start=(c == 0), stop=(c == 7))
                    rr_, rrB = rrs[f % 3]
                    k.op("act", ACT.activation, R=[pB], W=[rrB], out=rr_[:, :], in_=pt[:, :ST], func=AF.Relu)
                    e = "pool" if f % 2 == 0 else "dve"
                    k.op(e, k.eng[e].tensor_tensor, R=[rrB], W=[aTB], out=aT[:, f, :], in0=rr_[:, :], in1=rr_[:, :],
                         op=ALU.mult)
                for j in range(2):
                    xt, xtB = xts[j]
                    t = 2 * s_i + j
                    for n in range(2):
                        pt, pB = psum()
                        for f in range(32):
                            k.op("pe", PE.matmul, R=[aTB, WAB], W=[pB], out=pt[:, :],
                                 lhsT=aT[:, f, j * 128:(j + 1) * 128], rhs=w2[:, f, n * 512:(n + 1) * 512],
                                 start=(f == 0), stop=(f == 31))
                        k.op("dve", V.tensor_tensor, R=[pB, xtB], W=[xtB], out=xt[:, n * 512:(n + 1) * 512],
                             in0=pt[:, :], in1=xt[:, n * 512:(n + 1) * 512], op=ALU.add)
                    k.dma("sp", out_d[t * 128:(t + 1) * 128, :], xt[:, :], xtB, R=[xtB], W=[xdB[t]])
        k.barrier()
    k.finish()
    es.close()
    return nc, k


_INPUT_NAMES = ["x", "mem", "norm_mix", "w_in", "gdn_conv", "gdn_a_log", "gdn_dt_bias", "gdn_norm", "dsa_w_qb",
                "dsa_w_qi", "dsa_w_kvb", "dsa_q_norm", "dsa_k_norm", "conv_dw", "conv_dw_b", "conv_ln_g",
                "conv_ln_b", "w_out", "norm_mem", "norm_cross", "xa_wq", "xa_wkv", "xa_q_norm", "xa_k_norm",
                "xa_wo", "norm_mlp", "mlp_w1", "mlp_w2"]


def run(inputs, S=4096, NL=2, stop=None, ncores=8, trace=False):
    nc, kk = build(S=S, NL=NL, stop=stop)
    shared = {n: np.ascontiguousarray(np.asarray(inputs[n], dtype=np.float32)) for n in _INPUT_NAMES
              if n not in ("x", "mem")}
    x = np.asarray(inputs["x"], dtype=np.float32)
    mem = np.asarray(inputs["mem"], dtype=np.float32)
    in_maps = []
    for b in range(ncores):
        m = dict(shared)
        m["x"] = np.ascontiguousarray(x[b, :S])
        m["mem"] = np.ascontiguousarray(mem[b])
        in_maps.append(m)
    res = run_bass_kernel_spmd(nc, in_maps, core_ids=list(range(ncores)), trace=trace)
    out = np.stack([np.asarray(r["out"]) for r in res.results], axis=0)
    return out, res


def kernel(**inputs):
    out, _ = run(inputs)
    return out.astype(np.float32)
```

```python
import contextlib
import numpy as np
import concourse.bass as bass
import concourse.mybir as mybir
from concourse.bass_utils import run_bass_kernel_spmd

F32 = mybir.dt.float32
BF16 = mybir.dt.bfloat16
AF = mybir.ActivationFunctionType
ALU = mybir.AluOpType
AX = mybir.AxisListType

D = 1024
NIN = 3020
MEM = 256
EPS = 1e-6
C_A, C_B, C_CQ, C_CKV, C_KI, C_WI, C_UG = 2048, 2052, 2056, 2312, 2440, 2504, 2508
NEG_NC = -2.0e38
NEG_SEL = -3.0e38
ST_A = 128
ST_B = 256


class Buf:
    __slots__ = ("w", "r", "dsem", "dcnt", "name")

    def __init__(self, name=""):
        self.w = {}
        self.r = {}
        self.dsem = None
        self.dcnt = 0
        self.name = name


class K:
    def __init__(self, nc, es):
        self.nc = nc
        self.es = es
        self.eng = {"pe": nc.tensor, "act": nc.scalar, "dve": nc.vector, "pool": nc.gpsimd, "sp": nc.sync}
        self.sem = {e: es.enter_context(nc.semaphore("s_" + e)) for e in ("pe", "act", "dve", "pool")}
        self.cnt = {e: 0 for e in self.sem}
        self.waited = {e: {} for e in self.eng}
        self.nsem = 0
        self.dbufs = []
        self.ninst = 0

    def _wait(self, e, deps):
        need = {}
        for key, (sem, val) in deps:
            if key == "pe" and e == "pe":
                continue
            if need.get(key, (None, 0))[1] < val:
                need[key] = (sem, val)
        wd = self.waited[e]
        for key, (sem, val) in need.items():
            if wd.get(key, 0) >= val:
                continue
            self.eng[e].wait_ge(sem, val)
            wd[key] = val
            self.ninst += 1

    @staticmethod
    def _deps(reads, writes):
        deps = []
        for b in reads:
            deps.extend(b.w.items())
        for b in writes:
            deps.extend(b.w.items())
            deps.extend(b.r.items())
        return deps

    @staticmethod
    def _mark(tokkey, tok, reads, writes):
        for b in reads:
            b.r[tokkey] = tok
        for b in writes:
            if b.r:
                b.w = {}
                b.r = {}
            b.w[tokkey] = tok

    def op(self, e, fn, R=(), W=(), **kw):
        self._wait(e, self._deps(R, W))
        ins = fn(**kw)
        self.cnt[e] += 1
        ins.then_inc(self.sem[e], 1)
        self.ninst += 1
        self._mark(e, (self.sem[e], self.cnt[e]), R, W)

    def dma(self, q, out, in_, sb, R=(), W=(), group=False, **kw):
        if sb.dsem is None:
            sb.dsem = self.es.enter_context(self.nc.semaphore("d%d" % self.nsem))
            self.nsem += 1
            self.dbufs.append(sb)
        key = "d%d" % id(sb)
        deps = self._deps(R, W)
        if group:
            deps = [d for d in deps if d[0] != key]
        self._wait(q, deps)
        ins = self.eng[q].dma_start(out=out, in_=in_, **kw)
        sb.dcnt += 16
        ins.then_inc(sb.dsem, 16)
        self.ninst += 1
        self._mark(key, (sb.dsem, sb.dcnt), R, W)

    def barrier(self):
        deps = [("d%d" % id(b), (b.dsem, b.dcnt)) for b in self.dbufs]
        for e in self.sem:
            deps.append((e, (self.sem[e], self.cnt[e])))
        for e in self.eng:
            self._wait(e, [d for d in deps if d[0] != e])

    def finish(self):
        deps = [("d%d" % id(b), (b.dsem, b.dcnt)) for b in self.dbufs]
        for e in self.sem:
            deps.append((e, (self.sem[e], self.cnt[e])))
        self._wait("sp", deps)


def bc(ap, shape):
    return ap.to_broadcast(list(shape))


def build(S=4096, NL=2, stop=None):
    NT = S // 128
    nc = bass.Bass("TRN2", target_bir_lowering=False)
    es = contextlib.ExitStack()

    def din(name, shape):
        return nc.dram_tensor(name, list(shape), F32, kind="ExternalInput").ap()

    x_d = din("x", [S, D])
    mem_d = din("mem", [MEM, D])
    norm_mix_d = din("norm_mix", [2, D])
    w_in_d = din("w_in", [2, D, NIN])
    gdn_conv_d = din("gdn_conv", [2, 4, 1536])
    a_log_d = din("gdn_a_log", [2, 4])
    dt_bias_d = din("gdn_dt_bias", [2, 4])
    gdn_norm_d = din("gdn_norm", [2, 128])
    w_qb_d = din("dsa_w_qb", [2, 256, 256])
    w_qi_d = din("dsa_w_qi", [2, 256, 256])
    w_kvb_d = din("dsa_w_kvb", [2, 128, 128])
    dq_norm_d = din("dsa_q_norm", [2, 64])
    dk_norm_d = din("dsa_k_norm", [2, 64])
    conv_dw_d = din("conv_dw", [2, 31, 256])
    conv_b_d = din("conv_dw_b", [2, 256])
    ln_g_d = din("conv_ln_g", [2, 256])
    ln_b_d = din("conv_ln_b", [2, 256])
    w_out_d = din("w_out", [2, D, D])
    norm_mem_d = din("norm_mem", [D])
    norm_cross_d = din("norm_cross", [2, D])
    wq_d = din("xa_wq", [2, D, 512])
    wkv_d = din("xa_wkv", [2, D, 1024])
    xq_norm_d = din("xa_q_norm", [2, 128])
    xk_norm_d = din("xa_k_norm", [2, 128])
    wo_d = din("xa_wo", [2, 512, D])
    norm_mlp_d = din("norm_mlp", [2, D])
    w1_d = din("mlp_w1", [2, D, 4096])
    w2_d = din("mlp_w2", [2, 4096, D])
    out_d = nc.dram_tensor("out", [S, D], F32, kind="ExternalOutput").ap()

    k = K(nc, es)
    V, ACT, PE, POOL = nc.vector, nc.scalar, nc.tensor, nc.gpsimd

    def sb(name, shape, dt=F32):
        t = es.enter_context(nc.sbuf_tensor(name, list(shape), dt))
        return t, Buf(name)

    psb = []
    for i in range(8):
        t = es.enter_context(nc.psum_tensor("ps%d" % i, [128, 512], F32))
        psb.append((t, Buf("ps%d" % i)))
    pctr = [0]

    def psum():
        t, b = psb[pctr[0] % 7]
        pctr[0] += 1
        return t, b

    def bfv(t):
        return t[:, :].bitcast(BF16)

    ones_f, onesB = sb("ones_f", [128, 512])
    ident_f, identB = sb("ident_f", [128, 128])
    ident_b, identbB = sb("ident_b", [128, 128], BF16)
    ones128, o128B = sb("ones128", [128, 128])
    ones256, o256B = sb("ones256", [128, 128])
    onesS, onesSB = sb("onesS", [128, 128])
    sel, selB = sb("sel", [4, 4, 128])
    epsc, epsB = sb("epsc", [128, 1])
    onec, oneB = sb("onec", [128, 1])
    FILL0 = POOL.to_reg(0.0)
    FILLM = POOL.to_reg(-1.0e30)
    FILLNC = POOL.to_reg(NEG_NC)
    k.op("pool", POOL.memset, W=[onesB], ap=ones_f[:, :], constant=1.0)
    k.op("pool", POOL.memset, W=[o128B], ap=ones128[:, :], constant=1.0 / 128)
    k.op("pool", POOL.memset, W=[o256B], ap=ones256[:, :], constant=1.0 / 256)
    k.op("pool", POOL.memset, W=[onesSB], ap=onesS[:, :], constant=1.0)
    k.op("pool", POOL.memset, W=[epsB], ap=epsc[:, :], constant=EPS)
    k.op("pool", POOL.memset, W=[oneB], ap=onec[:, :], constant=1.0)
    k.op("pool", POOL.affine_select, R=[onesB], W=[identB], out=ident_f[:, :], in_=ones_f[:, 0:128],
         pattern=[[-1, 128]], compare_op=ALU.is_equal, fill=FILL0, base=0, channel_multiplier=1)
    k.op("pool", POOL.affine_select, R=[onesB], W=[identbB], out=ident_b[:, :], in_=ones_f[:, 0:128],
         pattern=[[-1, 128]], compare_op=ALU.is_equal, fill=FILL0, base=0, channel_multiplier=1)
    k.op("pool", POOL.affine_select, R=[onesB], W=[selB], out=sel[:, :, :],
         in_=ones_f[0:4, :].rearrange("p (a b) -> p a b", a=4),
         pattern=[[-1, 4], [0, 128]], compare_op=ALU.is_equal, fill=FILL0, base=0, channel_multiplier=1)

    NXT = 2
    xts = [sb("xt%d" % i, [128, D]) for i in range(NXT)]
    hb, hbB = sb("hb", [128, D], BF16)
    hT, hTB = sb("hT", [128, 8, ST_B], BF16)
    st5 = [sb("st5_%d" % i, [128, 8]) for i in range(4)]
    stage = [None, None]
    sctr = [0]
    gcol, gcolB = sb("gcol", [128, 8])

    def load_gcol(vec_ap):
        k.dma("sp", gcol[:, :], vec_ap.rearrange("(c p) -> p c", p=128), gcolB, W=[gcolB],
              allow_slow_non_contiguous=True)

    cast_rr = [0]

    def cast_rows(dst_ap, dstB, src_rows_ap, ncols, scol=None):
        for c0 in range(0, ncols, 2048):
            cw = min(2048, ncols - c0)
            stg, stgB = stage[sctr[0] % 2]
            sctr[0] += 1
            k.dma("sp", stg[:, :cw], src_rows_ap[:, c0:c0 + cw], stgB, W=[stgB])
            e = ("pool", "act")[cast_rr[0] % 2]
            cast_rr[0] += 1
            R = [stgB] + ([gcolB] if scol is not None else [])
            if e == "pool":
                if scol is None:
                    k.op("pool", POOL.tensor_copy, R=R, W=[dstB], out=dst_ap[:, c0:c0 + cw], in_=stg[:, :cw])
                else:
                    k.op("pool", POOL.tensor_scalar, R=R, W=[dstB], out=dst_ap[:, c0:c0 + cw], in0=stg[:, :cw],
                         scalar1=scol, scalar2=1.0, op0=ALU.mult, op1=ALU.mult)
            else:
                if scol is None:
                    k.op("act", ACT.copy, R=R, W=[dstB], out=dst_ap[:, c0:c0 + cw], in_=stg[:, :cw])
                else:
                    k.op("act", ACT.activation, R=R, W=[dstB], out=dst_ap[:, c0:c0 + cw], in_=stg[:, :cw],
                         func=AF.Copy, scale=scol)

    def norm_T(xt, xtB, ncol0):
        s5, s5B = st5[ncol0 // 128 % 4]
        k.op("act", ACT.activation, R=[xtB], W=[hbB, s5B], out=hb[:, :], in_=xt[:, :], func=AF.Square,
             accum_out=s5[:, 0:1])
        k.op("dve", V.tensor_scalar, R=[s5B], W=[s5B], out=s5[:, 1:2], in0=s5[:, 0:1], scalar1=1.0 / D,
             scalar2=EPS, op0=ALU.mult, op1=ALU.add)
        k.op("act", ACT.activation, R=[s5B], W=[s5B], out=s5[:, 2:3], in_=s5[:, 1:2], func=AF.Sqrt)
        k.op("dve", V.reciprocal, R=[s5B], W=[s5B], out=s5[:, 3:4], in_=s5[:, 2:3])
        k.op("act", ACT.activation, R=[xtB, s5B], W=[hbB], out=hb[:, :], in_=xt[:, :], func=AF.Copy,
             scale=s5[:, 3:4])
        pt, pB = psum()
        for c in range(8):
            k.op("pe", PE.transpose, R=[hbB, identbB], W=[pB], out=bfv(pt)[:, c * 128:(c + 1) * 128],
                 in_=hb[:, c * 128:(c + 1) * 128], identity=ident_b[:, :])
        k.op("dve", V.tensor_copy, R=[pB], W=[hTB], out=hT[:, :, ncol0:ncol0 + 128],
             in_=bfv(pt).rearrange("p (c t) -> p c t", c=8))

    xdB = [Buf("xd%d" % t) for t in range(NT)]

    for l in range(NL):
        xsrc = x_d if l == 0 else out_d
        with contextlib.ExitStack() as pa:
            def sba(name, shape, dt=F32):
                t = pa.enter_context(nc.sbuf_tensor("%s_l%d" % (name, l), list(shape), dt))
                return t, Buf(name)

            ST = ST_A
            NTS = ST // 128
            NST = S // ST
            WA, WAB = sba("WA", [128, 33536], BF16)
            prep = contextlib.ExitStack()
            for i in range(2):
                stage[i] = (prep.enter_context(nc.sbuf_tensor("stgA%d_%d" % (l, i), [128, 2048], F32)), Buf("stg"))
            w_in = WA[:, 0:8 * NIN].rearrange("p (c n) -> p c n", c=8)
            o1 = 8 * NIN
            w_out = WA[:, o1:o1 + 8 * D].rearrange("p (c n) -> p c n", c=8)
            o2 = o1 + 8 * D
            w_qb = WA[:, o2:o2 + 512].rearrange("p (c n) -> p c n", c=2)
            w_qi = WA[:, o2 + 512:o2 + 1024].rearrange("p (c n) -> p c n", c=2)
            w_kvb = WA[:, o2 + 1024:o2 + 1152]
            load_gcol(norm_mix_d[l])
            for c in range(8):
                cast_rows(w_in[:, c, :], WAB, w_in_d[l, c * 128:(c + 1) * 128, :], NIN, gcol[:, c:c + 1])
            for c in range(8):
                cast_rows(w_out[:, c, :], WAB, w_out_d[l, c * 128:(c + 1) * 128, :], D)
            for c in range(2):
                cast_rows(w_qb[:, c, :], WAB, w_qb_d[l, c * 128:(c + 1) * 128, :], 256)
                cast_rows(w_qi[:, c, :], WAB, w_qi_d[l, c * 128:(c + 1) * 128, :], 256)
            cast_rows(w_kvb, WAB, w_kvb_d[l, :, :], 128)
            k.barrier()
            prep.close()
            cw, cwB = sba("cw", [128, 12, 4])
            for c in range(12):
                k.dma("sp", cw[:, c, :], gdn_conv_d[l][:, c * 128:(c + 1) * 128].rearrange("j p -> p j"), cwB,
                      W=[cwB], group=True, allow_slow_non_contiguous=True)
            cdw, cdwB = sba("cdw", [128, 2, 31])
            for c in range(2):
                k.dma("sp", cdw[:, c, :], conv_dw_d[l][:, c * 128:(c + 1) * 128].rearrange("j p -> p j"), cdwB,
                      W=[cdwB], group=True, allow_slow_non_contiguous=True)
            cvec, cvecB = sba("cvec", [128, 3, 2])
            for i, v in enumerate((conv_b_d, ln_g_d, ln_b_d)):
                k.dma("sp", cvec[:, i, :], v[l].rearrange("(c p) -> p c", p=128), cvecB, W=[cvecB], group=True,
                      allow_slow_non_contiguous=True)
            gnc, gncB = sba("gnc", [128, 1])
            k.dma("sp", gnc[:, :], gdn_norm_d[l].rearrange("(p o) -> p o", o=1), gncB, W=[gncB])
            gv, gvB = sba("gv", [4, 4])
            k.dma("sp", gv[:, 0:1], a_log_d[l].rearrange("(p o) -> p o", o=1), gvB, W=[gvB])
            k.dma("sp", gv[:, 1:2], dt_bias_d[l].rearrange("(p o) -> p o", o=1), gvB, W=[gvB], group=True)
            k.op("act", ACT.activation, R=[gvB], W=[gvB], out=gv[:, 2:3], in_=gv[:, 0:1], func=AF.Exp)
            k.op("dve", V.tensor_scalar, R=[gvB], W=[gvB], out=gv[:, 3:4], in0=gv[:, 2:3], scalar1=-1.0,
                 scalar2=None, op0=ALU.mult)
            gq8, gq8B = sba("gq8", [128, 64])
            gkb, gkbB = sba("gkb", [128, 64])
            k.dma("sp", gq8[:, :], dq_norm_d[l].partition_broadcast(128), gq8B, W=[gq8B])
            k.dma("sp", gkb[:, :], dk_norm_d[l].partition_broadcast(128), gkbB, W=[gkbB])
            k.op("dve", V.tensor_scalar, R=[gq8B], W=[gq8B], out=gq8[:, :], in0=gq8[:, :], scalar1=0.125,
                 scalar2=None, op0=ALU.mult)
            dgc, dgcB = sba("dgc", [128, 2, 31, 128], BF16)
            for i in range(2):
                for j in range(31):
                    k.op("pool", POOL.tensor_scalar, R=[identB, cdwB], W=[dgcB], out=dgc[:, i, j, :],
                         in0=ident_f[:, :], scalar1=cdw[:, i, j:j + 1], scalar2=1.0, op0=ALU.mult, op1=ALU.mult)
            pre, preB = sba("pre", [128, 12, 3 + ST])
            gl, glB = sba("gl", [128, 2, 30 + ST], BF16)
            k.op("pool", POOL.memset, W=[preB], ap=pre[:, :, :], constant=0.0)
            k.op("pool", POOL.memset, W=[glB], ap=gl[:, :, :], constant=0.0)
            qkvT, _ = sba("qkvT", [128, 12, ST])
            qkvB = [Buf("qkv%d" % i) for i in range(12)]
            zs, zsB = sba("zs", [128, 4, ST])
            cqT, cqB = sba("cqT", [128, 2, ST], BF16)
            ckvT, ckvB = sba("ckvT", [128, ST], BF16)
            yT, yTB = sba("yT", [128, 8, ST], BF16)
            Sst = [sba("S%d" % i, [128, 4, 128]) for i in range(2)]
            k.op("pool", POOL.memset, W=[Sst[0][1]], ap=Sst[0][0][:, :, :], constant=0.0)
            KKT, _ = sba("KKT", [128, S], BF16)
            kkB = [Buf("kk%d" % t) for t in range(NT)]
            Vc, _ = sba("Vc", [128, NT, 65], BF16)
            vcB = [Buf("vc%d" % t) for t in range(NT)]
            VcI, VcIB = sba("VcI", [128, 1], BF16)
            work, workB = sba("work", [128, S])
            rows, rowsB = sba("rows", [4, 8, ST])
            cols = [sba("cols%d" % i, [128, 16]) for i in range(2)]
            kkt, kktB = sba("kkt", [128, 2, 128], BF16)
            wab, wabB = sba("wab", [128, 2, 8])
            gd = {n: sba("gd_" + n, [128, 512]) for n in
                  ("Kbd", "Kdec", "Vb", "QKT", "qd", "P0", "P1", "PT0", "PT1", "RT0", "RT1")}
            QQs = [sba("QQ%d" % i, [128, 512], BF16) for i in range(2)]
            mks = [sba("mk%d" % i, [128, 512], BF16) for i in range(2)]
            mkc = [0]
            print("sbuf remaining (phase A)", nc.sbuf_bytes_remaining)
            NSC = 8
            scr = [sba("scr%d" % i, [128, 512]) for i in range(NSC)]
            scrc = [0]

            def scratch():
                t, b = scr[scrc[0] % NSC]
                scrc[0] += 1
                return t, b
            NSB = 6
            scb = [sba("scb%d" % i, [128, 512], BF16) for i in range(NSB)]
            scbc = [0]

            def scratchb():
                t, b = scb[scbc[0] % NSB]
                scbc[0] += 1
                return t, b
            m8s = [sba("m8_%d" % i, [128, 8]) for i in range(2)]
            KB = 26
            pw2, pw2B = sba("pw2", [128, KB])
            for kk_ in range(KB):
                k.op("pool", POOL.memset, W=[pw2B], ap=pw2[:, kk_:kk_ + 1], constant=-(2.0 ** -(kk_ + 1)))
            bst, bstB = sba("bst", [128, 16])
            nst, nstB = sba("nst", [128, KB])
            junk8 = xts[1][0][:, :].bitcast(mybir.dt.int8)
            junkB = xts[1][1]
            sm = [sba("sm%d" % i, [128, 16]) for i in range(4)]
            smc = [0]

            def small():
                t, b = sm[smc[0] % 4]
                smc[0] += 1
                return t, b

            for t in range(NT):
                k.op("pool", POOL.memset, W=[vcB[t]], ap=Vc[:, t, 64:65], constant=1.0)

            def v3(ap, a=4):
                return ap.rearrange("p (a b) -> p a b", a=a)

            for s_i in range(NST):
                if stop == "W":
                    break
                t0 = s_i * ST
                tiles = [s_i * NTS + j for j in range(NTS)]
                for j in range(NTS):
                    xt, xtB = xts[j]
                    k.dma("sp", xt[:, :], xsrc[t0 + j * 128:t0 + (j + 1) * 128, :], xtB, R=[xdB[tiles[j]]],
                          W=[xtB])
                    norm_T(xt, xtB, j * 128)

                def featmajor(col0, M):
                    pt, pB = psum()
                    for c in range(8):
                        k.op("pe", PE.matmul, R=[WAB, hTB], W=[pB], out=pt[:M, :ST],
                             lhsT=w_in[:, c, col0:col0 + M], rhs=hT[:, c, 0:ST], start=(c == 0), stop=(c == 7))
                    return pt, pB

                for i in range(12):
                    pt, pB = featmajor(i * 128, 128)
                    k.op("act", ACT.copy, R=[pB], W=[preB], out=pre[:, i, 3:3 + ST], in_=pt[:, :ST])
                    k.op("dve", V.tensor_scalar, R=[preB, cwB], W=[qkvB[i]], out=qkvT[:, i, :], in0=pre[:, i, 0:ST],
                         scalar1=cw[:, i, 0:1], scalar2=None, op0=ALU.mult)
                    for j in range(1, 4):
                        k.op("dve", V.scalar_tensor_tensor, R=[preB, cwB, qkvB[i]], W=[qkvB[i]], out=qkvT[:, i, :],
                             in0=pre[:, i, j:j + ST], scalar=cw[:, i, j:j + 1], in1=qkvT[:, i, :], op0=ALU.mult,
                             op1=ALU.add)
                    k.op("act", ACT.activation, R=[qkvB[i]], W=[qkvB[i]], out=qkvT[:, i, :], in_=qkvT[:, i, :],
                         func=AF.Silu)
                k.op("dve", V.tensor_copy, R=[preB], W=[preB], out=pre[:, :, 0:3], in_=pre[:, :, ST:ST + 3])
                for i in range(8):
                    sq, sqB = scratch()
                    k.op("act", ACT.activation, R=[qkvB[i]], W=[sqB], out=sq[:, :ST], in_=qkvT[:, i, :],
                         func=AF.Square)
                    pt, pB = psum()
                    k.op("pe", PE.matmul, R=[onesSB, sqB], W=[pB], out=pt[:, :ST], lhsT=onesS[:, :],
                         rhs=sq[:, :ST], start=True, stop=True)
                    k.op("act", ACT.activation, R=[pB, epsB], W=[sqB], out=sq[:, :ST], in_=pt[:, :ST],
                         func=AF.Sqrt, bias=epsc[:, 0:1])
                    k.op("dve", V.reciprocal, R=[sqB], W=[sqB], out=sq[:, :ST], in_=sq[:, :ST])
                    k.op("dve", V.scalar_tensor_tensor, R=[qkvB[i], sqB], W=[qkvB[i]], out=qkvT[:, i, :],
                         in0=qkvT[:, i, :], scalar=(128 ** -0.5 if i < 4 else 1.0), in1=sq[:, :ST],
                         op0=ALU.mult, op1=ALU.mult)
                for i in range(4):
                    pt, pB = featmajor(1536 + i * 128, 128)
                    k.op("act", ACT.activation, R=[pB], W=[zsB], out=zs[:, i, :], in_=pt[:, :ST], func=AF.Silu)
                for i in range(2):
                    pt, pB = featmajor(C_CQ + i * 128, 128)
                    k.op("act", ACT.copy, R=[pB], W=[cqB], out=cqT[:, i, :], in_=pt[:, :ST])
                pt, pB = featmajor(C_CKV, 128)
                k.op("act", ACT.copy, R=[pB], W=[ckvB], out=ckvT[:, :], in_=pt[:, :ST])
                for i in range(2):
                    pu, puB = featmajor(C_UG + i * 128, 128)
                    pg, pgB = featmajor(C_UG + 256 + i * 128, 128)
                    sg, sgB = scratch()
                    k.op("act", ACT.activation, R=[pgB], W=[sgB], out=sg[:, :ST], in_=pg[:, :ST], func=AF.Sigmoid)
                    k.op("dve", V.tensor_tensor, R=[puB, sgB], W=[glB], out=gl[:, i, 30:30 + ST], in0=pu[:, :ST],
                         in1=sg[:, :ST], op=ALU.mult)
                for j in range(NTS):
                    pt, pB = psum()
                    for c in range(8):
                        k.op("pe", PE.matmul, R=[WAB, hTB], W=[pB], out=pt[:, :68],
                             lhsT=hT[:, c, j * 128:(j + 1) * 128], rhs=w_in[:, c, C_KI:C_KI + 68],
                             start=(c == 0), stop=(c == 7))
                    k.op("act", ACT.copy, R=[pB], W=[kktB], out=kkt[:, j, 64:128], in_=pt[:, 0:64])
                    k.op("act", ACT.activation, R=[pB], W=[wabB], out=wab[:, j, 0:4], in_=pt[:, 64:68],
                         func=AF.Abs, scale=1.0 / 16)
                    k.op("act", ACT.activation, R=[pB], W=[wabB], out=wab[:, j, 4:8], in_=pt[:, 64:68],
                         func=AF.Sign)
                if stop == "A1":
                    break
                pa_, paB = featmajor(C_A, 4)
                pb_, pbB = featmajor(C_B, 4)
                r = rows
                k.op("act", ACT.activation, R=[paB, gvB], W=[rowsB], out=r[:, 0, :], in_=pa_[:4, :ST],
                     func=AF.Abs, bias=gv[:, 1:2])
                k.op("act", ACT.activation, R=[rowsB], W=[rowsB], out=r[:, 1, :], in_=r[:, 0, :], func=AF.Exp,
                     scale=-1.0)
                k.op("act", ACT.activation, R=[rowsB, oneB], W=[rowsB], out=r[:, 1, :], in_=r[:, 1, :], func=AF.Ln,
                     bias=onec[0:4, 0:1])
                k.op("dve", V.tensor_scalar, R=[paB, gvB], W=[rowsB], out=r[:, 2, :], in0=pa_[:4, :ST],
                     scalar1=gv[:, 1:2], scalar2=0.0, op0=ALU.add, op1=ALU.max)
                k.op("dve", V.tensor_tensor, R=[rowsB], W=[rowsB], out=r[:, 2, :], in0=r[:, 2, :], in1=r[:, 1, :],
                     op=ALU.add)
                k.op("dve", V.tensor_scalar, R=[rowsB, gvB], W=[rowsB], out=r[:, 2, :], in0=r[:, 2, :],
                     scalar1=gv[:, 3:4], scalar2=None, op0=ALU.mult)
                for ch in range(NTS):
                    cs = slice(ch * 128, (ch + 1) * 128)
                    k.op("dve", V.tensor_tensor_scan, R=[rowsB, onesB], W=[rowsB], out=r[:, 3, cs],
                         data0=ones_f[0:4, 0:128], data1=r[:, 2, cs], initial=0.0, op0=ALU.mult, op1=ALU.add)
                k.op("act", ACT.activation, R=[pbB], W=[rowsB], out=r[:, 4, :], in_=pb_[:4, :ST], func=AF.Sigmoid)
                k.op("act", ACT.activation, R=[rowsB], W=[rowsB], out=r[:, 5, :], in_=r[:, 3, :], func=AF.Exp)
                for ch in range(NTS):
                    cs = slice(ch * 128, (ch + 1) * 128)
                    k.op("dve", V.tensor_scalar, R=[rowsB], W=[rowsB], out=r[:, 6, cs], in0=r[:, 3, cs],
                         scalar1=r[:, 3, ch * 128 + 127:ch * 128 + 128], scalar2=-1.0, op0=ALU.subtract,
                         op1=ALU.mult)
                k.op("act", ACT.activation, R=[rowsB], W=[rowsB], out=r[:, 6, :], in_=r[:, 6, :], func=AF.Exp)
                k.op("dve", V.tensor_tensor, R=[rowsB], W=[rowsB], out=r[:, 7, :], in0=r[:, 4, :], in1=r[:, 5, :],
                     op=ALU.mult)

                yc, ycB = scratch()
                ysq, ysqB = scratch()
                for i in range(2):
                    pc, pcB = psum()
                    for jj in range(31):
                        k.op("pe", PE.matmul, R=[dgcB, glB], W=[pcB], out=pc[:, :ST], lhsT=dgc[:, i, jj, :],
                             rhs=gl[:, i, jj:jj + ST], start=(jj == 0), stop=(jj == 30))
                    k.op("act", ACT.activation, R=[pcB, cvecB], W=[ycB], out=yc[:, i * ST:(i + 1) * ST], in_=pc[:, :ST],
                         func=AF.Identity, bias=cvec[:, 0, i:i + 1])
                    k.op("act", ACT.activation, R=[pcB, cvecB], W=[ysqB], out=ysq[:, i * ST:(i + 1) * ST],
                         in_=pc[:, :ST], func=AF.Square, bias=cvec[:, 0, i:i + 1])
                k.op("dve", V.tensor_copy, R=[glB], W=[glB], out=gl[:, :, 0:30], in_=gl[:, :, ST:ST + 30])
                pmu, pmuB = psum()
                pms, pmsB = psum()
                for i in range(2):
                    k.op("pe", PE.matmul, R=[o256B, ycB], W=[pmuB], out=pmu[:, :ST], lhsT=ones256[:, :],
                         rhs=yc[:, i * ST:(i + 1) * ST], start=(i == 0), stop=(i == 1))
                for i in range(2):
                    k.op("pe", PE.matmul, R=[o256B, ysqB], W=[pmsB], out=pms[:, :ST], lhsT=ones256[:, :],
                         rhs=ysq[:, i * ST:(i + 1) * ST], start=(i == 0), stop=(i == 1))
                mu, muB = scratch()
                k.op("act", ACT.copy, R=[pmuB], W=[muB], out=mu[:, 0:ST], in_=pmu[:, :ST])
                k.op("act", ACT.activation, R=[pmuB], W=[muB], out=mu[:, ST:2 * ST], in_=pmu[:, :ST], func=AF.Square)
                k.op("dve", V.tensor_tensor, R=[pmsB, muB], W=[muB], out=mu[:, ST:2 * ST], in0=pms[:, :ST],
                     in1=mu[:, ST:2 * ST], op=ALU.subtract)
                k.op("act", ACT.activation, R=[muB, epsB], W=[muB], out=mu[:, ST:2 * ST], in_=mu[:, ST:2 * ST],
                     func=AF.Sqrt, bias=epsc[:, 0:1])
                k.op("dve", V.reciprocal, R=[muB], W=[muB], out=mu[:, ST:2 * ST], in_=mu[:, ST:2 * ST])
                for i in range(2):
                    k.op("dve", V.tensor_tensor, R=[ycB, muB], W=[ycB], out=yc[:, i * ST:(i + 1) * ST],
                         in0=yc[:, i * ST:(i + 1) * ST], in1=mu[:, 0:ST], op=ALU.subtract)
                    k.op("dve", V.tensor_tensor, R=[ycB, muB], W=[ycB], out=yc[:, i * ST:(i + 1) * ST],
                         in0=yc[:, i * ST:(i + 1) * ST], in1=mu[:, ST:2 * ST], op=ALU.mult)
                    k.op("act", ACT.activation, R=[ycB, cvecB], W=[yTB], out=yT[:, 6 + i, :],
                         in_=yc[:, i * ST:(i + 1) * ST], func=AF.Silu, scale=cvec[:, 1, i:i + 1],
                         bias=cvec[:, 2, i:i + 1])

                def dsa_gen(j):
                    t = tiles[j]
                    cs = slice(j * 128, (j + 1) * 128)
                    pq, pqB = psum()
                    for c in range(2):
                        k.op("pe", PE.matmul, R=[cqB, WAB], W=[pqB], out=pq[:, 0:256], lhsT=cqT[:, c, cs],
                             rhs=w_qb[:, c, :], start=(c == 0), stop=(c == 1))
                    for c in range(2):
                        k.op("pe", PE.matmul, R=[cqB, WAB], W=[pqB], out=pq[:, 256:512], lhsT=cqT[:, c, cs],
                             rhs=w_qi[:, c, :], start=(c == 0), stop=(c == 1))
                    pkv, pkvB = psum()
                    k.op("pe", PE.matmul, R=[ckvB, WAB], W=[pkvB], out=pkv[:, 0:128], lhsT=ckvT[:, cs], rhs=w_kvb,
                         start=True, stop=True)
                    sq, sqB = scratch()
                    k.op("act", ACT.activation, R=[pqB], W=[sqB], out=sq[:, 0:256], in_=pq[:, 0:256], func=AF.Square)
                    k.op("act", ACT.activation, R=[pkvB], W=[sqB], out=sq[:, 256:320], in_=pkv[:, 0:64],
                         func=AF.Square)
                    ss, ssB = small()
                    k.op("dve", V.tensor_reduce, R=[sqB], W=[ssB], out=ss[:, 0:5],
                         in_=sq[:, 0:320].rearrange("p (a b) -> p a b", a=5), axis=AX.X, op=ALU.add)
                    k.op("dve", V.tensor_scalar, R=[ssB], W=[ssB], out=ss[:, 5:10], in0=ss[:, 0:5], scalar1=1.0 / 64,
                         scalar2=EPS, op0=ALU.mult, op1=ALU.add)
                    k.op("act", ACT.activation, R=[ssB], W=[ssB], out=ss[:, 5:10], in_=ss[:, 5:10], func=AF.Sqrt)
                    k.op("dve", V.reciprocal, R=[ssB], W=[ssB], out=ss[:, 10:15], in_=ss[:, 5:10])
                    tq, tqB = scratch()
                    k.op("dve", V.tensor_tensor, R=[pqB, ssB], W=[tqB], out=v3(tq[:, 0:256]), in0=v3(pq[:, 0:256]),
                         in1=bc(ss[:, 10:14].unsqueeze(2), [128, 4, 64]), op=ALU.mult)
                    QI, QIB = scratchb()
                    QI3 = v3(QI[:, :])
                    k.op("dve", V.tensor_tensor, R=[tqB, gq8B], W=[QIB], out=QI3[:, :, 0:64], in0=v3(tq[:, 0:256]),
                         in1=bc(gq8[:, :].unsqueeze(1), [128, 4, 64]), op=ALU.mult)
                    k.op("dve", V.tensor_tensor, R=[pqB, wabB, ssB], W=[QIB], out=QI3[:, :, 64:128], in0=v3(pq[:, 256:512]),
                         in1=bc(wab[:, j, 0:4].unsqueeze(2), [128, 4, 64]), op=ALU.mult)
                    k.op("dve", V.scalar_tensor_tensor, R=[pkvB, ssB, gkbB], W=[kktB], out=kkt[:, j, 0:64],
                         in0=pkv[:, 0:64], scalar=ss[:, 14:15], in1=gkb[:, :], op0=ALU.mult, op1=ALU.mult)
                    k.op("dve", V.tensor_copy, R=[pkvB, ssB], W=[vcB[t]], out=Vc[:, t, 0:64], in_=pkv[:, 64:128])
                    ptq, ptqB = psum()
                    for h in range(4):
                        k.op("pe", PE.transpose, R=[QIB, identbB], W=[ptqB], out=bfv(ptq)[:, h * 128:(h + 1) * 128],
                             in_=QI3[:, h, :], identity=ident_b[:, :])
                    QQ, QQB = QQs[t % 2]
                    k.op("act", ACT.copy, R=[ptqB], W=[QQB], out=QQ[:, :], in_=bfv(ptq)[:, 0:512])
                    ptk, ptkB = psum()
                    k.op("pe", PE.transpose, R=[kktB, identbB], W=[ptkB], out=bfv(ptk)[:, 0:128], in_=kkt[:, j, :],
                         identity=ident_b[:, :])
                    k.op("act", ACT.copy, R=[ptkB], W=[kkB[t]], out=KKT[:, t * 128:(t + 1) * 128],
                         in_=bfv(ptk)[:, 0:128])
                    Sw = (t + 1) * 128
                    for kc0 in range(0, Sw, 512):
                        wd = min(512, Sw - kc0)
                        kts = list(range(kc0 // 128, (kc0 + wd) // 128))
                        rr = []
                        for h in range(4):
                            ph, phB = psum()
                            k.op("pe", PE.matmul, R=[QQB] + [kkB[u] for u in kts], W=[phB], out=ph[:, :wd],
                                 lhsT=QQ[64:128, h * 128:(h + 1) * 128], rhs=KKT[64:128, kc0:kc0 + wd],
                                 start=True, stop=True)
                            rh, rhB = scratch()
                            k.op("act", ACT.activation, R=[phB], W=[rhB], out=rh[:, :wd], in_=ph[:, :wd], func=AF.Relu)
                            rr.append((rh, rhB))
                        k.op("dve", V.tensor_scalar, R=[rr[0][1], wabB], W=[workB], out=work[:, kc0:kc0 + wd],
                             in0=rr[0][0][:, :wd], scalar1=wab[:, j, 4:5], scalar2=None, op0=ALU.mult)
                        for h in range(1, 4):
                            k.op("dve", V.scalar_tensor_tensor, R=[rr[h][1], wabB, workB], W=[workB],
                                 out=work[:, kc0:kc0 + wd], in0=rr[h][0][:, :wd], scalar=wab[:, j, 4 + h:5 + h],
                                 in1=work[:, kc0:kc0 + wd], op0=ALU.mult, op1=ALU.add)
                    if t >= 2:
                        k.op("dve", V.tensor_reduce, R=[workB], W=[bstB], out=bst[:, 0:1], in_=work[:, :Sw], axis=AX.X,
                             op=ALU.min)
                        m8, m8B = m8s[0]
                        k.op("dve", V.max, R=[workB], W=[m8B], out=m8[:, :], in_=work[:, :Sw])
                    k.op("pool", POOL.affine_select, R=[workB], W=[workB], out=work[:, t * 128:(t + 1) * 128],
                         in_=work[:, t * 128:(t + 1) * 128], pattern=[[-1, 128]], compare_op=ALU.is_ge, fill=FILLNC,
                         base=0, channel_multiplier=1)
                    if t >= 2:
                        k.op("dve", V.tensor_tensor, R=[m8B, bstB], W=[bstB], out=bst[:, 2:3], in0=m8[:, 0:1],
                             in1=bst[:, 0:1], op=ALU.subtract)
                        k.op("dve", V.tensor_scalar, R=[bstB, pw2B], W=[nstB], out=nst[:, :], in0=pw2[:, :],
                             scalar1=bst[:, 2:3], scalar2=None, op0=ALU.mult)
                        k.op("dve", V.tensor_scalar, R=[bstB, m8B], W=[bstB], out=bst[:, 4:5], in0=bst[:, 0:1],
                             scalar1=m8[:, 0:1], scalar2=-0.5, op0=ALU.add, op1=ALU.mult)
                        k.op("dve", V.tensor_copy, R=[bstB], W=[bstB], out=bst[:, 3:4], in_=bst[:, 0:1])
                        thr = float(511 - Sw)
                        yield
                        for it in range(KB):
                            nm = bst[:, 4 + it % 2:5 + it % 2]
                            nmn = bst[:, 4 + (it + 1) % 2:5 + (it + 1) % 2]
                            cn = bst[:, 6 + it % 2:7 + it % 2]
                            k.op("act", ACT.activation, R=[workB, bstB], W=[junkB, bstB], out=junk8[:, :Sw],
                                 in_=work[:, :Sw], func=AF.Sign, bias=nm, accum_out=cn)
                            k.op("dve", V.tensor_scalar, R=[bstB], W=[bstB], out=bst[:, 8:9], in0=cn, scalar1=thr,
                                 scalar2=0.5, op0=ALU.is_ge, op1=ALU.subtract)
                            k.op("dve", V.scalar_tensor_tensor, R=[bstB, nstB], W=[bstB], out=nmn, in0=bst[:, 8:9],
                                 scalar=nst[:, it:it + 1], in1=nm, op0=ALU.mult, op1=ALU.add)
                            k.op("dve", V.tensor_scalar, R=[bstB], W=[bstB], out=bst[:, 9:10], in0=bst[:, 8:9],
                                 scalar1=0.5, scalar2=1.0e30, op0=ALU.subtract, op1=ALU.mult)
                            k.op("dve", V.scalar_tensor_tensor, R=[bstB], W=[bstB], out=bst[:, 3:4], in0=bst[:, 9:10],
                                 scalar=nm, in1=bst[:, 3:4], op0=ALU.subtract, op1=ALU.max)
                            yield
                    if t < 2:
                        yield
                    pso, psoB = psb[7]
                    for kc0 in range(0, Sw, 512):
                        wd = min(512, Sw - kc0)
                        mk, mkB = mks[mkc[0] % 2]
                        mkc[0] += 1
                        if t >= 2:
                            k.op("dve", V.tensor_scalar, R=[workB, bstB], W=[mkB], out=mk[:, :wd],
                                 in0=work[:, kc0:kc0 + wd], scalar1=bst[:, 3:4], scalar2=None, op0=ALU.is_ge)
                        else:
                            k.op("dve", V.tensor_single_scalar, R=[workB], W=[mkB], out=mk[:, :wd],
                                 in_=work[:, kc0:kc0 + wd], scalar=-1.0e38, op=ALU.is_gt)
                        for u in range(wd // 128):
                            kt = kc0 // 128 + u
                            pmk, pmkB = psum()
                            k.op("pe", PE.transpose, R=[mkB, identbB], W=[pmkB], out=bfv(pmk)[:, 0:128],
                                 in_=mk[:, u * 128:(u + 1) * 128], identity=ident_b[:, :])
                            pl, plB = psum()
                            k.op("pe", PE.matmul, R=[kkB[kt], QQB], W=[plB], out=pl[:, :],
                                 lhsT=KKT[0:64, kt * 128:(kt + 1) * 128], rhs=QQ[0:64, :], start=True, stop=True)
                            pT_, pTB = scratchb()
                            k.op("act", ACT.activation, R=[plB], W=[pTB], out=pT_[:, :], in_=pl[:, :], func=AF.Exp)
                            pmm, pmmB = scratchb()
                            k.op("dve", V.tensor_tensor, R=[pTB, pmkB], W=[pmmB], out=v3(pmm[:, :]), in0=v3(pT_[:, :]),
                                 in1=bc(bfv(pmk)[:, 0:128].unsqueeze(1), [128, 4, 128]), op=ALU.mult)
                            for h in range(4):
                                k.op("pe", PE.matmul, R=[pmmB, vcB[kt]], W=[psoB], out=pso[:, h * 65:(h + 1) * 65],
                                     lhsT=pmm[:, h * 128:(h + 1) * 128], rhs=Vc[:, kt, :],
                                     start=(kt == 0 and h == 0), stop=(kt == t and h == 3))
                    pso3 = pso[:, 0:260].rearrange("p (a b) -> p a b", a=4)
                    rd, rdB = small()
                    k.op("dve", V.reciprocal, R=[psoB], W=[rdB], out=rd[:, 0:4], in_=pso3[:, :, 64])
                    yb, ybB = scratchb()
                    k.op("dve", V.tensor_tensor, R=[psoB, rdB], W=[ybB], out=v3(yb[:, 0:256]), in0=pso3[:, :, 0:64],
                         in1=bc(rd[:, 0:4].unsqueeze(2), [128, 4, 64]), op=ALU.mult)
                    pty, ptyB = psum()
                    for c in range(2):
                        k.op("pe", PE.transpose, R=[ybB, identbB], W=[ptyB], out=bfv(pty)[:, c * 128:(c + 1) * 128],
                             in_=yb[:, c * 128:(c + 1) * 128], identity=ident_b[:, :])
                    k.op("act", ACT.copy, R=[ptyB], W=[yTB], out=yT[:, 4:6, cs],
                         in_=bfv(pty)[:, 0:256].rearrange("p (a b) -> p a b", a=2))

                dsa_g = dsa_gen(0)
                next(dsa_g)
                nticks = [0]

                def tick(n=1):
                    for _ in range(n):
                        if tiles[0] >= 2 and nticks[0] < KB:
                            nticks[0] += 1
                            next(dsa_g)

                if stop == "A2":
                    break
                for ch in range(NTS):
                    cs = slice(ch * 128, (ch + 1) * 128)
                    col, colB = cols[ch]
                    pt, pB = psum()
                    for qi_, rq in enumerate((3, 4, 7, 6)):
                        k.op("pe", PE.matmul, R=[rowsB, identB], W=[pB], out=pt[:, qi_ * 4:(qi_ + 1) * 4],
                             lhsT=r[:, rq, cs], rhs=ident_f[0:4, 0:4], start=True, stop=True)
                    k.op("act", ACT.copy, R=[pB], W=[colB], out=col[:, :], in_=pt[:, 0:16])
                    c_b, c_beta, c_be, c_elb = (col[:, 0:4], col[:, 4:8], col[:, 8:12], col[:, 12:16])
                    prb, prbB = psum()
                    pre_, preB_ = psum()
                    for h in range(4):
                        k.op("pe", PE.matmul, R=[selB, rowsB], W=[prbB], out=prb[:, h * 128:(h + 1) * 128],
                             lhsT=sel[:, h, :], rhs=r[:, 3, cs], start=True, stop=True)
                    for h in range(4):
                        k.op("pe", PE.matmul, R=[selB, rowsB], W=[preB_], out=pre_[:, h * 128:(h + 1) * 128],
                             lhsT=sel[:, h, :], rhs=r[:, 5, cs], start=True, stop=True)
                    qd, qdB = gd["qd"]
                    k.op("dve", V.tensor_tensor, R=[preB_] + qkvB[0:4], W=[qdB], out=v3(qd[:, :]),
                         in0=qkvT[:, 0:4, cs], in1=v3(pre_[:, :]), op=ALU.mult)
                    ebl, eblB = small()
                    k.op("dve", V.tensor_copy, R=[preB_], W=[eblB], out=ebl[:, 0:4], in_=v3(pre_[:, :])[:, :, 127])
                    pk, pkB = psum()
                    pv, pvB = psum()
                    for h in range(4):
                        k.op("pe", PE.transpose, R=[qkvB[4 + h], identB], W=[pkB], out=pk[:, h * 128:(h + 1) * 128],
                             in_=qkvT[:, 4 + h, cs], identity=ident_f[:, :])
                    for h in range(4):
                        k.op("pe", PE.transpose, R=[qkvB[8 + h], identB], W=[pvB], out=pv[:, h * 128:(h + 1) * 128],
                             in_=qkvT[:, 8 + h, cs], identity=ident_f[:, :])
                    Kbd, KbdB = gd["Kbd"]
                    Kdec, KdecB = gd["Kdec"]
                    Vb, VbB = gd["Vb"]
                    k.op("dve", V.tensor_tensor, R=[pkB, colB], W=[KbdB], out=v3(Kbd[:, :]), in0=v3(pk[:, :]),
                         in1=bc(c_be.unsqueeze(2), [128, 4, 128]), op=ALU.mult)
                    k.op("dve", V.tensor_tensor, R=[pkB, colB], W=[KdecB], out=v3(Kdec[:, :]), in0=v3(pk[:, :]),
                         in1=bc(c_elb.unsqueeze(2), [128, 4, 128]), op=ALU.mult)
                    k.op("dve", V.tensor_tensor, R=[pvB, colB], W=[VbB], out=v3(Vb[:, :]), in0=v3(pv[:, :]),
                         in1=bc(c_beta.unsqueeze(2), [128, 4, 128]), op=ALU.mult)
                    tick()
                    if stop == "A3a":
                        break
                    pg1, pg1B = psum()
                    pg2, pg2B = psum()
                    for h in range(4):
                        k.op("pe", PE.matmul, R=[qkvB[4 + h]], W=[pg1B], out=pg1[:, h * 128:(h + 1) * 128],
                             lhsT=qkvT[:, 4 + h, cs], rhs=qkvT[:, 4 + h, cs], start=True, stop=True)
                    for h in range(4):
                        k.op("pe", PE.matmul, R=[qkvB[4 + h], qkvB[h]], W=[pg2B], out=pg2[:, h * 128:(h + 1) * 128],
                             lhsT=qkvT[:, 4 + h, cs], rhs=qkvT[:, h, cs], start=True, stop=True)
                    GT, GTB = scratch()
                    k.op("dve", V.tensor_tensor, R=[prbB, colB], W=[GTB], out=v3(GT[:, :]), in0=v3(prb[:, :]),
                         in1=bc(c_b.unsqueeze(2), [128, 4, 128]), op=ALU.subtract)
                    k.op("pool", POOL.affine_select, R=[GTB], W=[GTB], out=v3(GT[:, :]), in_=v3(GT[:, :]),
                         pattern=[[0, 4], [1, 128]], compare_op=ALU.is_ge, fill=FILLM, base=0,
                         channel_multiplier=-1)
                    k.op("act", ACT.activation, R=[GTB], W=[GTB], out=GT[:, :], in_=GT[:, :], func=AF.Exp)
                    tick()
                    QKT, QKTB = gd["QKT"]
                    k.op("dve", V.tensor_tensor, R=[pg2B, GTB], W=[QKTB], out=QKT[:, :], in0=pg2[:, :], in1=GT[:, :],
                         op=ALU.mult)
                    MT, MTB = scratch()
                    k.op("dve", V.tensor_tensor, R=[pg1B, GTB], W=[MTB], out=MT[:, :], in0=pg1[:, :], in1=GT[:, :],
                         op=ALU.mult)
                    k.op("pool", POOL.affine_select, R=[MTB], W=[MTB], out=v3(MT[:, :]), in_=v3(MT[:, :]),
                         pattern=[[0, 4], [1, 128]], compare_op=ALU.is_gt, fill=FILL0, base=0, channel_multiplier=-1)
                    tick()
                    if stop == "A3b":
                        break
                    pA, pAB = psum()
                    for h in range(4):
                        k.op("pe", PE.transpose, R=[MTB, identB], W=[pAB], out=pA[:, h * 128:(h + 1) * 128],
                             in_=MT[:, h * 128:(h + 1) * 128], identity=ident_f[:, :])
                    P, PB = gd["P0"]
                    k.op("dve", V.tensor_tensor, R=[pAB, colB], W=[PB], out=v3(P[:, :]), in0=v3(pA[:, :]),
                         in1=bc(c_beta.unsqueeze(2), [128, 4, 128]), op=ALU.mult)
                    tick()
                    if stop == "A3b1":
                        break
                    pAT, pATB = psum()
                    for h in range(4):
                        k.op("pe", PE.transpose, R=[PB, identB], W=[pATB], out=pAT[:, h * 128:(h + 1) * 128],
                             in_=P[:, h * 128:(h + 1) * 128], identity=ident_f[:, :])
                    if stop == "A3b1a":
                        break
                    PT_, PTB = gd["PT0"]
                    k.op("act", ACT.copy, R=[pATB], W=[PTB], out=PT_[:, :], in_=pAT[:, :])
                    if stop == "A3b1b":
                        break
                    RT, RTB = gd["RT0"]
                    for h in range(4):
                        hs = slice(h * 128, (h + 1) * 128)
                        k.op("dve", V.tensor_tensor, R=[identB, PTB], W=[RTB], out=RT[:, hs], in0=ident_f[:, :],
                             in1=PT_[:, hs], op=ALU.subtract)
                    tick()
                    if stop == "A3b2":
                        break
                    for lvl in range(6):
                        if stop == "A3b3" and lvl == 1:
                            break
                        p2, p2B = psum()
                        for h in range(4):
                            hs = slice(h * 128, (h + 1) * 128)
                            k.op("pe", PE.matmul, R=[PTB, PB], W=[p2B], out=p2[:, hs], lhsT=PT_[:, hs], rhs=P[:, hs],
                                 start=True, stop=True)
                        if lvl < 5:
                            p2t, p2tB = psum()
                            for h in range(4):
                                hs = slice(h * 128, (h + 1) * 128)
                                k.op("pe", PE.matmul, R=[PTB, PB], W=[p2tB], out=p2t[:, hs], lhsT=P[:, hs],
                                     rhs=PT_[:, hs], start=True, stop=True)
                        Pn, PnB = gd["P%d" % ((lvl + 1) % 2)]
                        k.op("act", ACT.copy, R=[p2B], W=[PnB], out=Pn[:, :], in_=p2[:, :])
                        tick()
                        if lvl < 5:
                            PTn, PTnB = gd["PT%d" % ((lvl + 1) % 2)]
                            k.op("act", ACT.copy, R=[p2tB], W=[PTnB], out=PTn[:, :], in_=p2t[:, :])
                        p3, p3B = psum()
                        for h in range(4):
                            hs = slice(h * 128, (h + 1) * 128)
                            k.op("pe", PE.matmul, R=[PnB, RTB], W=[p3B], out=p3[:, hs], lhsT=Pn[:, hs], rhs=RT[:, hs],
                                 start=True, stop=True)
                        RTn, RTnB = gd["RT%d" % ((lvl + 1) % 2)]
                        k.op("dve", V.tensor_tensor, R=[p3B, RTB], W=[RTnB], out=RTn[:, :], in0=p3[:, :], in1=RT[:, :],
                             op=ALU.add)
                        tick(2)
                        RT, RTB = RTn, RTnB
                        P, PB = Pn, PnB
                        if lvl < 5:
                            PT_, PTB = PTn, PTnB
                    if stop in ("A3c", "A3b3"):
                        break
                    pw, pwB = psum()
                    pu_, puB_ = psum()
                    for h in range(4):
                        hs = slice(h * 128, (h + 1) * 128)
                        k.op("pe", PE.matmul, R=[KbdB, RTB], W=[pwB], out=pw[:, hs], lhsT=Kbd[:, hs], rhs=RT[:, hs],
                             start=True, stop=True)
                    for h in range(4):
                        hs = slice(h * 128, (h + 1) * 128)
                        k.op("pe", PE.matmul, R=[VbB, RTB], W=[puB_], out=pu_[:, hs], lhsT=RT[:, hs], rhs=Vb[:, hs],
                             start=True, stop=True)
                    WT, WTB = scratch()
                    U, UB = scratch()
                    k.op("act", ACT.copy, R=[pwB], W=[WTB], out=WT[:, :], in_=pw[:, :])
                    k.op("act", ACT.copy, R=[puB_], W=[UB], out=U[:, :], in_=pu_[:, :])
                    tick()
                    tg = NTS * s_i + ch
                    Sc, ScB = Sst[tg % 2]
                    Sn, SnB = Sst[(tg + 1) % 2]
                    pws, pwsB = psum()
                    for h in range(4):
                        hs = slice(h * 128, (h + 1) * 128)
                        k.op("pe", PE.matmul, R=[WTB, ScB], W=[pwsB], out=pws[:, hs], lhsT=WT[:, hs], rhs=Sc[:, h, :],
                             start=True, stop=True)
                    Vn, VnB = scratch()
                    k.op("dve", V.scalar_tensor_tensor, R=[UB, pwsB], W=[VnB], out=Vn[:, :], in0=pws[:, :], scalar=-1.0,
                         in1=U[:, :], op0=ALU.mult, op1=ALU.add)
                    po, poB = psum()
                    for h in range(4):
                        hs = slice(h * 128, (h + 1) * 128)
                        k.op("pe", PE.matmul, R=[ScB, qdB], W=[poB], out=po[:, hs], lhsT=Sc[:, h, :], rhs=qd[:, hs],
                             start=True, stop=False)
                        k.op("pe", PE.matmul, R=[VnB, QKTB], W=[poB], out=po[:, hs], lhsT=Vn[:, hs], rhs=QKT[:, hs],
                             start=False, stop=True)
                    ps_, psB_ = psum()
                    for h in range(4):
                        hs = slice(h * 128, (h + 1) * 128)
                        k.op("pe", PE.matmul, R=[KdecB, VnB], W=[psB_], out=ps_[:, hs], lhsT=Kdec[:, hs], rhs=Vn[:, hs],
                             start=True, stop=True)
                    k.op("dve", V.tensor_tensor, R=[ScB, eblB], W=[SnB], out=Sn[:, :, :], in0=Sc[:, :, :],
                         in1=bc(ebl[:, 0:4].unsqueeze(2), [128, 4, 128]), op=ALU.mult)
                    k.op("dve", V.tensor_tensor, R=[SnB, psB_], W=[SnB], out=Sn[:, :, :], in0=Sn[:, :, :],
                         in1=v3(ps_[:, :]), op=ALU.add)
                    tick()
                    o32, o32B = scratch()
                    osq, osqB = scratch()
                    k.op("act", ACT.copy, R=[poB], W=[o32B], out=o32[:, :], in_=po[:, :])
                    k.op("act", ACT.activation, R=[poB], W=[osqB], out=osq[:, :], in_=po[:, :], func=AF.Square)
                    pm_, pmB_ = psum()
                    k.op("pe", PE.matmul, R=[o128B, osqB], W=[pmB_], out=pm_[:, :], lhsT=ones128[:, :], rhs=osq[:, :],
                         start=True, stop=True)
                    k.op("act", ACT.activation, R=[pmB_, epsB], W=[osqB], out=osq[:, :], in_=pm_[:, :], func=AF.Sqrt,
                         bias=epsc[:, 0:1])
                    k.op("dve", V.reciprocal, R=[osqB], W=[osqB], out=osq[:, :], in_=osq[:, :])
                    k.op("dve", V.scalar_tensor_tensor, R=[o32B, osqB, gncB], W=[o32B], out=o32[:, :], in0=o32[:, :],
                         scalar=gnc[:, 0:1], in1=osq[:, :], op0=ALU.mult, op1=ALU.mult)
                    k.op("dve", V.tensor_tensor, R=[o32B, zsB], W=[yTB], out=yT[:, 0:4, cs], in0=v3(o32[:, :]),
                         in1=zs[:, :, cs], op=ALU.mult)

                if stop in ("A3", "A3a", "A3b", "A3c", "A3b1", "A3b2", "A3b3", "A3b1a", "A3b1b"):
                    break
                for _ in dsa_g:
                    pass
                if stop == "A4":
                    break
                for j in range(NTS):
                    xt, xtB = xts[j]
                    t = tiles[j]
                    for n in range(2):
                        pt, pB = psum()
                        for c in range(8):
                            k.op("pe", PE.matmul, R=[yTB, WAB], W=[pB], out=pt[:, :],
                                 lhsT=yT[:, c, j * 128:(j + 1) * 128], rhs=w_out[:, c, n * 512:(n + 1) * 512],
                                 start=(c == 0), stop=(c == 7))
                        k.op("dve", V.tensor_tensor, R=[pB, xtB], W=[xtB], out=xt[:, n * 512:(n + 1) * 512],
                             in0=pt[:, :], in1=xt[:, n * 512:(n + 1) * 512], op=ALU.add)
                    k.dma("sp", out_d[t * 128:(t + 1) * 128, :], xt[:, :], xtB, R=[xtB], W=[xdB[t]])
        k.barrier()
        if stop in ("A", "W", "A1", "A2", "A3", "A3a", "A3b", "A3c", "A4", "A3b1", "A3b2", "A3b3", "A3b1a", "A3b1b"):
            break

        with contextlib.ExitStack() as pa:
            def sba(name, shape, dt=F32):
                t = pa.enter_context(nc.sbuf_tensor("%s_x%d" % (name, l), list(shape), dt))
                return t, Buf(name)
            WA, WAB = sba("WA", [128, 16384], BF16)
            for i in range(2):
                stage[i] = sba("stgX%d" % i, [128, 2048])
            wq = WA[:, 0:8 * 512].rearrange("p (c n) -> p c n", c=8)
            wkv = WA[:, 4096:4096 + 8 * 1024].rearrange("p (c n) -> p c n", c=8)
            wo = WA[:, 12288:12288 + 4 * 1024].rearrange("p (c n) -> p c n", c=4)
            load_gcol(norm_cross_d[l])
            for c in range(8):
                cast_rows(wq[:, c, :], WAB, wq_d[l, c * 128:(c + 1) * 128, :], 512, gcol[:, c:c + 1])
            load_gcol(norm_mem_d)
            for c in range(8):
                cast_rows(wkv[:, c, :], WAB, wkv_d[l, c * 128:(c + 1) * 128, :], 1024, gcol[:, c:c + 1])
            for c in range(4):
                cast_rows(wo[:, c, :], WAB, wo_d[l, c * 128:(c + 1) * 128, :], D)
            gxq, gxqB = sba("gxq", [128, 128])
            gxk, gxkB = sba("gxk", [128, 128])
            k.dma("sp", gxq[:, :], xq_norm_d[l].partition_broadcast(128), gxqB, W=[gxqB])
            k.dma("sp", gxk[:, :], xk_norm_d[l].partition_broadcast(128), gxkB, W=[gxkB])
            k.op("dve", V.tensor_scalar, R=[gxqB], W=[gxqB], out=gxq[:, :], in0=gxq[:, :], scalar1=128 ** -0.5,
                 scalar2=None, op0=ALU.mult)
            kmT, kmTB = sba("kmT", [128, 4, MEM], BF16)
            vm1, vm1B = sba("vm1", [128, 2, 4, 129], BF16)
            k.op("pool", POOL.memset, W=[vm1B], ap=vm1[:, :, :, :], constant=1.0)
            scr = [sba("xscr%d" % i, [128, 512]) for i in range(4)]
            scb = [sba("xscb%d" % i, [128, 1024], BF16) for i in range(4)]
            sm = [sba("xsm%d" % i, [128, 16]) for i in range(4)]
            cx = [0, 0, 0]

            def scratch():
                cx[0] += 1
                return scr[cx[0] % 4]

            def scratchb():
                cx[1] += 1
                return scb[cx[1] % 4]

            def small():
                cx[2] += 1
                return sm[cx[2] % 4]

            def v3(ap, a=4):
                return ap.rearrange("p (a b) -> p a b", a=a)

            def head_rms(pt, pB, gtile, gB, dst3, dstB):
                sq, sqB = scratch()
                k.op("act", ACT.activation, R=[pB], W=[sqB], out=sq[:, :], in_=pt[:, :], func=AF.Square)
                ss, ssB = small()
                k.op("dve", V.tensor_reduce, R=[sqB], W=[ssB], out=ss[:, 0:4], in_=v3(sq[:, :]), axis=AX.X, op=ALU.add)
                k.op("dve", V.tensor_scalar, R=[ssB], W=[ssB], out=ss[:, 4:8], in0=ss[:, 0:4], scalar1=1.0 / 128,
                     scalar2=EPS, op0=ALU.mult, op1=ALU.add)
                k.op("act", ACT.activation, R=[ssB], W=[ssB], out=ss[:, 4:8], in_=ss[:, 4:8], func=AF.Sqrt)
                k.op("dve", V.reciprocal, R=[ssB], W=[ssB], out=ss[:, 8:12], in_=ss[:, 4:8])
                k.op("dve", V.tensor_tensor, R=[pB, ssB], W=[sqB], out=v3(sq[:, :]), in0=v3(pt[:, :]),
                     in1=bc(ss[:, 8:12].unsqueeze(2), [128, 4, 128]), op=ALU.mult)
                k.op("dve", V.tensor_tensor, R=[sqB, gB], W=[dstB], out=dst3, in0=v3(sq[:, :]),
                     in1=bc(gtile[:, :].unsqueeze(1), [128, 4, 128]), op=ALU.mult)

            for mt in range(2):
                xt, xtB = xts[mt]
                k.dma("sp", xt[:, :], mem_d[mt * 128:(mt + 1) * 128, :], xtB, W=[xtB])
                norm_T(xt, xtB, 0)
                pk_, pkB_ = psum()
                pv_, pvB_ = psum()
                for c in range(8):
                    k.op("pe", PE.matmul, R=[hTB, WAB], W=[pkB_], out=pk_[:, :], lhsT=hT[:, c, 0:128],
                         rhs=wkv[:, c, 0:512], start=(c == 0), stop=(c == 7))
                for c in range(8):
                    k.op("pe", PE.matmul, R=[hTB, WAB], W=[pvB_], out=pv_[:, :], lhsT=hT[:, c, 0:128],
                         rhs=wkv[:, c, 512:1024], start=(c == 0), stop=(c == 7))
                kn, knB = scratchb()
                head_rms(pk_, pkB_, gxk, gxkB, v3(kn[:, 0:512]), knB)
                ptk, ptkB = psum()
                for h in range(4):
                    k.op("pe", PE.transpose, R=[knB, identbB], W=[ptkB], out=bfv(ptk)[:, h * 128:(h + 1) * 128],
                         in_=kn[:, h * 128:(h + 1) * 128], identity=ident_b[:, :])
                k.op("act", ACT.copy, R=[ptkB], W=[kmTB], out=kmT[:, :, mt * 128:(mt + 1) * 128],
                     in_=v3(bfv(ptk)[:, 0:512]))
                k.op("act", ACT.copy, R=[pvB_], W=[vm1B], out=vm1[:, mt, :, 0:128], in_=v3(pv_[:, :]))

            for t in range(NT):
                xt, xtB = xts[t % 2]
                k.dma("sp", xt[:, :], out_d[t * 128:(t + 1) * 128, :], xtB, R=[xdB[t]], W=[xtB])
                norm_T(xt, xtB, 0)
                pq, pqB = psum()
                for c in range(8):
                    k.op("pe", PE.matmul, R=[hTB, WAB], W=[pqB], out=pq[:, :], lhsT=hT[:, c, 0:128], rhs=wq[:, c, :],
                         start=(c == 0), stop=(c == 7))
                qn, qnB = scratchb()
                head_rms(pq, pqB, gxq, gxqB, v3(qn[:, 0:512]), qnB)
                ptq, ptqB = psum()
                for h in range(4):
                    k.op("pe", PE.transpose, R=[qnB, identbB], W=[ptqB], out=bfv(ptq)[:, h * 128:(h + 1) * 128],
                         in_=qn[:, h * 128:(h + 1) * 128], identity=ident_b[:, :])
                qT, qTB = scratchb()
                k.op("act", ACT.copy, R=[ptqB], W=[qTB], out=qT[:, 0:512], in_=bfv(ptq)[:, 0:512])
                pT_, pTB = scratchb()
                for mt in range(2):
                    pl, plB = psum()
                    for h in range(4):
                        k.op("pe", PE.matmul, R=[kmTB, qTB], W=[plB], out=pl[:, h * 128:(h + 1) * 128],
                             lhsT=kmT[:, h, mt * 128:(mt + 1) * 128], rhs=qT[:, h * 128:(h + 1) * 128], start=True,
                             stop=True)
                    k.op("act", ACT.activation, R=[plB], W=[pTB], out=pT_[:, mt * 512:(mt + 1) * 512], in_=pl[:, :],
                         func=AF.Exp)
                on, onB = scratchb()
                for hh in range(2):
                    po, poB = psum()
                    for h2 in range(2):
                        h = hh * 2 + h2
                        for mt in range(2):
                            k.op("pe", PE.matmul, R=[pTB, vm1B], W=[poB], out=po[:, h2 * 129:(h2 + 1) * 129],
                                 lhsT=pT_[:, mt * 512 + h * 128:mt * 512 + (h + 1) * 128], rhs=vm1[:, mt, h, :],
                                 start=(mt == 0), stop=(mt == 1))
                    po3 = po[:, 0:258].rearrange("p (a b) -> p a b", a=2)
                    rd, rdB = small()
                    k.op("dve", V.reciprocal, R=[poB], W=[rdB], out=rd[:, 0:2], in_=po3[:, :, 128])
                    k.op("dve", V.tensor_tensor, R=[poB, rdB], W=[onB],
                         out=on[:, hh * 256:(hh + 1) * 256].rearrange("p (a b) -> p a b", a=2), in0=po3[:, :, 0:128],
                         in1=bc(rd[:, 0:2].unsqueeze(2), [128, 2, 128]), op=ALU.mult)
                pto, ptoB = psum()
                for h in range(4):
                    k.op("pe", PE.transpose, R=[onB, identbB], W=[ptoB], out=bfv(pto)[:, h * 128:(h + 1) * 128],
                         in_=on[:, h * 128:(h + 1) * 128], identity=ident_b[:, :])
                oT, oTB = scratchb()
                k.op("act", ACT.copy, R=[ptoB], W=[oTB], out=oT[:, 0:512], in_=bfv(pto)[:, 0:512])
                for n in range(2):
                    pt, pB = psum()
                    for h in range(4):
                        k.op("pe", PE.matmul, R=[oTB, WAB], W=[pB], out=pt[:, :], lhsT=oT[:, h * 128:(h + 1) * 128],
                             rhs=wo[:, h, n * 512:(n + 1) * 512], start=(h == 0), stop=(h == 3))
                    k.op("dve", V.tensor_tensor, R=[pB, xtB], W=[xtB], out=xt[:, n * 512:(n + 1) * 512], in0=pt[:, :],
                         in1=xt[:, n * 512:(n + 1) * 512], op=ALU.add)
                k.dma("sp", out_d[t * 128:(t + 1) * 128, :], xt[:, :], xtB, R=[xtB], W=[xdB[t]])
        k.barrier()
        if stop == "A2":
            break

        with contextlib.ExitStack() as pa:
            def sba(name, shape, dt=F32):
                t = pa.enter_context(nc.sbuf_tensor("%s_m%d" % (name, l), list(shape), dt))
                return t, Buf(name)
            ST = ST_B
            NST = S // ST
            WA, WAB = sba("WA", [128, 65536], BF16)
            for i in range(2):
                stage[i] = sba("stgM%d" % i, [128, 2048])
            w1 = WA[:, 0:8 * 4096].rearrange("p (c n) -> p c n", c=8)
            w2 = WA[:, 32768:32768 + 32 * 1024].rearrange("p (c n) -> p c n", c=32)
            load_gcol(norm_mlp_d[l])
            for c in range(8):
                cast_rows(w1[:, c, :], WAB, w1_d[l, c * 128:(c + 1) * 128, :], 4096, gcol[:, c:c + 1])
            for c in range(32):
                cast_rows(w2[:, c, :], WAB, w2_d[l, c * 128:(c + 1) * 128, :], D)
            aT, aTB = sba("aT", [128, 32, ST], BF16)
            rrs = [sba("rr%d" % i, [128, ST]) for i in range(3)]
            for s_i in range(NST):
                t0 = s_i * ST
                for j in range(2):
                    xt, xtB = xts[j]
                    k.dma("sp", xt[:, :], out_d[t0 + j * 128:t0 + (j + 1) * 128, :], xtB, R=[xdB[2 * s_i + j]],
                          W=[xtB])
                    norm_T(xt, xtB, j * 128)
                for f in range(32):
                    pt, pB = psum()
                    for c in range(8):
                        k.op("pe", PE.matmul, R=[WAB, hTB], W=[pB], out=pt[:, :ST], lhsT=w1[:, c, f * 128:(f + 1) * 128],
                             rhs=hT[:, c, :], start=(c == 0), stop=(c == 7))
                    rr_, rrB = rrs[f % 3]
                    k.op("act", ACT.activation, R=[pB], W=[rrB], out=rr_[:, :], in_=pt[:, :ST], func=AF.Relu)
                    e = "pool" if f % 2 == 0 else "dve"
                    k.op(e, k.eng[e].tensor_tensor, R=[rrB], W=[aTB], out=aT[:, f, :], in0=rr_[:, :], in1=rr_[:, :],
                         op=ALU.mult)
                for j in range(2):
                    xt, xtB = xts[j]
                    t = 2 * s_i + j
                    for n in range(2):
                        pt, pB = psum()
                        for f in range(32):
                            k.op("pe", PE.matmul, R=[aTB, WAB], W=[pB], out=pt[:, :],
                                 lhsT=aT[:, f, j * 128:(j + 1) * 128], rhs=w2[:, f, n * 512:(n + 1) * 512],
                                 start=(f == 0), stop=(f == 31))
                        k.op("dve", V.tensor_tensor, R=[pB, xtB], W=[xtB], out=xt[:, n * 512:(n + 1) * 512],
                             in0=pt[:, :], in1=xt[:, n * 512:(n + 1) * 512], op=ALU.add)
                    k.dma("sp", out_d[t * 128:(t + 1) * 128, :], xt[:, :], xtB, R=[xtB], W=[xdB[t]])
        k.barrier()
    k.finish()
    es.close()
    return nc, k


_INPUT_NAMES = ["x", "mem", "norm_mix", "w_in", "gdn_conv", "gdn_a_log", "gdn_dt_bias", "gdn_norm", "dsa_w_qb",
                "dsa_w_qi", "dsa_w_kvb", "dsa_q_norm", "dsa_k_norm", "conv_dw", "conv_dw_b", "conv_ln_g",
                "conv_ln_b", "w_out", "norm_mem", "norm_cross", "xa_wq", "xa_wkv", "xa_q_norm", "xa_k_norm",
                "xa_wo", "norm_mlp", "mlp_w1", "mlp_w2"]


def run(inputs, S=4096, NL=2, stop=None, ncores=8, trace=False):
    nc, kk = build(S=S, NL=NL, stop=stop)
    shared = {n: np.ascontiguousarray(np.asarray(inputs[n], dtype=np.float32)) for n in _INPUT_NAMES
              if n not in ("x", "mem")}
    x = np.asarray(inputs["x"], dtype=np.float32)
    mem = np.asarray(inputs["mem"], dtype=np.float32)
    in_maps = []
    for b in range(ncores):
        m = dict(shared)
        m["x"] = np.ascontiguousarray(x[b, :S])
        m["mem"] = np.ascontiguousarray(mem[b])
        in_maps.append(m)
    res = run_bass_kernel_spmd(nc, in_maps, core_ids=list(range(ncores)), trace=trace)
    out = np.stack([np.asarray(r["out"]) for r in res.results], axis=0)
    return out, res


def kernel(**inputs):
    out, _ = run(inputs)
    return out.astype(np.float32)
```
